# Optimizing a Trainium2 kernel written in Bass

```python
import jax, jax.numpy as jnp
from jax import lax
import numpy as np

D_MODEL = 1024
BATCH = 16
SEQ = 2048
DEPTH = 2

HEAD_DIM = 64
Q_BLOCK = 128
NEG_INF = -1e30
MLA_HEADS = 8
MLA_Q_RANK = 512
MLA_KV_RANK = 256
MLA_NOPE = 64
MLA_ROPE = 32
MLA_V = 64
ROPE_THETA = 10000.0
DIL_PATTERNS = ((128, 1), (512, 4), (2048, 16))
DIL_GROUPS = 3
DIL_HPG = 4
DIL_HEADS = DIL_GROUPS * DIL_HPG
WIN_HEADS = 8
WIN_KV_HEADS = 2
WIN_RADIUS = 128
WIN_BLOCK = 128
MLA_COLS = MLA_Q_RANK + MLA_KV_RANK + MLA_ROPE
DIL_COLS = 3 * DIL_HEADS * HEAD_DIM
WIN_COLS = (WIN_HEADS + 2 * WIN_KV_HEADS) * HEAD_DIM
GATE_COLS = 3 * D_MODEL
IN_COLS = MLA_COLS + DIL_COLS + WIN_COLS + GATE_COLS
MLA_OUT = MLA_HEADS * MLA_V
DIL_OUT = DIL_HPG * HEAD_DIM
WIN_OUT = WIN_HEADS * HEAD_DIM
N_EXPERT_GROUPS = 4
EXPERTS_PER_GROUP = 8
N_EXPERTS = N_EXPERT_GROUPS * EXPERTS_PER_GROUP
TOP_K = 2
D_EXPERT = 384
MOE_BLOCK = 128

kernel_name = 'hybrid_mla_dilated_window_hmoe_encoder'


def rms_norm(x, g, eps=1e-6):
    xf = x.astype(jnp.float32)
    y = xf * lax.rsqrt(jnp.mean(xf * xf, axis=-1, keepdims=True) + eps)
    return (y * g.astype(jnp.float32)).astype(x.dtype)


def alibi_slopes(n):
    return 2.0 ** (-8.0 * jnp.arange(1, n + 1, dtype=jnp.float32) / n)


def apply_rope(x, pos):
    half = x.shape[-1] // 2
    inv_freq = ROPE_THETA ** (-jnp.arange(half, dtype=jnp.float32) / half)
    ang = pos.astype(jnp.float32)[..., None] * inv_freq
    ang = ang.reshape(ang.shape[:2] + (1,) * (x.ndim - 3) + (half,))
    cos, sin = jnp.cos(ang), jnp.sin(ang)
    xf = x.astype(jnp.float32)
    x1, x2 = xf[..., :half], xf[..., half:]
    return jnp.concatenate([x1 * cos - x2 * sin, x1 * sin + x2 * cos], axis=-1).astype(x.dtype)


def to_blocks(t, block):
    B, S = t.shape[:2]
    return jnp.moveaxis(t.reshape(B, S // block, block, *t.shape[2:]), 1, 0)


def from_blocks(o):
    o = jnp.moveaxis(o, 0, 1)
    return o.reshape(o.shape[0], o.shape[1] * o.shape[2], -1)


def mla_mixer(c_q, c_kv, k_rope, pos, g_cq, w_uq, g_ckv, w_ukv, g_q, g_k):
    B, S, _ = c_q.shape
    q = (rms_norm(c_q, g_cq) @ w_uq).reshape(B, S, MLA_HEADS, MLA_NOPE + MLA_ROPE)
    kv = (rms_norm(c_kv, g_ckv) @ w_ukv).reshape(B, S, MLA_HEADS, MLA_NOPE + MLA_V)
    q_nope = rms_norm(q[..., :MLA_NOPE], g_q[:MLA_NOPE])
    q_rope = apply_rope(rms_norm(q[..., MLA_NOPE:], g_q[MLA_NOPE:]), pos)
    k_nope = rms_norm(kv[..., :MLA_NOPE], g_k[:MLA_NOPE])
    v = kv[..., MLA_NOPE:]
    k_rope = apply_rope(rms_norm(k_rope, g_k[MLA_NOPE:]), pos)
    scale = (MLA_NOPE + MLA_ROPE) ** -0.5

    def attend(blk):
        qn, qr = blk
        s = (jnp.einsum('bqhd,bkhd->bhqk', qn, k_nope)
             + jnp.einsum('bqhd,bkd->bhqk', qr, k_rope)).astype(jnp.float32) * scale
        p = jax.nn.softmax(s, axis=-1)
        return jnp.einsum('bhqk,bkhd->bqhd', p.astype(v.dtype), v)

    o = lax.map(attend, (to_blocks(q_nope, Q_BLOCK), to_blocks(q_rope, Q_BLOCK)))
    return from_blocks(o)


def dilated_group(q, k, v, pos, window, dil, slope):
    B, S = q.shape[:2]
    n_side = (window // 2) // dil
    offsets = jnp.arange(-n_side, n_side + 1, dtype=jnp.int32) * dil
    starts = jnp.arange(S // Q_BLOCK, dtype=jnp.int32) * Q_BLOCK
    scale = HEAD_DIM ** -0.5

    def attend(args):
        qb, start = args
        q_idx = start + jnp.arange(Q_BLOCK, dtype=jnp.int32)
        k_idx = q_idx[:, None] + offsets[None, :]
        valid = (k_idx >= 0) & (k_idx < S)
        k_idx = jnp.clip(k_idx, 0, S - 1)
        kb = jnp.take(k, k_idx, axis=1)
        vb = jnp.take(v, k_idx, axis=1)
        pos_q = lax.dynamic_slice_in_dim(pos, start, Q_BLOCK, axis=1)
        pos_k = jnp.take(pos, k_idx, axis=1)
        dist = jnp.abs(pos_q[:, :, None] - pos_k).astype(jnp.float32)
        s = jnp.einsum('bqhd,bqjhd->bhqj', qb, kb).astype(jnp.float32) * scale
        s = s - slope[None, :, None, None] * dist[:, None]
        s = jnp.where(valid[None, None], s, NEG_INF)
        lse = jax.nn.logsumexp(s, axis=-1)
        p = jnp.exp(s - lse[..., None])
        o = jnp.einsum('bhqj,bqjhd->bqhd', p.astype(vb.dtype), vb)
        return o, lse

    return lax.map(attend, (to_blocks(q, Q_BLOCK), starts))


def dilated_mixer(qkv, pos, g_q, g_k):
    B, S, _ = qkv.shape
    qkv = qkv.reshape(B, S, 3, DIL_GROUPS, DIL_HPG, HEAD_DIM)
    q = rms_norm(qkv[:, :, 0], g_q)
    k = rms_norm(qkv[:, :, 1], g_k)
    v = qkv[:, :, 2]
    slopes = alibi_slopes(DIL_HEADS).reshape(DIL_GROUPS, DIL_HPG)
    outs, lses = [], []
    for gi, (window, dil) in enumerate(DIL_PATTERNS):
        o, lse = dilated_group(q[:, :, gi], k[:, :, gi], v[:, :, gi], pos, window, dil, slopes[gi])
        outs.append(o)
        lses.append(lse)
    outs = jnp.stack(outs)
    w = jax.nn.softmax(jnp.stack(lses), axis=0)
    o = jnp.einsum('gnbhq,gnbqhd->nbqhd', w.astype(outs.dtype), outs)
    return from_blocks(o)


def window_mixer(qkv, pos, g_q, g_k, sink):
    B, S, _ = qkv.shape
    rep = WIN_HEADS // WIN_KV_HEADS
    nq, nk = WIN_HEADS * HEAD_DIM, WIN_KV_HEADS * HEAD_DIM
    q = rms_norm(qkv[..., :nq].reshape(B, S, WIN_KV_HEADS, rep, HEAD_DIM), g_q)
    k = rms_norm(qkv[..., nq:nq + nk].reshape(B, S, WIN_KV_HEADS, HEAD_DIM), g_k)
    v = qkv[..., nq + nk:].reshape(B, S, WIN_KV_HEADS, HEAD_DIM)
    pad = ((0, 0), (WIN_BLOCK, WIN_BLOCK), (0, 0), (0, 0))
    kp, vp = jnp.pad(k, pad), jnp.pad(v, pad)
    pp = jnp.pad(pos, ((0, 0), (WIN_BLOCK, WIN_BLOCK)))
    slopes = alibi_slopes(WIN_HEADS).reshape(WIN_KV_HEADS, rep)
    sink_l = sink.astype(jnp.float32).reshape(1, WIN_KV_HEADS, rep, 1, 1)
    starts = jnp.arange(S // WIN_BLOCK, dtype=jnp.int32) * WIN_BLOCK
    scale = HEAD_DIM ** -0.5
    band = 3 * WIN_BLOCK

    def attend(args):
        qb, start = args
        kb = lax.dynamic_slice_in_dim(kp, start, band, axis=1)
        vb = lax.dynamic_slice_in_dim(vp, start, band, axis=1)
        pos_k = lax.dynamic_slice_in_dim(pp, start, band, axis=1)
        pos_q = lax.dynamic_slice_in_dim(pos, start, WIN_BLOCK, axis=1)
        q_idx = start + jnp.arange(WIN_BLOCK, dtype=jnp.int32)
        k_idx = start - WIN_BLOCK + jnp.arange(band, dtype=jnp.int32)
        valid = ((jnp.abs(q_idx[:, None] - k_idx[None, :]) <= WIN_RADIUS)
                 & (k_idx >= 0)[None, :] & (k_idx < S)[None, :])
        dist = jnp.abs(pos_q[:, :, None] - pos_k[:, None, :]).astype(jnp.float32)
        s = jnp.einsum('bqgrd,bkgd->bgrqk', qb, kb).astype(jnp.float32) * scale
        s = s - slopes[None, :, :, None, None] * dist[:, None, None]
        s = jnp.where(valid, s, NEG_INF)
        m = jnp.maximum(jnp.max(s, axis=-1, keepdims=True), sink_l)
        e = jnp.exp(s - m)
        p = e / (jnp.sum(e, axis=-1, keepdims=True) + jnp.exp(sink_l - m))
        return jnp.einsum('bgrqk,bkgd->bqgrd', p.astype(vb.dtype), vb)

    o = lax.map(attend, (to_blocks(q, WIN_BLOCK), starts))
    return from_blocks(o)


def hybrid_mixer(h, pos, w_in, g_cq, w_uq, g_ckv, w_ukv, g_q_mla, g_k_mla, g_q_dil, g_k_dil,
                 g_q_win, g_k_win, sink_win, w_br_mla, w_br_dil, w_br_win, w_out):
    cols = h @ w_in
    sizes = [MLA_Q_RANK, MLA_KV_RANK, MLA_ROPE, DIL_COLS, WIN_COLS]
    bounds, acc = [], 0
    for sz in sizes:
        acc += sz
        bounds.append(acc)
    c_q, c_kv, k_rope, dil_qkv, win_qkv, gate_cols = jnp.split(cols, bounds, axis=-1)
    g_mla, g_dil, g_win = jnp.split(jax.nn.sigmoid(gate_cols), 3, axis=-1)
    y_mla = mla_mixer(c_q, c_kv, k_rope, pos, g_cq, w_uq, g_ckv, w_ukv, g_q_mla, g_k_mla) @ w_br_mla
    y_dil = dilated_mixer(dil_qkv, pos, g_q_dil, g_k_dil) @ w_br_dil
    y_win = window_mixer(win_qkv, pos, g_q_win, g_k_win, sink_win) @ w_br_win
    return (g_mla * y_mla + g_dil * y_dil + g_win * y_win) @ w_out


def hier_moe(h, w_gr, b_gr, w_er, b_er, w1, w3, w2):
    B, S, D = h.shape
    t = h.reshape(-1, D)
    T = t.shape[0]
    g_prob = jax.nn.softmax((t @ w_gr).astype(jnp.float32) + b_gr.astype(jnp.float32), axis=-1)
    g_w, g_idx = lax.top_k(g_prob, 1)
    e_logits = ((t @ w_er).astype(jnp.float32) + b_er.astype(jnp.float32)).reshape(
        T, N_EXPERT_GROUPS, EXPERTS_PER_GROUP)
    e_logits = jnp.take_along_axis(e_logits, g_idx[:, :, None], axis=1)[:, 0]
    e_w, e_idx = lax.top_k(jax.nn.softmax(e_logits, axis=-1), TOP_K)
    gate = g_w * e_w / jnp.sum(e_w, axis=-1, keepdims=True)
    expert = g_idx * EXPERTS_PER_GROUP + e_idx
    flat_e, flat_gate = expert.reshape(-1), gate.reshape(-1)
    n_assign = flat_e.shape[0]
    order = jnp.argsort(flat_e)
    sorted_e = flat_e[order]
    token = order // TOP_K
    counts = jnp.bincount(flat_e, length=N_EXPERTS)
    padded = (counts + MOE_BLOCK - 1) // MOE_BLOCK * MOE_BLOCK
    start_sorted = jnp.cumsum(counts) - counts
    start_padded = jnp.cumsum(padded) - padded
    dest = start_padded[sorted_e] + jnp.arange(n_assign) - start_sorted[sorted_e]
    n_blocks = -(-n_assign // MOE_BLOCK) + N_EXPERTS
    rows = jnp.zeros((n_blocks * MOE_BLOCK, D), t.dtype).at[dest].set(t[token])
    block_start = jnp.arange(n_blocks) * MOE_BLOCK
    block_expert = jnp.minimum(
        jnp.sum(block_start[:, None] >= (start_padded + padded)[None, :], axis=1), N_EXPERTS - 1)

    def expert_ffn(args):
        xb, e = args
        return (jax.nn.silu(xb @ w1[e]) * (xb @ w3[e])) @ w2[e]

    y = lax.map(expert_ffn, (rows.reshape(n_blocks, MOE_BLOCK, D), block_expert)).reshape(-1, D)
    y = y[dest] * flat_gate[order][:, None].astype(y.dtype)
    return jnp.zeros_like(t).at[token].add(y).reshape(B, S, D)


def setup_inputs(seed: int = 0) -> dict:
    key = jax.random.key(seed)
    ks = iter(jax.random.split(key, 40))
    L, D = DEPTH, D_MODEL

    def nrm(shape, fan_in, s=1.0):
        return (s * fan_in ** -0.5) * jax.random.normal(next(ks), shape, jnp.float32)

    def gain(shape):
        return 1.0 + 0.05 * jax.random.normal(next(ks), shape, jnp.float32)

    x = jax.random.normal(next(ks), (BATCH, SEQ, D), jnp.float32)
    c = jax.random.normal(next(ks), (BATCH, D), jnp.float32)
    pos = (jnp.arange(SEQ, dtype=jnp.int32)[None, :]
           + jax.random.randint(next(ks), (BATCH, 1), 0, SEQ, dtype=jnp.int32))
    return {
        'x': x, 'c': c, 'pos': pos,
        'w_ada': nrm((L, D, 6 * D), D, 0.5),
        'b_ada': 0.02 * jax.random.normal(next(ks), (L, 6 * D), jnp.float32),
        'g_norm1': gain((L, D)),
        'w_in': nrm((L, D, IN_COLS), D),
        'g_cq': gain((L, MLA_Q_RANK)),
        'w_uq': nrm((L, MLA_Q_RANK, MLA_HEADS * (MLA_NOPE + MLA_ROPE)), MLA_Q_RANK),
        'g_ckv': gain((L, MLA_KV_RANK)),
        'w_ukv': nrm((L, MLA_KV_RANK, MLA_HEADS * (MLA_NOPE + MLA_V)), MLA_KV_RANK),
        'g_q_mla': gain((L, MLA_NOPE + MLA_ROPE)),
        'g_k_mla': gain((L, MLA_NOPE + MLA_ROPE)),
        'g_q_dil': gain((L, HEAD_DIM)),
        'g_k_dil': gain((L, HEAD_DIM)),
        'g_q_win': gain((L, HEAD_DIM)),
        'g_k_win': gain((L, HEAD_DIM)),
        'sink_win': 0.5 * jax.random.normal(next(ks), (L, WIN_HEADS), jnp.float32),
        'w_br_mla': nrm((L, MLA_OUT, D), MLA_OUT),
        'w_br_dil': nrm((L, DIL_OUT, D), DIL_OUT),
        'w_br_win': nrm((L, WIN_OUT, D), WIN_OUT),
        'w_out': nrm((L, D, D), D),
        'g_norm2': gain((L, D)),
        'w_gr': nrm((L, D, N_EXPERT_GROUPS), D),
        'b_gr': 0.01 * jax.random.normal(next(ks), (L, N_EXPERT_GROUPS), jnp.float32),
        'w_er': nrm((L, D, N_EXPERTS), D),
        'b_er': 0.01 * jax.random.normal(next(ks), (L, N_EXPERTS), jnp.float32),
        'w1': nrm((L, N_EXPERTS, D, D_EXPERT), D),
        'w3': nrm((L, N_EXPERTS, D, D_EXPERT), D),
        'w2': nrm((L, N_EXPERTS, D_EXPERT, D), D_EXPERT),
    }


def reference(x, c, pos, w_ada, b_ada, g_norm1, w_in, g_cq, w_uq, g_ckv, w_ukv, g_q_mla, g_k_mla,
              g_q_dil, g_k_dil, g_q_win, g_k_win, sink_win, w_br_mla, w_br_dil, w_br_win, w_out,
              g_norm2, w_gr, b_gr, w_er, b_er, w1, w3, w2):
    cond = jax.nn.silu(c)
    for l in range(DEPTH):
        mod = cond @ w_ada[l] + b_ada[l]
        sh1, sc1, gt1, sh2, sc2, gt2 = jnp.split(mod[:, None, :], 6, axis=-1)
        h = rms_norm(x, g_norm1[l]) * (1 + sc1) + sh1
        x = x + gt1 * hybrid_mixer(h, pos, w_in[l], g_cq[l], w_uq[l], g_ckv[l], w_ukv[l],
                                   g_q_mla[l], g_k_mla[l], g_q_dil[l], g_k_dil[l],
                                   g_q_win[l], g_k_win[l], sink_win[l],
                                   w_br_mla[l], w_br_dil[l], w_br_win[l], w_out[l])
        h = rms_norm(x, g_norm2[l]) * (1 + sc2) + sh2
        x = x + gt2 * hier_moe(h, w_gr[l], b_gr[l], w_er[l], b_er[l], w1[l], w3[l], w2[l])
    return x
```

```python
import math
from contextlib import ExitStack
import numpy as np
import ml_dtypes
import concourse.bass as bass
import concourse.mybir as mybir
from concourse.bass_utils import run_bass_kernel_spmd

F32 = mybir.dt.float32
BF16 = mybir.dt.bfloat16
I32 = mybir.dt.int32
ALU = mybir.AluOpType
AF = mybir.ActivationFunctionType
AX = mybir.AxisListType

D = 1024
S = 2048
NSEQ = 2
T = NSEQ * S
NT = T // 128
L = 2
IN_COLS = 6944
OFF_CQ, OFF_CKV, OFF_KR, OFF_DIL, OFF_WIN, OFF_GATE = 0, 512, 768, 800, 3104, 3872
NE = 32
DE = 384
NBLK = 2 * T // 128 + NE
NSLOT = NBLK * 128
EPS = 1e-6
BIG = 1.0e6
MLA_SCALE = 96 ** -0.5
HD_SCALE = 64 ** -0.5
DIL_R = (1, 2, 8)
DIL_PAT = ((128, 1), (512, 4), (2048, 16))
MASK_BASE = (0, 3, 8, 25)
NMASK = 28


def alibi(n):
    return [2.0 ** (-8.0 * (i + 1) / n) for i in range(n)]


class FW:
    def __init__(self, nc, same_engine_sync=True):
        self.nc = nc
        self.eng = {'pe': nc.tensor, 'act': nc.scalar, 'dve': nc.vector, 'pool': nc.gpsimd, 'sp': nc.sync}
        self.semobj = {}
        self.cnt = {}
        self.dcnt = {}
        for e in ['pe', 'act', 'dve', 'pool']:
            self.semobj[e] = nc.alloc_semaphore("s_" + e)
            self.cnt[e] = 0
        self.pool = [nc.alloc_semaphore(f"s_dma{i}") for i in range(84)]
        self.allsems = [self.semobj[e] for e in ['pe', 'act', 'dve', 'pool']] + list(self.pool)
        self.seen = {e: {} for e in self.eng}
        self.lastw = {}
        self.readers = {}
        self.same = same_engine_sync
        self.nwaits = 0
        self.ninst = 0

    def clear_all(self):
        for sm in self.allsems:
            self.nc.gpsimd.sem_clear(sm)
        self.nc.all_engine_barrier()

    def _wait(self, E, ev):
        key, val = ev
        if key == E and (E == 'pe' or not self.same):
            return
        if key in self.dcnt:
            val = self.dcnt[key]
        if self.seen[E].get(key, 0) >= val:
            return
        self.eng[E].wait_ge(self.semobj[key], val)
        self.seen[E][key] = val
        self.nwaits += 1

    def _deps(self, E, reads, writes):
        for t in reads:
            ev = self.lastw.get(t)
            if ev is not None:
                self._wait(E, ev)
        for t in writes:
            ev = self.lastw.get(t)
            if ev is not None:
                self._wait(E, ev)
            for ev in self.readers.get(t, ()):
                self._wait(E, ev)

    def _commit(self, ev, reads, writes):
        for t in reads:
            lst = self.readers.setdefault(t, [])
            lst.append(ev)
            if len(lst) > 16:
                d = {}
                for k, v in lst:
                    d[k] = max(d.get(k, 0), v)
                self.readers[t] = list(d.items())
        for t in writes:
            self.lastw[t] = ev
            self.readers[t] = []

    def op(self, E, fn, reads=(), writes=()):
        self._deps(E, reads, writes)
        inst = fn(self.eng[E])
        self.cnt[E] += 1
        inst.then_inc(self.semobj[E], 1)
        self.ninst += 1
        self._commit((E, self.cnt[E]), reads, writes)
        return inst

    def dma(self, Q, fn, reads=(), writes=(), sem=None):
        if sem is None:
            sem = 'd_' + (writes[0] if writes else reads[0])
        if sem not in self.semobj:
            self.semobj[sem] = self.pool.pop()
            self.dcnt[sem] = 0
        self._deps(Q, reads, writes)
        if Q == 'pool':
            hist = self.__dict__.setdefault('pool_hist', [])
            if len(hist) >= 2:
                self._wait('pool', hist[-2])
        inst = fn(self.eng[Q])
        self.dcnt[sem] += 16
        inst.then_inc(self.semobj[sem], 16)
        self.ninst += 1
        self._commit((sem, self.dcnt[sem]), reads, writes)
        if Q == 'pool':
            self.pool_hist.append((sem, self.dcnt[sem]))
            if len(self.pool_hist) > 4:
                self.pool_hist.pop(0)
        return inst

    def throttle(self, Q, sem, lag):
        v = self.dcnt.get(sem, 0) - 16 * lag
        if v > 0 and self.seen[Q].get(sem, 0) < v:
            self.eng[Q].wait_ge(self.semobj[sem], v)
            self.seen[Q][sem] = v
            self.nwaits += 1

    def barrier(self):
        for E in self.eng:
            for k, v in self.cnt.items():
                if v > 0:
                    self._wait(E, (k, v))
            for k, v in self.dcnt.items():
                if v > 0:
                    self._wait(E, (k, v))
        self.lastw = {}
        self.readers = {}


def host_consts():
    c = {}
    c['c_ident_f'] = np.eye(128, dtype=np.float32)
    c['c_ident_b'] = np.eye(128, dtype=np.float32).astype(ml_dtypes.bfloat16)
    c['c_ones_b'] = np.ones((128, 128), np.float32).astype(ml_dtypes.bfloat16)
    blk = np.zeros((128, 128), np.float32)
    blk[0:64, 0:64] = 1
    blk[64:96, 64:96] = 1
    c['c_blk96_b'] = blk.astype(ml_dtypes.bfloat16)
    j = np.arange(128)
    c['c_ustrict_b'] = (j[:, None] < j[None, :]).astype(np.float32).astype(ml_dtypes.bfloat16)
    masks = np.zeros((128, NMASK, 128), np.float32)
    k = np.arange(128)[:, None]
    q = np.arange(128)[None, :]
    for g in range(3):
        win, dil = DIL_PAT[g]
        R = DIL_R[g]
        for dl in range(-R, R + 1):
            delta = 128 * dl + k - q
            valid = (np.abs(delta) <= win // 2) & (delta % dil == 0)
            masks[:, MASK_BASE[g] + dl + R, :] = np.where(valid, 0.0, BIG)
    for dl in range(-1, 2):
        delta = 128 * dl + k - q
        valid = np.abs(delta) <= 128
        masks[:, MASK_BASE[3] + dl + 1, :] = np.where(valid, 0.0, BIG)
    c['c_masks'] = masks
    col = np.zeros((128, 8), np.float32)
    col[:, 0] = 1.0
    col[0:64, 0] = 1.0 / 64
    col[64:96, 0] = 1.0 / 32
    col[:, 1] = EPS
    invf = 10000.0 ** (-np.arange(16, dtype=np.float32) / 16)
    col[64:80, 2] = -invf
    col[80:96, 2] = invf
    col[64:80, 3] = invf
    col[80:96, 3] = invf
    col[:, 4] = 1.0 / 64
    col[:, 5] = np.arange(128)
    col[:, 6] = math.pi / 2
    col[:, 7] = 0.0
    c['c_col'] = col
    c['c_bstart'] = np.tile((np.arange(NBLK, dtype=np.float32) * 128)[None, :], (128, 1))
    return c


CONST_SPECS = [('c_ident_f', [128, 128], F32), ('c_ident_b', [128, 128], BF16), ('c_ones_b', [128, 128], BF16),
               ('c_blk96_b', [128, 128], BF16), ('c_ustrict_b', [128, 128], BF16),
               ('c_masks', [128, NMASK, 128], F32), ('c_col', [128, 8], F32), ('c_bstart', [128, NBLK], F32)]

IN_SPECS = [('x', [T, D], F32), ('cT', [128, 8, 2], F32), ('pos', [NSEQ, S], I32), ('posT', [128, NSEQ, 16], I32),
            ('w_ada', [L, D, 6 * D], F32), ('b_ada', [L, 1, 6 * D], F32), ('g1T', [L, 128, 8], F32),
            ('w_in', [L, D, IN_COLS], F32), ('gcqT', [L, 128, 4], F32), ('w_uq', [L, 512, 768], F32),
            ('gckvT', [L, 128, 2], F32), ('w_ukv', [L, 256, 1024], F32), ('pcol', [L, 128, 8], F32),
            ('sink', [L, 1, 8], F32), ('w_br_mla', [L, 512, D], F32), ('w_br_dil', [L, 256, D], F32),
            ('w_br_win', [L, 512, D], F32), ('w_out', [L, D, D], F32), ('g_norm2', [L, 1, D], F32),
            ('w_r', [L, D, 36], F32), ('b_r', [L, 1, 36], F32),
            ('w1', [L, NE, D, DE], F32), ('w3', [L, NE, D, DE], F32), ('w2', [L, NE, DE, D], F32)]


def host_layout(inp, core):
    b0 = core * NSEQ
    m = {}
    m['x'] = np.ascontiguousarray(inp['x'][b0:b0 + NSEQ].reshape(T, D))
    c = inp['c'][b0:b0 + NSEQ]
    m['cT'] = np.ascontiguousarray(c.reshape(NSEQ, 8, 128).transpose(2, 1, 0))
    pos = np.ascontiguousarray(inp['pos'][b0:b0 + NSEQ]).astype(np.int32)
    m['pos'] = pos
    m['posT'] = np.ascontiguousarray(pos.reshape(NSEQ, 16, 128).transpose(2, 0, 1))
    return m


def host_shared(inp):
    m = {}
    m['w_ada'] = inp['w_ada']
    m['b_ada'] = inp['b_ada'].reshape(L, 1, 6 * D)
    m['g1T'] = np.ascontiguousarray(inp['g_norm1'].reshape(L, 8, 128).transpose(0, 2, 1))
    m['w_in'] = inp['w_in']
    m['gcqT'] = np.ascontiguousarray(inp['g_cq'].reshape(L, 4, 128).transpose(0, 2, 1))
    m['w_uq'] = inp['w_uq']
    m['gckvT'] = np.ascontiguousarray(inp['g_ckv'].reshape(L, 2, 128).transpose(0, 2, 1))
    m['w_ukv'] = inp['w_ukv']
    pcol = np.zeros((L, 128, 8), np.float32)
    for l in range(L):
        gq, gk = inp['g_q_mla'][l], inp['g_k_mla'][l]
        pcol[l, 0:96, 0] = gq
        pcol[l, 64:80, 1] = gq[80:96]
        pcol[l, 80:96, 1] = gq[64:80]
        pcol[l, 0:96, 2] = gk
        pcol[l, 64:80, 3] = gk[80:96]
        pcol[l, 80:96, 3] = gk[64:80]
        pcol[l, 0:64, 4] = inp['g_q_dil'][l]
        pcol[l, 0:64, 5] = inp['g_k_dil'][l]
        pcol[l, 0:64, 6] = inp['g_q_win'][l]
        pcol[l, 0:64, 7] = inp['g_k_win'][l]
    m['pcol'] = pcol
    m['sink'] = inp['sink_win'].reshape(L, 1, 8)
    m['w_br_mla'] = inp['w_br_mla']
    m['w_br_dil'] = inp['w_br_dil']
    m['w_br_win'] = inp['w_br_win']
    m['w_out'] = inp['w_out']
    m['g_norm2'] = inp['g_norm2'].reshape(L, 1, D)
    m['w_r'] = np.ascontiguousarray(np.concatenate([inp['w_gr'], inp['w_er']], axis=2))
    m['b_r'] = np.ascontiguousarray(np.concatenate([inp['b_gr'], inp['b_er']], axis=1)).reshape(L, 1, 36)
    m['w1'] = inp['w1']
    m['w3'] = inp['w3']
    m['w2'] = inp['w2']
    m = {k: np.ascontiguousarray(v.astype(np.float32)) for k, v in m.items()}
    m.update(host_consts())
    return m


def build_program(stop_after=None, nlayers=L, same_engine_sync=True, tiny_moe=False, seqs=(0, 1), moe_only=False):
    nc = bass.Bass("TRN2", target_bir_lowering=False)
    fw = FW(nc, same_engine_sync=same_engine_sync)
    dr = {}
    for name, shape, dt in IN_SPECS + CONST_SPECS:
        if tiny_moe and name in ('w1', 'w3', 'w2'):
            shape = [L, 1] + list(shape[2:])
        dr[name] = nc.dram_tensor(name, list(shape), dt, kind="ExternalInput").ap()
    out = nc.dram_tensor("out", [T, D], F32, kind="ExternalOutput").ap()
    dbg = out[0:128, 0:512]
    hT_d = nc.dram_tensor("hT_d", [NSEQ, 128, 8, S], BF16, kind="Internal").ap()
    bc_d = nc.dram_tensor("bc_d", [3, NSEQ, 128, D], F32, kind="Internal").ap()
    rows_d = nc.dram_tensor("rows_d", [NSLOT, D], BF16, kind="Internal").ap()
    y_d = nc.dram_tensor("y_d", [NSLOT, D], F32, kind="Internal").ap()

    ps = [nc.alloc_psum_tensor(f"ps{i}", [128, 512], F32) for i in range(8)]

    def V(fn, r=(), w=()):
        return fw.op('dve', fn, r, w)

    def A(fn, r=(), w=()):
        return fw.op('act', fn, r, w)

    def G(fn, r=(), w=()):
        return fw.op('pool', fn, r, w)

    def PE(fn, r=(), w=()):
        return fw.op('pe', fn, r, w)

    def MM(o, lhsT, rhs, start, stop, r, w):
        return fw.op('pe', lambda e: e.matmul(o, lhsT=lhsT, rhs=rhs, start=start, stop=stop), r, w)

    def DMA(q, o, i, r, w, sem=None):
        return fw.dma(q, lambda e: e.dma_start(out=o, in_=i), r, w, sem=sem)

    uid = [0]

    def sb(es, name, shape, dt):
        uid[0] += 1
        return es.enter_context(nc.sbuf_tensor(f"{name}_{uid[0]}", list(shape), dt))

    top = ExitStack()
    cs = {}
    for name, shape, dt in CONST_SPECS:
        cs[name] = sb(top, "s_" + name, shape, dt)
        DMA('sp', cs[name][:], dr[name], [], [name])
    ident_f, ident_b, ones_b = cs['c_ident_f'], cs['c_ident_b'], cs['c_ones_b']
    blk96, ustrict, masks, col, bstart = cs['c_blk96_b'], cs['c_ustrict_b'], cs['c_masks'], cs['c_col'], cs['c_bstart']
    CONST_T = [n for n, _, _ in CONST_SPECS]
    ones_f = sb(top, "ones_f", [128, 128], F32)
    V(lambda e: e.memset(ones_f[:], 1.0), [], ['ones_f'])
    condT = sb(top, "condT", [128, 8, 2], F32)
    condrep = sb(top, "condrep", [128, 8, 2, 128], F32)
    modT = sb(top, "modT", [128, 16, 2], F32)
    A1 = sb(top, "A1", [128, 8, 2], F32)
    gt1b = sb(top, "gt1b", [128, NSEQ, D], F32)
    pcol = sb(top, "pcol", [128, 8], F32)
    g1T = sb(top, "g1T", [128, 8], F32)
    gcqT = sb(top, "gcqT", [128, 4], F32)
    gckvT = sb(top, "gckvT", [128, 2], F32)
    esink = sb(top, "esink", [128, 8], F32)
    poskT = sb(top, "poskT", [128, NSEQ, 16], F32)
    poskT_i = sb(top, "poskT_i", [128, NSEQ, 16], I32)
    DMA('sp', condT[:], dr['cT'], [], ['condT'])
    A(lambda e: e.activation(out=condT[:], in_=condT[:], func=AF.Silu), ['condT'], ['condT'])
    for b in range(NSEQ):
        V(lambda e: e.tensor_copy(out=condrep[:, :, b, :], in_=condT[:, :, b:b + 1].to_broadcast([128, 8, 128])),
          ['condT'], ['condrep'])
    DMA('sp', poskT_i[:], dr['posT'], [], ['poskT_i'])
    V(lambda e: e.tensor_copy(out=poskT[:], in_=poskT_i[:]), ['poskT_i'], ['poskT'])

    bank_rr = [0]

    def nb(lo=0, hi=8):
        b = lo + bank_rr[0] % (hi - lo)
        bank_rr[0] += 1
        return b

    xsrc = [dr['x']]

    def stage0(l):
        with ExitStack() as es:
            wblk = [sb(es, f"wada{i}", [128, 8, 512], F32) for i in range(2)]
            brow = sb(es, "brow", [1, 6 * D], F32)
            g2b = sb(es, "g2b", [128, D], F32)
            stg = [sb(es, f"stg{i}", [128, D], F32) for i in range(2)]
            DMA('sp', brow[:], dr['b_ada'][l], [], ['brow'])
            DMA('sp', g2b[:], dr['g_norm2'][l, 0].partition_broadcast(128), [], ['g2b'])
            DMA('sp', pcol[:], dr['pcol'][l], [], ['pcol'])
            DMA('sp', g1T[:], dr['g1T'][l], [], ['g1T'])
            DMA('sp', gcqT[:], dr['gcqT'][l], [], ['gcqT'])
            DMA('sp', gckvT[:], dr['gckvT'][l], [], ['gckvT'])
            DMA('sp', esink[:], dr['sink'][l, 0].partition_broadcast(128), [], ['esink'])
            A(lambda e: e.activation(out=esink[:], in_=esink[:], func=AF.Exp), ['esink'], ['esink'])
            wv = dr['w_ada'][l].rearrange("(kc p) n -> p kc n", p=128)
            for cb in range(12):
                wb = wblk[cb % 2]
                wt = f"wada{cb % 2}"
                DMA('sp', wb[:], wv[:, :, cb * 512:(cb + 1) * 512], [], [wt])
                if cb < 4:
                    for j in range(4):
                        bk = nb()
                        pt = f"ps{bk}"
                        for kc in range(8):
                            MM(ps[bk][:, 0:2], wb[:, kc, j * 128:(j + 1) * 128], condT[:, kc, :], kc == 0, False,
                               [wt, 'condT'], [pt])
                        MM(ps[bk][:, 0:2], brow[0:1, cb * 512 + j * 128: cb * 512 + (j + 1) * 128], ones_f[0:1, 0:2],
                           False, True, ['brow', 'ones_f'], [pt])
                        V(lambda e: e.tensor_copy(out=modT[:, cb * 4 + j, :], in_=ps[bk][:, 0:2]), [], [pt, 'modT'])
                else:
                    which = (cb - 4) // 2
                    nh = (cb - 4) % 2
                    for b in range(NSEQ):
                        bk = nb()
                        pt = f"ps{bk}"
                        for kc in range(8):
                            MM(ps[bk][:, :], condrep[:, kc, b, :], wb[:, kc, :], kc == 0, False, [wt, 'condrep'], [pt])
                        MM(ps[bk][:, :], ones_f[0:1, 0:128], brow[0:1, cb * 512:(cb + 1) * 512], False, True,
                           ['brow', 'ones_f'], [pt])
                        if which == 0:
                            V(lambda e: e.tensor_copy(out=gt1b[:, b, nh * 512:(nh + 1) * 512], in_=ps[bk][:, :]),
                              [], [pt, 'gt1b'])
                        else:
                            st = stg[b]
                            stt = f"stg{b}"
                            if which == 2:
                                V(lambda e: e.scalar_tensor_tensor(out=st[:, nh * 512:(nh + 1) * 512], in0=ps[bk][:, :],
                                                                   scalar=1.0, in1=g2b[:, nh * 512:(nh + 1) * 512],
                                                                   op0=ALU.add, op1=ALU.mult), ['g2b'], [pt, stt])
                            else:
                                V(lambda e: e.tensor_copy(out=st[:, nh * 512:(nh + 1) * 512], in_=ps[bk][:, :]),
                                  [], [pt, stt])
                            if nh == 1:
                                DMA('sp', bc_d[which - 1, b], st[:], [stt], ['bc_d'])
            V(lambda e: e.scalar_tensor_tensor(out=A1[:], in0=modT[:, 8:16, :], scalar=1.0,
                                               in1=g1T[:].unsqueeze(2).to_broadcast([128, 8, 2]),
                                               op0=ALU.add, op1=ALU.mult), ['modT', 'g1T'], ['A1'])
            fw.barrier()

    def stageA(l, b):
        with ExitStack() as es:
            xt = [sb(es, f"xa{i}", [128, D], F32) for i in range(2)]
            xn = [sb(es, f"xn{i}", [128, D], F32) for i in range(2)]
            junk = sb(es, "junkA", [128, D], BF16)
            st = [sb(es, f"stA{i}", [128, 4], F32) for i in range(2)]
            hT = [sb(es, f"hTa{i}", [128, 8, 128], BF16) for i in range(2)]
            for t in range(16):
                i = t % 2
                r0 = b * S + t * 128
                DMA('sp', xt[i][:], xsrc[0][r0:r0 + 128, :], ['xres'], [f'xa{i}'])
                A(lambda e: e.activation(out=junk[:], in_=xt[i][:], func=AF.Square, accum_out=st[i][:, 0:1]),
                  [f'xa{i}'], ['junkA', f'stA{i}'])
                A(lambda e: e.activation(out=st[i][:, 1:2], in_=st[i][:, 0:1], func=AF.Ln, scale=1.0 / D, bias=col[:, 1:2]),
                  [f'stA{i}', 'c_col'], [f'stA{i}'])
                A(lambda e: e.activation(out=st[i][:, 2:3], in_=st[i][:, 1:2], func=AF.Exp, scale=-0.5),
                  [f'stA{i}'], [f'stA{i}'])
                V(lambda e: e.tensor_scalar(out=xn[i][:], in0=xt[i][:], scalar1=st[i][:, 2:3], scalar2=None, op0=ALU.mult),
                  [f'xa{i}', f'stA{i}'], [f'xn{i}'])
                for half in range(2):
                    bk = nb()
                    pt = f"ps{bk}"
                    for q4 in range(4):
                        kc = half * 4 + q4
                        PE(lambda e: e.transpose(ps[bk][:, q4 * 128:(q4 + 1) * 128], xn[i][:, kc * 128:(kc + 1) * 128], ident_f[:]),
                           [f'xn{i}', 'c_ident_f'], [pt])
                    for q4 in range(4):
                        kc = half * 4 + q4
                        V(lambda e: e.tensor_scalar(out=hT[i][:, kc, :], in0=ps[bk][:, q4 * 128:(q4 + 1) * 128],
                                                    scalar1=A1[:, kc, b:b + 1], scalar2=modT[:, kc, b:b + 1],
                                                    op0=ALU.mult, op1=ALU.add), ['A1', 'modT'], [pt, f'hTa{i}'])
                DMA('sp', hT_d[b][:, :, t * 128:(t + 1) * 128], hT[i][:], [f'hTa{i}'], ['hT_d'])
            fw.barrier()

    def headnorm(P, nrows, rs, gcol, out_ap, out_tok, tmp, Psw=None, gsw=None, CC=None, SS=None, P_tok=(), extra_r=()):
        qs, sq, sd, qn, t2 = tmp['qs'], tmp['sq'], tmp['sd'], tmp['qn'], tmp['t2']
        n = nrows
        if rs is not None:
            V(lambda e: e.tensor_tensor(out=qs[0:n, :], in0=P, in1=rs, op=ALU.mult), list(extra_r), list(P_tok) + ['hn_qs'])
            src = qs[0:n, :]
            src_r, src_w = ['hn_qs'], []
        else:
            src = P
            src_r, src_w = [], list(P_tok)
        A(lambda e: e.activation(out=sq[0:n, :], in_=src, func=AF.Square), src_r, src_w + ['hn_sq'])
        bk = nb(6, 8)
        pt = f"ps{bk}"
        blk = blk96 if n == 96 else ones_b
        MM(ps[bk][0:n, :], blk[0:n, 0:n], sq[0:n, :], True, True, ['hn_sq', 'c_blk96_b', 'c_ones_b'], [pt])
        sc = col[0:n, 0:1] if n == 96 else col[0:n, 4:5]
        A(lambda e: e.activation(out=sd[0:n, :], in_=ps[bk][0:n, :], func=AF.Ln, scale=sc, bias=col[0:n, 1:2]),
          ['c_col'], [pt, 'hn_sd'])
        A(lambda e: e.activation(out=sd[0:n, :], in_=sd[0:n, :], func=AF.Exp, scale=-0.5), ['hn_sd'], ['hn_sd'])
        if Psw is None:
            V(lambda e: e.scalar_tensor_tensor(out=out_ap, in0=src, scalar=gcol, in1=sd[0:n, :], op0=ALU.mult, op1=ALU.mult),
              src_r + ['hn_sd', 'pcol'], src_w + [out_tok])
            return
        V(lambda e: e.scalar_tensor_tensor(out=qn[0:n, :], in0=src, scalar=gcol, in1=sd[0:n, :], op0=ALU.mult, op1=ALU.mult),
          src_r + ['hn_sd', 'pcol'], src_w + ['hn_qn'])
        Psw_ap, Psw_tok = Psw
        if rs is not None:
            V(lambda e: e.scalar_tensor_tensor(out=t2[0:n, :], in0=Psw_ap, scalar=gsw, in1=rs, op0=ALU.mult, op1=ALU.mult),
              ['pcol'] + list(extra_r), list(Psw_tok) + ['hn_t2'])
        else:
            V(lambda e: e.tensor_scalar(out=t2[0:n, :], in0=Psw_ap, scalar1=gsw, scalar2=None, op0=ALU.mult),
              ['pcol'], list(Psw_tok) + ['hn_t2'])
        G(lambda e: e.tensor_tensor(out=t2[0:n, :], in0=t2[0:n, :], in1=sd[0:n, :], op=ALU.mult), ['hn_sd', 'hn_t2'], ['hn_t2'])
        G(lambda e: e.tensor_tensor(out=t2[0:n, :], in0=t2[0:n, :], in1=SS, op=ALU.mult), ['hn_t2', 'ropeT'], ['hn_t2'])
        G(lambda e: e.tensor_tensor(out=qn[0:n, :], in0=qn[0:n, :], in1=CC, op=ALU.mult), ['hn_qn', 'ropeT'], ['hn_qn'])
        V(lambda e: e.tensor_tensor(out=out_ap, in0=qn[0:n, :], in1=t2[0:n, :], op=ALU.add), ['hn_qn', 'hn_t2'], [out_tok])

    def mk_hn_tmp(es):
        return {'qs': sb(es, "hn_qs", [128, 512], F32), 'sq': sb(es, "hn_sq", [128, 512], BF16),
                'sd': sb(es, "hn_sd", [128, 512], F32), 'qn': sb(es, "hn_qn", [128, 512], F32),
                't2': sb(es, "hn_t2", [128, 512], F32)}

    def load_w_cast(dst_ap, src_ap, tok):
        fw.dma('pool', lambda e: e.dma_start(out=dst_ap, in_=src_ap), [], [tok])

    def proj_fm(bk, lhs_list, rhs_list, n, reads, rows=None):
        pt = f"ps{bk}"
        o = ps[bk][0:n, :] if rows is None else ps[bk][rows[0]:rows[1], :]
        K = len(lhs_list)
        for k in range(K):
            MM(o, lhs_list[k], rhs_list[k], k == 0, k == K - 1, reads, [pt])

    def stage_mla(l, b, oT_mla):
        TWO_PI = 2.0 * math.pi
        C1 = 6.28125
        C2 = TWO_PI - C1
        with ExitStack() as es:
            tmp = mk_hn_tmp(es)
            CC = sb(es, "CC", [128, S], F32)
            SS = sb(es, "SS", [128, S], F32)
            ropei = sb(es, "ropei", [128, 512], I32)
            for ch in range(4):
                cs_ = slice(ch * 512, (ch + 1) * 512)
                DMA('sp', ropei[0:96, :], dr['pos'][b, ch * 512:(ch + 1) * 512].partition_broadcast(96), ['hn_qs'], ['ropei'])
                V(lambda e: e.tensor_copy(out=tmp['qs'][0:96, :], in_=ropei[0:96, :]), ['ropei'], ['hn_qs'])
                for tab, ci, phase in ((CC, 3, math.pi / 2), (SS, 2, 0.0)):
                    ang, kf, r_ = tmp['sd'], tmp['qn'], tmp['t2']
                    V(lambda e: e.tensor_scalar(out=ang[0:96, :], in0=tmp['qs'][0:96, :], scalar1=col[0:96, ci:ci + 1],
                                                scalar2=phase, op0=ALU.mult, op1=ALU.add), ['hn_qs', 'c_col'], ['hn_sd'])
                    V(lambda e: e.tensor_scalar(out=ropei[0:96, :], in0=ang[0:96, :], scalar1=1.0 / TWO_PI, scalar2=None,
                                                op0=ALU.mult), ['hn_sd'], ['ropei'])
                    V(lambda e: e.tensor_copy(out=kf[0:96, :], in_=ropei[0:96, :]), ['ropei'], ['hn_qn'])
                    V(lambda e: e.scalar_tensor_tensor(out=r_[0:96, :], in0=kf[0:96, :], scalar=-C1, in1=ang[0:96, :],
                                                       op0=ALU.mult, op1=ALU.add), ['hn_qn', 'hn_sd'], ['hn_t2'])
                    V(lambda e: e.scalar_tensor_tensor(out=r_[0:96, :], in0=kf[0:96, :], scalar=-C2, in1=r_[0:96, :],
                                                       op0=ALU.mult, op1=ALU.add), ['hn_qn', 'hn_t2'], ['hn_t2'])
                    V(lambda e: e.tensor_scalar(out=kf[0:96, :], in0=r_[0:96, :], scalar1=math.pi, scalar2=-TWO_PI,
                                                op0=ALU.is_gt, op1=ALU.mult), ['hn_t2'], ['hn_qn'])
                    V(lambda e: e.tensor_tensor(out=r_[0:96, :], in0=r_[0:96, :], in1=kf[0:96, :], op=ALU.add),
                      ['hn_qn', 'hn_t2'], ['hn_t2'])
                    V(lambda e: e.tensor_scalar(out=kf[0:96, :], in0=r_[0:96, :], scalar1=-math.pi, scalar2=TWO_PI,
                                                op0=ALU.is_lt, op1=ALU.mult), ['hn_t2'], ['hn_qn'])
                    V(lambda e: e.tensor_tensor(out=r_[0:96, :], in0=r_[0:96, :], in1=kf[0:96, :], op=ALU.add),
                      ['hn_qn', 'hn_t2'], ['hn_t2'])
                    A(lambda e: e.activation(out=tab[0:96, cs_], in_=r_[0:96, :], func=AF.Sin), ['hn_t2'], ['ropeT'])
            wA = sb(es, "wA", [128, 8, 768], BF16)
            wkr = sb(es, "wkr", [128, 8, 96], BF16)
            wkrs = sb(es, "wkrs", [128, 8, 96], BF16)
            win_v = dr['w_in'][l].rearrange("(kc p) n -> p kc n", p=128)
            load_w_cast(wA[:], win_v[:, :, 0:768], 'wA')
            G(lambda e: e.memset(wkr[:, :, 0:64], 0.0), [], ['wkr'])
            G(lambda e: e.memset(wkrs[:, :, 0:64], 0.0), [], ['wkrs'])
            load_w_cast(wkr[:, :, 64:96], win_v[:, :, 768:800], 'wkr')
            load_w_cast(wkrs[:, :, 64:80], win_v[:, :, 784:800], 'wkrs')
            load_w_cast(wkrs[:, :, 80:96], win_v[:, :, 768:784], 'wkrs')
            QT = sb(es, "QT", [128, 4, S], BF16)
            KT = sb(es, "KT", [128, 4, S], BF16)
            KR = sb(es, "KR", [128, S], BF16)
            Vm = sb(es, "Vm", [128, 16, 256], BF16)
            wuq = sb(es, "wuq", [128, 4, 384], BF16)
            wuqs = sb(es, "wuqs", [128, 4, 384], BF16)
            wukv = sb(es, "wukv", [128, 2, 512], BF16)
            hc = [sb(es, f"hc{i}", [128, 8, 512], BF16) for i in range(2)]
            cqg = sb(es, "cqg", [128, 4, 512], BF16)
            ckvg = sb(es, "ckvg", [128, 2, 512], BF16)
            sqc = sb(es, "sqc", [128, 4, 512], BF16)
            sqk = sb(es, "sqk", [128, 2, 512], BF16)
            rs_cq = sb(es, "rs_cq", [128, 512], F32)
            rs_ckv = sb(es, "rs_ckv", [128, 512], F32)
            rs_col = sb(es, "rs_col", [128, 4], F32)
            pT = [sb(es, f"pT{i}", [128, 512], BF16) for i in range(2)]
            rden = sb(es, "rden", [128, 512], F32)
            uq_v = dr['w_uq'][l].rearrange("(kc p) n -> p kc n", p=128)
            ukv_v = dr['w_ukv'][l].rearrange("(kc p) n -> p kc n", p=128)
            for half in range(2):
                hh0 = half * 4
                load_w_cast(wuq[:], uq_v[:, :, hh0 * 96:(hh0 + 4) * 96], 'wuq')
                load_w_cast(wukv[:], ukv_v[:, :, hh0 * 128:(hh0 + 4) * 128], 'wukv')
                wq4 = wuq[:].rearrange("p k (h c) -> p k h c", c=96)
                ws4 = wuqs[:].rearrange("p k (h c) -> p k h c", c=96)
                G(lambda e: e.memset(wuqs[:], 0.0), [], ['wuqs'])
                for kc in range(4):
                    G(lambda e: e.tensor_copy(out=ws4[:, kc, :, 64:80], in_=wq4[:, kc, :, 80:96]), ['wuq'], ['wuqs'])
                    G(lambda e: e.tensor_copy(out=ws4[:, kc, :, 80:96], in_=wq4[:, kc, :, 64:80]), ['wuq'], ['wuqs'])
                for ch in range(4):
                    cs_ = slice(ch * 512, (ch + 1) * 512)
                    hcb = hc[ch % 2]
                    hct = f"hc{ch % 2}"
                    DMA('sp', hcb[:], hT_d[b][:, :, cs_], ['hT_d'], [hct])
                    for ft in range(6):
                        bk = nb(0, 6)
                        proj_fm(bk, [wA[:, kc, ft * 128:(ft + 1) * 128] for kc in range(8)], [hcb[:, kc, :] for kc in range(8)],
                                128, ['wA', hct])
                        pt = f"ps{bk}"
                        if ft < 4:
                            V(lambda e: e.tensor_scalar(out=cqg[:, ft, :], in0=ps[bk][:, :], scalar1=gcqT[:, ft:ft + 1],
                                                        scalar2=None, op0=ALU.mult), ['gcqT'], [pt, 'cqg'])
                            A(lambda e: e.activation(out=sqc[:, ft, :], in_=ps[bk][:, :], func=AF.Square), [], [pt, 'sqc'])
                        else:
                            f2 = ft - 4
                            V(lambda e: e.tensor_scalar(out=ckvg[:, f2, :], in0=ps[bk][:, :], scalar1=gckvT[:, f2:f2 + 1],
                                                        scalar2=None, op0=ALU.mult), ['gckvT'], [pt, 'ckvg'])
                            A(lambda e: e.activation(out=sqk[:, f2, :], in_=ps[bk][:, :], func=AF.Square), [], [pt, 'sqk'])
                    for (sq_, nf, rs_, rtok, sqtok, dim) in ((sqc, 4, rs_cq, 'rs_cq', 'sqc', 512), (sqk, 2, rs_ckv, 'rs_ckv', 'sqk', 256)):
                        bk = nb(6, 8)
                        pt = f"ps{bk}"
                        for ft in range(nf):
                            MM(ps[bk][:, :], ones_b[:, :], sq_[:, ft, :], ft == 0, ft == nf - 1, [sqtok, 'c_ones_b'], [pt])
                        A(lambda e: e.activation(out=rs_[:, :], in_=ps[bk][:, :], func=AF.Ln, scale=1.0 / dim, bias=col[:, 1:2]),
                          ['c_col'], [pt, rtok])
                        A(lambda e: e.activation(out=rs_[:, :], in_=rs_[:, :], func=AF.Exp, scale=-0.5), [rtok], [rtok])
                    bk = nb(6, 8)
                    pt = f"ps{bk}"
                    for tt in range(4):
                        for ft in range(2):
                            MM(ps[bk][:, tt:tt + 1], sqk[:, ft, tt * 128:(tt + 1) * 128], ones_b[:, 0:1], ft == 0, ft == 1,
                               ['sqk', 'c_ones_b'], [pt])
                    A(lambda e: e.activation(out=rs_col[:, :], in_=ps[bk][:, 0:4], func=AF.Ln, scale=1.0 / 256, bias=col[:, 1:2]),
                      ['c_col'], [pt, 'rs_col'])
                    A(lambda e: e.activation(out=rs_col[:, :], in_=rs_col[:, :], func=AF.Exp, scale=-0.5), ['rs_col'], ['rs_col'])
                    bkA, bkB = nb(0, 6), nb(0, 6)
                    proj_fm(bkA, [wkr[:, kc, :] for kc in range(8)], [hcb[:, kc, :] for kc in range(8)], 96, ['wkr', hct])
                    proj_fm(bkB, [wkrs[:, kc, :] for kc in range(8)], [hcb[:, kc, :] for kc in range(8)], 96, ['wkrs', hct])
                    headnorm(ps[bkA][0:96, :], 96, None, pcol[0:96, 2:3], KR[0:96, cs_], 'KR', tmp,
                             Psw=(ps[bkB][0:96, :], [f"ps{bkB}"]), gsw=pcol[0:96, 3:4], CC=CC[0:96, cs_], SS=SS[0:96, cs_],
                             P_tok=[f"ps{bkA}"])
                    for h in range(4):
                        bkA, bkB = nb(0, 6), nb(0, 6)
                        proj_fm(bkA, [wuq[:, kc, h * 96:(h + 1) * 96] for kc in range(4)], [cqg[:, kc, :] for kc in range(4)],
                                96, ['wuq', 'cqg'])
                        proj_fm(bkB, [wuqs[:, kc, h * 96:(h + 1) * 96] for kc in range(4)], [cqg[:, kc, :] for kc in range(4)],
                                96, ['wuqs', 'cqg'])
                        headnorm(ps[bkA][0:96, :], 96, rs_cq[0:96, :], pcol[0:96, 0:1], QT[0:96, h, cs_], 'QT', tmp,
                                 Psw=(ps[bkB][0:96, :], [f"ps{bkB}"]), gsw=pcol[0:96, 1:2], CC=CC[0:96, cs_], SS=SS[0:96, cs_],
                                 P_tok=[f"ps{bkA}"], extra_r=['rs_cq'])
                        bkA = nb(0, 6)
                        proj_fm(bkA, [wukv[:, kc, h * 128:h * 128 + 64] for kc in range(2)], [ckvg[:, kc, :] for kc in range(2)],
                                64, ['wukv', 'ckvg'])
                        headnorm(ps[bkA][0:64, :], 64, rs_ckv[0:64, :], pcol[0:64, 2:3], KT[0:64, h, cs_], 'KT', tmp,
                                 P_tok=[f"ps{bkA}"], extra_r=['rs_ckv'])
                        G(lambda e: e.tensor_copy(out=KT[64:96, h, cs_], in_=KR[64:96, cs_]), ['KR'], ['KT'])
                    wv4 = wukv[:].rearrange("p k (h c) -> p k h c", c=128)
                    for tt in range(4):
                        bk = nb(0, 6)
                        pt = f"ps{bk}"
                        for kc in range(2):
                            MM(ps[bk][:, 0:256].rearrange("p (h c) -> p h c", c=64), ckvg[:, kc, tt * 128:(tt + 1) * 128],
                               wv4[:, kc, :, 64:128], kc == 0, kc == 1, ['ckvg', 'wukv'], [pt])
                        V(lambda e: e.tensor_scalar(out=Vm[:, ch * 4 + tt, :], in0=ps[bk][:, 0:256], scalar1=rs_col[:, tt:tt + 1],
                                                    scalar2=None, op0=ALU.mult), ['rs_col'], [pt, 'Vm'])
                it = 0
                for h in range(4):
                    hg = hh0 + h
                    prow = (hg % 2) * 64
                    for qc in range(4):
                        bo, bd = 2 + it % 2, 4 + it % 2
                        it += 1
                        for kt in range(16):
                            bs = kt % 2
                            MM(ps[bs][:, :], KT[0:96, h, kt * 128:(kt + 1) * 128], QT[0:96, h, qc * 512:(qc + 1) * 512], True, True,
                               ['KT', 'QT'], [f"ps{bs}"])
                            A(lambda e: e.activation(out=pT[bs][:, :], in_=ps[bs][:, :], func=AF.Exp, scale=MLA_SCALE),
                              [], [f"ps{bs}", f"pT{bs}"])
                            MM(ps[bo][prow:prow + 64, :], Vm[:, kt, h * 64:(h + 1) * 64], pT[bs][:, :], kt == 0, kt == 15,
                               ['Vm', f"pT{bs}"], [f"ps{bo}"])
                            MM(ps[bd][prow:prow + 64, :], ones_b[:, 0:64], pT[bs][:, :], kt == 0, kt == 15,
                               ['c_ones_b', f"pT{bs}"], [f"ps{bd}"])
                        A(lambda e: e.activation(out=rden[prow:prow + 64, :], in_=ps[bd][prow:prow + 64, :], func=AF.Ln),
                          [], [f"ps{bd}", 'rden'])
                        A(lambda e: e.activation(out=rden[prow:prow + 64, :], in_=rden[prow:prow + 64, :], func=AF.Exp, scale=-1.0),
                          ['rden'], ['rden'])
                        V(lambda e: e.tensor_tensor(out=oT_mla[prow:prow + 64, hg // 2, qc * 512:(qc + 1) * 512],
                                                    in0=ps[bo][prow:prow + 64, :], in1=rden[prow:prow + 64, :], op=ALU.mult),
                          ['rden'], [f"ps{bo}", 'oT_mla'])
            fw.barrier()

    def band_group(l, b, es, tmp, wv_cols, n_k, gq_col, gk_col, R, mask_base, slopes, epilogue, posq, hcbufs, tag):
        with ExitStack() as gs:
            wG = sb(gs, "wG", [128, 8, 256 + 2 * 64 * n_k], BF16)
            QT = sb(gs, "QTb", [64, 4, S], BF16)
            KT = sb(gs, "KTb", [64, n_k, S], BF16)
            Vt = sb(gs, "Vtb", [128, 16, n_k, 65], BF16)
            tS = [sb(gs, f"tS{i}", [128, 4, 128], F32) for i in range(2)]
            pB = [sb(gs, f"pB{i}", [128, 4, 128], BF16) for i in range(2)]
            Dt = [sb(gs, f"Dt{i}", [128, 128], F32) for i in range(2)]
            win_v = dr['w_in'][l].rearrange("(kc p) n -> p kc n", p=128)
            q0, k0, v0 = wv_cols
            nkc = 64 * n_k
            load_w_cast(wG[:, :, 0:256], win_v[:, :, q0:q0 + 256], 'wG')
            load_w_cast(wG[:, :, 256:256 + nkc], win_v[:, :, k0:k0 + nkc], 'wG')
            load_w_cast(wG[:, :, 256 + nkc:256 + 2 * nkc], win_v[:, :, v0:v0 + nkc], 'wG')
            G(lambda e: e.memset(Vt[:, :, :, 64:65], 1.0), [], ['Vtb'])
            for ch in range(4):
                cs_ = slice(ch * 512, (ch + 1) * 512)
                hcb = hcbufs[ch % 2]
                hct = f"hc{ch % 2}"
                DMA('sp', hcb[:], hT_d[b][:, :, cs_], ['hT_d'], [hct])
                for hh in range(4):
                    bk = nb(0, 4)
                    proj_fm(bk, [wG[:, kc, hh * 64:(hh + 1) * 64] for kc in range(8)], [hcb[:, kc, :] for kc in range(8)],
                            64, ['wG', hct])
                    headnorm(ps[bk][0:64, :], 64, None, gq_col, QT[0:64, hh, cs_], 'QTb', tmp, P_tok=[f"ps{bk}"])
                for hk in range(n_k):
                    bk = nb(0, 4)
                    proj_fm(bk, [wG[:, kc, 256 + hk * 64:256 + (hk + 1) * 64] for kc in range(8)],
                            [hcb[:, kc, :] for kc in range(8)], 64, ['wG', hct])
                    headnorm(ps[bk][0:64, :], 64, None, gk_col, KT[0:64, hk, cs_], 'KTb', tmp, P_tok=[f"ps{bk}"])
                for tt in range(4):
                    bk = nb(0, 4)
                    pt = f"ps{bk}"
                    for kc in range(8):
                        MM(ps[bk][:, 0:nkc], hcb[:, kc, tt * 128:(tt + 1) * 128], wG[:, kc, 256 + nkc:256 + 2 * nkc],
                           kc == 0, kc == 7, ['wG', hct], [pt])
                    V(lambda e: e.tensor_copy(out=Vt[:, ch * 4 + tt, :, 0:64],
                                              in_=ps[bk][:, 0:nkc].rearrange("p (h c) -> p h c", c=64)), [], [pt, 'Vtb'])
            it = 0
            for blk in range(16):
                kts = list(range(max(0, blk - R), min(15, blk + R) + 1))
                for j, kt in enumerate(kts):
                    dl = kt - blk
                    i2 = it % 2
                    it += 1
                    d_ = Dt[i2]
                    dtok = f"Dt{i2}"
                    V(lambda e: e.scalar_tensor_tensor(out=d_[:, :], in0=posq[:, blk * 128:(blk + 1) * 128],
                                                       scalar=poskT[:, b, kt:kt + 1], in1=masks[:, mask_base + dl + R, :],
                                                       op0=ALU.subtract, op1=ALU.add), ['posq', 'poskT', 'c_masks'], [dtok])
                    V(lambda e: e.scalar_tensor_tensor(out=d_[:, :], in0=d_[:, :], scalar=-1.0, in1=d_[:, :],
                                                       op0=ALU.mult, op1=ALU.max), [dtok], [dtok])
                    bs = i2
                    for hh in range(4):
                        hk = hh if n_k == 4 else 0
                        MM(ps[bs][:, hh * 128:(hh + 1) * 128], KT[0:64, hk, kt * 128:(kt + 1) * 128],
                           QT[0:64, hh, blk * 128:(blk + 1) * 128], True, True, ['KTb', 'QTb'], [f"ps{bs}"])
                    for hh in range(4):
                        V(lambda e: e.scalar_tensor_tensor(out=tS[i2][:, hh, :], in0=d_[:, :], scalar=-slopes[hh] / HD_SCALE,
                                                           in1=ps[bs][:, hh * 128:(hh + 1) * 128], op0=ALU.mult, op1=ALU.add),
                          [dtok], [f"ps{bs}", f"tS{i2}"])
                    A(lambda e: e.activation(out=pB[i2][:], in_=tS[i2][:], func=AF.Exp, scale=HD_SCALE),
                      [f"tS{i2}"], [f"pB{i2}"])
                    for hh in range(4):
                        hk = hh if n_k == 4 else 0
                        MM(ps[4 + hh][:, 0:65], pB[i2][:, hh, :], Vt[:, kt, hk, :], j == 0, j == len(kts) - 1,
                           [f"pB{i2}", 'Vtb'], [f"ps{4 + hh}"])
                epilogue(blk)
            fw.barrier()

    def stage_band(l, b, oT_dil, oT_win):
        sl12 = alibi(12)
        sl8 = alibi(8)
        with ExitStack() as es:
            tmp = mk_hn_tmp(es)
            posq = sb(es, "posq", [128, S], F32)
            posq_i = sb(es, "posq_i", [128, S], I32)
            hcbufs = [sb(es, f"hc{i}", [128, 8, 512], BF16) for i in range(2)]
            acc = sb(es, "acc", [128, 16, 4, 65], F32)
            osb = sb(es, "osb", [128, 4, 64], BF16)
            rdn = sb(es, "rdn", [128, 4], F32)
            DMA('sp', posq_i[:], dr['pos'][b].partition_broadcast(128), [], ['posq_i'])
            V(lambda e: e.tensor_copy(out=posq[:], in_=posq_i[:]), ['posq_i'], ['posq'])

            def transpose_out(blk, oT, ft0):
                for i in range(2):
                    bk = nb(0, 4)
                    pt = f"ps{bk}"
                    pbf = ps[bk][:].bitcast(BF16)
                    PE(lambda e: e.transpose(pbf[:, 0:128], osb[:, 2 * i:2 * i + 2, :].rearrange("p h c -> p (h c)"), ident_b[:]),
                       ['osb', 'c_ident_b'], [pt])
                    V(lambda e: e.tensor_copy(out=oT[:, ft0 + i, blk * 128:(blk + 1) * 128], in_=pbf[:, 0:128]),
                      [], [pt, 'oT_band'])

            for g in range(3):
                def epi_dil(blk, g=g):
                    for hh in range(4):
                        if g == 0:
                            V(lambda e: e.tensor_copy(out=acc[:, blk, hh, :], in_=ps[4 + hh][:, 0:65]), [], [f"ps{4 + hh}", 'acc'])
                        else:
                            V(lambda e: e.tensor_tensor(out=acc[:, blk, hh, :], in0=acc[:, blk, hh, :], in1=ps[4 + hh][:, 0:65],
                                                        op=ALU.add), [], [f"ps{4 + hh}", 'acc'])
                    if g == 2:
                        V(lambda e: e.reciprocal(out=rdn[:, :], in_=acc[:, blk, :, 64]), ['acc'], ['rdn'])
                        V(lambda e: e.tensor_tensor(out=osb[:], in0=acc[:, blk, :, 0:64],
                                                    in1=rdn[:, :].unsqueeze(2).to_broadcast([128, 4, 64]), op=ALU.mult),
                          ['acc', 'rdn'], ['osb'])
                        transpose_out(blk, oT_dil, 0)
                q0 = OFF_DIL + g * 256
                band_group(l, b, es, tmp, (q0, q0 + 768, q0 + 1536), 4, pcol[0:64, 4:5], pcol[0:64, 5:6], DIL_R[g],
                           MASK_BASE[g], sl12[g * 4:(g + 1) * 4], epi_dil, posq, hcbufs, f"d{g}")
            for kvg in range(2):
                def epi_win(blk, kvg=kvg):
                    for hh in range(4):
                        V(lambda e: e.tensor_tensor(out=rdn[:, hh:hh + 1], in0=ps[4 + hh][:, 64:65],
                                                    in1=esink[:, kvg * 4 + hh:kvg * 4 + hh + 1], op=ALU.add),
                          ['esink'], [f"ps{4 + hh}", 'rdn'])
                    V(lambda e: e.reciprocal(out=rdn[:, :], in_=rdn[:, :]), ['rdn'], ['rdn'])
                    for hh in range(4):
                        V(lambda e: e.tensor_scalar(out=osb[:, hh, :], in0=ps[4 + hh][:, 0:64], scalar1=rdn[:, hh:hh + 1],
                                                    scalar2=None, op0=ALU.mult), ['rdn'], [f"ps{4 + hh}", 'osb'])
                    transpose_out(blk, oT_win, kvg * 2)
                band_group(l, b, es, tmp, (OFF_WIN + kvg * 256, OFF_WIN + 512 + kvg * 64, OFF_WIN + 640 + kvg * 64), 1,
                           pcol[0:64, 6:7], pcol[0:64, 7:8], 1, MASK_BASE[3], sl8[kvg * 4:(kvg + 1) * 4], epi_win,
                           posq, hcbufs, f"w{kvg}")
            fw.barrier()

    def stage_merge(l, b, oT_mla, oT_dil, oT_win):
        with ExitStack() as es:
            wg = [sb(es, f"wg{i}", [128, 8, D], BF16) for i in range(2)]
            wbr_m = sb(es, "wbr_m", [128, 4, D], BF16)
            wbr_d = sb(es, "wbr_d", [128, 2, D], BF16)
            wbr_w = sb(es, "wbr_w", [128, 4, D], BF16)
            wout = sb(es, "wout", [128, 8, D], BF16)
            hcb = sb(es, "hcm", [128, 8, 512], BF16)
            mgT = sb(es, "mgT", [128, 8, 512], BF16)
            mg = sb(es, "mg", [128, 8, 512], F32)
            sig = [sb(es, f"sig{i}", [128, 512], F32) for i in range(2)]
            tmpm = sb(es, "tmpm", [128, 512], F32)
            xt = [sb(es, f"xm{i}", [128, D], F32) for i in range(2)]
            win_v = dr['w_in'][l].rearrange("(kc p) n -> p kc n", p=128)
            load_w_cast(wbr_m[:], dr['w_br_mla'][l].rearrange("(kc p) n -> p kc n", p=128), 'wbr_m')
            load_w_cast(wbr_d[:], dr['w_br_dil'][l].rearrange("(kc p) n -> p kc n", p=128), 'wbr_d')
            load_w_cast(wbr_w[:], dr['w_br_win'][l].rearrange("(kc p) n -> p kc n", p=128), 'wbr_w')
            load_w_cast(wout[:], dr['w_out'][l].rearrange("(kc p) n -> p kc n", p=128), 'wout')
            srcs = ((oT_mla, 4, wbr_m, 'wbr_m', 'oT_mla'), (oT_dil, 2, wbr_d, 'wbr_d', 'oT_band'),
                    (oT_win, 4, wbr_w, 'wbr_w', 'oT_band'))
            it = 0
            for ch in range(4):
                cs_ = slice(ch * 512, (ch + 1) * 512)
                DMA('sp', hcb[:], hT_d[b][:, :, cs_], ['hT_d'], ['hcm'])
                for m in range(3):
                    wgb = wg[(ch * 3 + m) % 2]
                    wgt = f"wg{(ch * 3 + m) % 2}"
                    load_w_cast(wgb[:], win_v[:, :, OFF_GATE + m * D:OFF_GATE + (m + 1) * D], wgt)
                    oT, nk, wbr, wbrt, otok = srcs[m]
                    for ft in range(8):
                        i2 = it % 2
                        it += 1
                        bg, by = i2, 2 + i2
                        for kc in range(8):
                            MM(ps[bg][:, :], wgb[:, kc, ft * 128:(ft + 1) * 128], hcb[:, kc, :], kc == 0, kc == 7,
                               [wgt, 'hcm'], [f"ps{bg}"])
                        A(lambda e: e.activation(out=sig[i2][:], in_=ps[bg][:, :], func=AF.Sigmoid), [], [f"ps{bg}", f"sig{i2}"])
                        for k in range(nk):
                            MM(ps[by][:, :], wbr[:, k, ft * 128:(ft + 1) * 128], oT[:, k, cs_], k == 0, k == nk - 1,
                               [wbrt, otok], [f"ps{by}"])
                        if m == 0:
                            V(lambda e: e.tensor_tensor(out=mg[:, ft, :], in0=ps[by][:, :], in1=sig[i2][:], op=ALU.mult),
                              [f"sig{i2}"], [f"ps{by}", 'mg'])
                        else:
                            V(lambda e: e.tensor_tensor(out=tmpm[:], in0=ps[by][:, :], in1=sig[i2][:], op=ALU.mult),
                              [f"sig{i2}"], [f"ps{by}", 'tmpm'])
                            if m == 1:
                                G(lambda e: e.tensor_tensor(out=mg[:, ft, :], in0=mg[:, ft, :], in1=tmpm[:], op=ALU.add),
                                  ['tmpm', 'mg'], ['mg'])
                            else:
                                G(lambda e: e.tensor_tensor(out=mgT[:, ft, :], in0=mg[:, ft, :], in1=tmpm[:], op=ALU.add),
                                  ['tmpm', 'mg'], ['mgT'])
                for tt in range(4):
                    i2 = tt % 2
                    r0 = b * S + ch * 512 + tt * 128
                    DMA('sp', xt[i2][:], xsrc[0][r0:r0 + 128, :], ['xres'], [f"xm{i2}"])
                    for nh in range(2):
                        bk = 4 + (tt * 2 + nh) % 4
                        for ft in range(8):
                            MM(ps[bk][:, :], mgT[:, ft, tt * 128:(tt + 1) * 128], wout[:, ft, nh * 512:(nh + 1) * 512],
                               ft == 0, ft == 7, ['mgT', 'wout'], [f"ps{bk}"])
                        V(lambda e: e.tensor_tensor(out=tmpm[:], in0=ps[bk][:, :], in1=gt1b[:, b, nh * 512:(nh + 1) * 512],
                                                    op=ALU.mult), ['gt1b'], [f"ps{bk}", 'tmpm'])
                        G(lambda e: e.tensor_tensor(out=xt[i2][:, nh * 512:(nh + 1) * 512], in0=xt[i2][:, nh * 512:(nh + 1) * 512],
                                                    in1=tmpm[:], op=ALU.add), ['tmpm', f"xm{i2}"], [f"xm{i2}"])
                    DMA('sp', out[r0:r0 + 128, :], xt[i2][:], [f"xm{i2}"], ['xout'])
            fw.barrier()

    def stage_moe(l):
        w1v = dr['w1'].rearrange("l e (p k) n -> (l e p k) n", k=8)
        w3v = dr['w3'].rearrange("l e (p k) n -> (l e p k) n", k=8)
        w2v = dr['w2'].rearrange("l e (p c) n -> (l e p c) n", c=3)
        with ExitStack() as es:
            gates = sb(es, "gates", [128, NT, 2], F32)
            d1i = sb(es, "d1i", [128, NT], I32)
            d2i = sb(es, "d2i", [128, NT], I32)
            widx = sb(es, "widx", [128, NBLK], I32)
            widx4 = sb(es, "widx8", [128, 8, NBLK], I32)
            widx3 = sb(es, "widx3", [128, 3, NBLK], I32)
            gt2b = sb(es, "gt2b", [128, NSEQ, D], F32)
            DMA('sp', gt2b[:], bc_d[2].rearrange("b p n -> p b n"), ['bc_d'], ['gt2b'])
            with ExitStack() as e1:
                h2bf = sb(e1, "h2bf", [128, NT, D], BF16)
                A2b = sb(e1, "A2b", [128, NSEQ, D], F32)
                B2b = sb(e1, "B2b", [128, NSEQ, D], F32)
                DMA('sp', A2b[:], bc_d[1].rearrange("b p n -> p b n"), ['bc_d'], ['A2b'])
                DMA('sp', B2b[:], bc_d[0].rearrange("b p n -> p b n"), ['bc_d'], ['B2b'])
                wr = sb(e1, "wr", [128, 8, 36], F32)
                brow = sb(e1, "browr", [1, 36], F32)
                DMA('sp', wr[:], dr['w_r'][l].rearrange("(kc p) n -> p kc n", p=128), [], ['wr'])
                DMA('sp', brow[:], dr['b_r'][l], [], ['browr'])
                M1a = sb(e1, "M1a", [128, NT, 32], F32)
                M2a = sb(e1, "M2a", [128, NT, 32], F32)
                xt = [sb(e1, f"xe{i}", [128, D], F32) for i in range(2)]
                h2 = [sb(e1, f"h2{i}", [128, D], F32) for i in range(2)]
                h2T = [sb(e1, f"h2T{i}", [128, 8, 128], F32) for i in range(2)]
                junk = sb(e1, "junkE", [128, D], BF16)
                st = [sb(e1, f"stE{i}", [128, 4], F32) for i in range(2)]
                rt = sb(e1, "rt", [128, 64], F32)
                r2 = sb(e1, "r2", [128, 64], F32)
                for t in range(NT):
                    i = t % 2
                    b = t // 16
                    r0 = t * 128
                    DMA('sp', xt[i][:], out[r0:r0 + 128, :], ['xout'], [f'xe{i}'])
                    A(lambda e: e.activation(out=junk[:], in_=xt[i][:], func=AF.Square, accum_out=st[i][:, 0:1]),
                      [f'xe{i}'], ['junkE', f'stE{i}'])
                    A(lambda e: e.activation(out=st[i][:, 1:2], in_=st[i][:, 0:1], func=AF.Ln, scale=1.0 / D, bias=col[:, 1:2]),
                      [f'stE{i}', 'c_col'], [f'stE{i}'])
                    A(lambda e: e.activation(out=st[i][:, 2:3], in_=st[i][:, 1:2], func=AF.Exp, scale=-0.5),
                      [f'stE{i}'], [f'stE{i}'])
                    V(lambda e: e.scalar_tensor_tensor(out=h2[i][:], in0=xt[i][:], scalar=st[i][:, 2:3], in1=A2b[:, b, :],
                                                       op0=ALU.mult, op1=ALU.mult), [f'xe{i}', f'stE{i}', 'A2b'], [f'h2{i}'])
                    G(lambda e: e.tensor_tensor(out=h2[i][:], in0=h2[i][:], in1=B2b[:, b, :], op=ALU.add), [f'h2{i}', 'B2b'], [f'h2{i}'])
                    A(lambda e: e.copy(out=h2bf[:, t, :], in_=h2[i][:]), [f'h2{i}'], ['h2bf'])
                    for half in range(2):
                        bk = nb(0, 4)
                        pt = f"ps{bk}"
                        for q4 in range(4):
                            kc = half * 4 + q4
                            PE(lambda e: e.transpose(ps[bk][:, q4 * 128:(q4 + 1) * 128], h2[i][:, kc * 128:(kc + 1) * 128], ident_f[:]),
                               [f'h2{i}', 'c_ident_f'], [pt])
                        V(lambda e: e.tensor_copy(out=h2T[i][:, half * 4:half * 4 + 4, :],
                                                  in_=ps[bk][:, :].rearrange("p (q c) -> p q c", c=128)), [], [pt, f'h2T{i}'])
                    bk = nb(4, 8)
                    pt = f"ps{bk}"
                    for kc in range(8):
                        MM(ps[bk][:, 0:36], h2T[i][:, kc, :], wr[:, kc, :], kc == 0, False, [f'h2T{i}', 'wr'], [pt])
                    MM(ps[bk][:, 0:36], ones_f[0:1, 0:128], brow[0:1, :], False, True, ['ones_f', 'browr'], [pt])
                    lg = rt[:, 0:36]
                    gmax, ngmax, gsum, gw = rt[:, 36:37], rt[:, 37:38], rt[:, 38:39], rt[:, 39:40]
                    ohg, ge = rt[:, 40:44], rt[:, 44:48]
                    m1, m2, dd, ed, rr = rt[:, 48:49], rt[:, 49:50], rt[:, 50:51], rt[:, 51:52], rt[:, 52:53]
                    sel, mask1, sel2, mask2, t48 = r2[:, 0:8], r2[:, 8:16], r2[:, 16:24], r2[:, 24:32], r2[:, 32:64]
                    t48v = t48.rearrange("p (g e) -> p g e", e=8)
                    RT = ['rt']
                    V(lambda e: e.tensor_copy(out=lg, in_=ps[bk][:, 0:36]), [], [pt, 'rt'])
                    V(lambda e: e.tensor_reduce(out=gmax, in_=rt[:, 0:4], axis=AX.X, op=ALU.max), RT, RT)
                    V(lambda e: e.tensor_scalar(out=ohg, in0=rt[:, 0:4], scalar1=gmax, scalar2=None, op0=ALU.is_equal), RT, RT)
                    V(lambda e: e.tensor_scalar(out=ngmax, in0=gmax, scalar1=-1.0, scalar2=None, op0=ALU.mult), RT, RT)
                    A(lambda e: e.activation(out=ge, in_=rt[:, 0:4], func=AF.Exp, bias=ngmax, scale=1.0, accum_out=gsum), RT, RT)
                    V(lambda e: e.reciprocal(out=gw, in_=gsum), RT, RT)
                    V(lambda e: e.tensor_tensor(out=t48v, in0=rt[:, 4:36].rearrange("p (g e) -> p g e", e=8),
                                                in1=ohg.unsqueeze(2).to_broadcast([128, 4, 8]), op=ALU.mult), RT, RT)
                    V(lambda e: e.tensor_reduce(out=sel, in_=t48v.rearrange("p g e -> p e g"), axis=AX.X, op=ALU.add), RT, RT)
                    V(lambda e: e.tensor_reduce(out=m1, in_=sel, axis=AX.X, op=ALU.max), RT, RT)
                    V(lambda e: e.tensor_scalar(out=mask1, in0=sel, scalar1=m1, scalar2=None, op0=ALU.is_equal), RT, RT)
                    V(lambda e: e.scalar_tensor_tensor(out=sel2, in0=mask1, scalar=-1.0e30, in1=sel, op0=ALU.mult, op1=ALU.add), RT, RT)
                    V(lambda e: e.tensor_reduce(out=m2, in_=sel2, axis=AX.X, op=ALU.max), RT, RT)
                    V(lambda e: e.tensor_scalar(out=mask2, in0=sel2, scalar1=m2, scalar2=None, op0=ALU.is_equal), RT, RT)
                    V(lambda e: e.tensor_tensor(out=dd, in0=m2, in1=m1, op=ALU.subtract), RT, RT)
                    A(lambda e: e.activation(out=ed, in_=dd, func=AF.Exp), RT, RT)
                    V(lambda e: e.tensor_scalar(out=rr, in0=ed, scalar1=1.0, scalar2=None, op0=ALU.add), RT, RT)
                    V(lambda e: e.reciprocal(out=rr, in_=rr), RT, RT)
                    V(lambda e: e.tensor_tensor(out=gates[:, t, 0:1], in0=gw, in1=rr, op=ALU.mult), RT, ['gates'])
                    V(lambda e: e.tensor_tensor(out=gates[:, t, 1:2], in0=gw, in1=gates[:, t, 0:1], op=ALU.subtract),
                      RT + ['gates'], ['gates'])
                    V(lambda e: e.tensor_tensor(out=M1a[:, t, :].rearrange("p (g e) -> p g e", e=8),
                                                in0=ohg.unsqueeze(2).to_broadcast([128, 4, 8]),
                                                in1=mask1.unsqueeze(1).to_broadcast([128, 4, 8]), op=ALU.mult), RT, ['M1a'])
                    V(lambda e: e.tensor_tensor(out=M2a[:, t, :].rearrange("p (g e) -> p g e", e=8),
                                                in0=ohg.unsqueeze(2).to_broadcast([128, 4, 8]),
                                                in1=mask2.unsqueeze(1).to_broadcast([128, 4, 8]), op=ALU.mult), RT, ['M2a'])
                if stop_after in (('e1tiles', l), ('moe_e1tiles', l)):
                    dsb = sb(e1, "dsb", [128, 512], F32)
                    V(lambda e: e.memset(dsb[:], 0.0), [], ['dsb'])
                    V(lambda e: e.tensor_copy(out=dsb[:, 160:224], in_=gates[:].rearrange("p t k -> p (t k)")), ['gates'], ['dsb'])
                    V(lambda e: e.tensor_copy(out=dsb[:, 288:320], in_=M1a[:, 0, :]), ['M1a'], ['dsb'])
                    V(lambda e: e.tensor_copy(out=dsb[:, 320:352], in_=M2a[:, 0, :]), ['M2a'], ['dsb'])
                    V(lambda e: e.tensor_copy(out=dsb[:, 352:405], in_=rt[:, 0:53]), ['rt'], ['dsb'])
                    DMA('sp', dbg, dsb[:], ['dsb'], ['dbg'])
                    fw.barrier()
                    return
                Mt_bf = sb(e1, "Mt_bf", [128, NT * 32], BF16)
                CSs = sb(e1, "CSs", [128, NT, 32], F32)
                pref = sb(e1, "pref", [128, NT, 32], F32)
                destf = sb(e1, "destf", [128, NT, 32], F32)
                tmpd = sb(e1, "tmpd", [128, NT, 32], F32)
                cmp2 = sb(e1, "cmp2", [128, NBLK, 32], F32)
                sc = sb(e1, "scn", [128, 8, 32], F32)
                dkf = sb(e1, "dkf", [128, 2, NT], F32)
                bef = sb(e1, "bef", [128, NBLK], F32)
                SL = ['slot']
                V(lambda e: e.tensor_tensor(out=Mt_bf[:], in0=M1a[:].rearrange("p t e -> p (t e)"),
                                            in1=M2a[:].rearrange("p t e -> p (t e)"), op=ALU.add), ['M1a', 'M2a'], ['Mt_bf'])
                for hf in range(2):
                    MM(ps[hf][:, :], ustrict[:, :], Mt_bf[:, hf * 512:(hf + 1) * 512], True, True, ['Mt_bf', 'c_ustrict_b'], [f"ps{hf}"])
                    MM(ps[2 + hf][:, :], ones_b[:, :], Mt_bf[:, hf * 512:(hf + 1) * 512], True, True, ['Mt_bf', 'c_ones_b'], [f"ps{2 + hf}"])
                    V(lambda e: e.tensor_copy(out=CSs[:, hf * 16:(hf + 1) * 16, :].rearrange("p t e -> p (t e)"), in_=ps[2 + hf][:, :]),
                      [], [f"ps{2 + hf}"] + SL)
                V(lambda e: e.memset(pref[:, 0, :], 0.0), SL, SL)
                for t in range(1, NT):
                    V(lambda e: e.tensor_tensor(out=pref[:, t, :], in0=pref[:, t - 1, :], in1=CSs[:, t - 1, :], op=ALU.add), SL, SL)
                cnts, nblk_, padd, incA, incB, startp = sc[:, 0, :], sc[:, 1, :], sc[:, 2, :], sc[:, 3, :], sc[:, 4, :], sc[:, 5, :]
                V(lambda e: e.tensor_tensor(out=cnts, in0=pref[:, NT - 1, :], in1=CSs[:, NT - 1, :], op=ALU.add), SL, SL)
                c3 = cmp2[:, 0:32, :]
                V(lambda e: e.tensor_tensor(out=c3, in0=cnts.unsqueeze(2).to_broadcast([128, 32, 32]),
                                            in1=bstart[:, 0:32].unsqueeze(1).to_broadcast([128, 32, 32]), op=ALU.is_gt),
                  SL + ['c_bstart'], SL)
                V(lambda e: e.tensor_reduce(out=nblk_, in_=c3, axis=AX.X, op=ALU.add), SL, SL)
                V(lambda e: e.tensor_scalar(out=padd, in0=nblk_, scalar1=128.0, scalar2=None, op0=ALU.mult), SL, SL)
                V(lambda e: e.tensor_copy(out=incA, in_=padd), SL, SL)
                cur, nxt = incA, incB
                for s_ in (1, 2, 4, 8, 16):
                    V(lambda e: e.tensor_copy(out=nxt[:, 0:s_], in_=cur[:, 0:s_]), SL, SL)
                    V(lambda e: e.tensor_tensor(out=nxt[:, s_:32], in0=cur[:, s_:32], in1=cur[:, 0:32 - s_], op=ALU.add), SL, SL)
                    cur, nxt = nxt, cur
                endp = cur
                V(lambda e: e.tensor_tensor(out=startp, in0=endp, in1=padd, op=ALU.subtract), SL, SL)
                V(lambda e: e.tensor_tensor(out=tmpd[:], in0=pref[:], in1=startp.unsqueeze(1).to_broadcast([128, NT, 32]),
                                            op=ALU.add), SL, SL)
                for hf in range(2):
                    V(lambda e: e.tensor_tensor(out=destf[:, hf * 16:(hf + 1) * 16, :].rearrange("p t e -> p (t e)"),
                                                in0=tmpd[:, hf * 16:(hf + 1) * 16, :].rearrange("p t e -> p (t e)"),
                                                in1=ps[hf][:, :], op=ALU.add), SL, [f"ps{hf}"] + SL)
                for k_, (Ma, Mtok, dki) in enumerate(((M1a, 'M1a', d1i), (M2a, 'M2a', d2i))):
                    V(lambda e: e.tensor_tensor(out=tmpd[:], in0=destf[:], in1=Ma[:], op=ALU.mult), SL + [Mtok], SL)
                    V(lambda e: e.tensor_reduce(out=dkf[:, k_, :], in_=tmpd[:], axis=AX.X, op=ALU.add), SL, SL)
                    V(lambda e: e.tensor_copy(out=dki[:], in_=dkf[:, k_, :]), SL, ['dki'])
                V(lambda e: e.tensor_tensor(out=cmp2[:], in0=endp.unsqueeze(1).to_broadcast([128, NBLK, 32]),
                                            in1=bstart[:, :].unsqueeze(2).to_broadcast([128, NBLK, 32]), op=ALU.is_le),
                  SL + ['c_bstart'], SL)
                V(lambda e: e.tensor_reduce(out=bef[:], in_=cmp2[:], axis=AX.X, op=ALU.add), SL, SL)
                V(lambda e: e.tensor_scalar(out=bef[:], in0=bef[:], scalar1=31.0, scalar2=128.0, op0=ALU.min, op1=ALU.mult), SL, SL)
                V(lambda e: e.tensor_scalar(out=bef[:], in0=bef[:], scalar1=col[:, 5:6], scalar2=float(l * NE * 128),
                                            op0=ALU.add, op1=ALU.add), SL + ['c_col'], SL)
                V(lambda e: e.tensor_copy(out=widx[:], in_=bef[:]), SL, ['widx'])
                for q_ in range(8):
                    V(lambda e: e.tensor_scalar(out=widx4[:, q_, :], in0=bef[:], scalar1=8.0, scalar2=float(q_),
                                                op0=ALU.mult, op1=ALU.add), SL, ['widx'])
                for c_ in range(3):
                    V(lambda e: e.tensor_scalar(out=widx3[:, c_, :], in0=bef[:], scalar1=3.0, scalar2=float(c_),
                                                op0=ALU.mult, op1=ALU.add), SL, ['widx'])
                if stop_after in (('slots', l), ('moe_slots', l)):
                    dsb = sb(e1, "dsb", [128, 512], F32)
                    V(lambda e: e.memset(dsb[:], 0.0), [], ['dsb'])
                    V(lambda e: e.tensor_copy(out=dsb[:, 0:32], in_=d1i[:]), ['dki'], ['dsb'])
                    V(lambda e: e.tensor_copy(out=dsb[:, 32:64], in_=d2i[:]), ['dki'], ['dsb'])
                    V(lambda e: e.tensor_copy(out=dsb[:, 64:160], in_=widx[:]), ['widx'], ['dsb'])
                    V(lambda e: e.tensor_copy(out=dsb[:, 160:224], in_=gates[:].rearrange("p t k -> p (t k)")), ['gates'], ['dsb'])
                    V(lambda e: e.tensor_copy(out=dsb[:, 224:256], in_=cnts), SL, ['dsb'])
                    V(lambda e: e.tensor_copy(out=dsb[:, 256:288], in_=startp), SL, ['dsb'])
                    V(lambda e: e.tensor_copy(out=dsb[:, 288:320], in_=M1a[:, 0, :]), ['M1a'], ['dsb'])
                    V(lambda e: e.tensor_copy(out=dsb[:, 320:352], in_=M2a[:, 0, :]), ['M2a'], ['dsb'])
                    V(lambda e: e.tensor_copy(out=dsb[:, 352:405], in_=rt[:, 0:53]), ['rt'], ['dsb'])
                    DMA('sp', dbg, dsb[:], ['dsb'], ['dbg'])
                    fw.barrier()
                    return
                for t in range(NT):
                    for dki in (d1i, d2i):
                        fw.throttle('pool', 'd_rows_d', 2)
                        fw.dma('pool', lambda e: e.indirect_dma_start(
                            out=rows_d, out_offset=bass.IndirectOffsetOnAxis(ap=dki[:, t:t + 1], axis=0),
                            in_=h2bf[:, t, :], in_offset=None), ['h2bf', 'dki'], ['rows_d'])
                fw.barrier()
            with ExitStack() as e2:
                Xb = [sb(e2, f"Xb{i}", [128, D], BF16) for i in range(2)]
                XT = [sb(e2, f"XT{i}", [128, 8, 128], BF16) for i in range(2)]
                w1b = [sb(e2, f"w1b{i}", [128, 8, DE], BF16) for i in range(2)]
                w3b = [sb(e2, f"w3b{i}", [128, 8, DE], BF16) for i in range(2)]
                w2b = [sb(e2, f"w2b{i}", [128, 3, D], BF16) for i in range(2)]
                s1 = sb(e2, "s1", [128, 384], F32)
                actT = sb(e2, "actT", [128, 3, 128], BF16)
                ysb = [sb(e2, f"ysb{i}", [128, D], F32) for i in range(2)]

                def issue_loads(bi):
                    i = bi % 2
                    DMA('sp', Xb[i][:], rows_d[bi * 128:(bi + 1) * 128, :], ['rows_d'], [f"Xb{i}"])
                    fw.throttle('pool', 'd_wg', 0)
                    for q_ in range(8):
                        off = bass.IndirectOffsetOnAxis(ap=widx4[:, q_, bi:bi + 1], axis=0)
                        for (wb_, wv_, wt_) in ((w1b[i], w1v, f"w1b{i}"), (w3b[i], w3v, f"w3b{i}")):
                            fw.throttle('pool', 'd_wg', 2)
                            fw.dma('pool', lambda e: e.indirect_dma_start(out=wb_[:, q_, :], out_offset=None,
                                                                          in_=wv_, in_offset=off), ['widx'], [wt_], sem='d_wg')
                    fw.throttle('pool', 'd_wg', 0)
                    for c_ in range(3):
                        off = bass.IndirectOffsetOnAxis(ap=widx3[:, c_, bi:bi + 1], axis=0)
                        fw.throttle('pool', 'd_wg', 2)
                        fw.dma('pool', lambda e: e.indirect_dma_start(out=w2b[i][:, c_, :], out_offset=None, in_=w2v,
                                                                      in_offset=off), ['widx'], [f"w2b{i}"], sem='d_wg')
                    fw.throttle('pool', 'd_wg', 0)

                issue_loads(0)
                for bi in range(NBLK):
                    i = bi % 2
                    if bi + 1 < NBLK:
                        issue_loads(bi + 1)
                    xv = Xb[i][:].rearrange("s (p k) -> s p k", k=8)
                    for half in range(2):
                        bk = nb(0, 2)
                        pt = f"ps{bk}"
                        pbf = ps[bk][:].bitcast(BF16)
                        for q4 in range(4):
                            kc = half * 4 + q4
                            PE(lambda e: e.transpose(pbf[:, q4 * 128:(q4 + 1) * 128], xv[:, :, kc], ident_b[:]),
                               [f"Xb{i}", 'c_ident_b'], [pt])
                        V(lambda e: e.tensor_copy(out=XT[i][:, half * 4:half * 4 + 4, :],
                                                  in_=pbf[:, 0:512].rearrange("p (q c) -> p q c", c=128)), [], [pt, f"XT{i}"])
                    b1, b3 = 2 + (bi % 2) * 2, 3 + (bi % 2) * 2
                    for (wb_, wt_, bk) in ((w1b[i], f"w1b{i}", b1), (w3b[i], f"w3b{i}", b3)):
                        for c in range(3):
                            for kc in range(8):
                                MM(ps[bk][:, c * 128:(c + 1) * 128], wb_[:, kc, :].rearrange("p (q c) -> p q c", c=3)[:, :, c],
                                   XT[i][:, kc, :], kc == 0, kc == 7, [wt_, f"XT{i}"], [f"ps{bk}"])
                    A(lambda e: e.activation(out=s1[:], in_=ps[b1][:, 0:384], func=AF.Silu), [], [f"ps{b1}", 's1'])
                    V(lambda e: e.tensor_tensor(out=actT[:].rearrange("p c s -> p (c s)"), in0=ps[b3][:, 0:384], in1=s1[:], op=ALU.mult),
                      ['s1'], [f"ps{b3}", 'actT'])
                    for nh in range(2):
                        bk = 6 + nh
                        for c in range(3):
                            MM(ps[bk][:, :], actT[:, c, :], w2b[i][:, c, nh * 512:(nh + 1) * 512], c == 0, c == 2,
                               ['actT', f"w2b{i}"], [f"ps{bk}"])
                        if nh == 0:
                            A(lambda e: e.copy(out=ysb[i][:, 0:512], in_=ps[bk][:, :]), [], [f"ps{bk}", f"ysb{i}"])
                        else:
                            V(lambda e: e.tensor_copy(out=ysb[i][:, 512:1024], in_=ps[bk][:, :]), [], [f"ps{bk}", f"ysb{i}"])
                    DMA('sp', y_d[bi * 128:(bi + 1) * 128, :], ysb[i][:], [f"ysb{i}"], ['y_d'])
                    if stop_after == ('moe_blk0', l) and bi == 0:
                        dd_ = sb(e2, "dd_", [128, D], F32)
                        V(lambda e: e.tensor_copy(out=dd_[:], in_=Xb[0][:]), ['Xb0'], ['dd_'])
                        DMA('sp', out[0:128, :], dd_[:], ['dd_'], ['dbgo'])
                        V(lambda e: e.memset(dd_[:], 0.0), [], ['dd_'])
                        V(lambda e: e.tensor_copy(out=dd_[:, 0:768], in_=w1b[0][:, 0:2, :].rearrange("p a b -> p (a b)")), ['w1b0'], ['dd_'])
                        DMA('sp', out[128:256, :], dd_[:], ['dd_'], ['dbgo'])
                        V(lambda e: e.tensor_copy(out=dd_[:], in_=w2b[0][:, 0, :]), ['w2b0'], ['dd_'])
                        DMA('sp', out[256:384, :], dd_[:], ['dd_'], ['dbgo'])
                        DMA('sp', out[384:512, :], ysb[0][:], ['ysb0'], ['dbgo'])
                        V(lambda e: e.memset(dd_[:], 0.0), [], ['dd_'])
                        V(lambda e: e.tensor_copy(out=dd_[:, 0:32], in_=d1i[:]), ['dki'], ['dd_'])
                        V(lambda e: e.tensor_copy(out=dd_[:, 32:64], in_=d2i[:]), ['dki'], ['dd_'])
                        V(lambda e: e.tensor_copy(out=dd_[:, 64:160], in_=widx[:]), ['widx'], ['dd_'])
                        V(lambda e: e.tensor_copy(out=dd_[:, 160:256], in_=widx4[:, 1, :]), ['widx'], ['dd_'])
                        V(lambda e: e.tensor_copy(out=dd_[:, 256:640], in_=actT[:].rearrange("p c s -> p (c s)")), ['actT'], ['dd_'])
                        V(lambda e: e.tensor_copy(out=dd_[:, 640:768], in_=XT[0][:, 3, :]), ['XT0'], ['dd_'])
                        DMA('sp', out[640:768, :], dd_[:], ['dd_'], ['dbgo'])
                        fw.barrier()
                        return
                fw.barrier()
            with ExitStack() as e3:
                xt = [sb(e3, f"xc{i}", [128, D], F32) for i in range(2)]
                y1 = [sb(e3, f"y1{i}", [128, D], F32) for i in range(2)]
                y2 = [sb(e3, f"y2{i}", [128, D], F32) for i in range(2)]
                for t in range(NT):
                    i = t % 2
                    b = t // 16
                    r0 = t * 128
                    DMA('sp', xt[i][:], out[r0:r0 + 128, :], ['xout'], [f"xc{i}"])
                    fw.throttle('pool', 'd_yg', 2)
                    fw.dma('pool', lambda e: e.indirect_dma_start(out=y1[i][:], out_offset=None, in_=y_d,
                                                                  in_offset=bass.IndirectOffsetOnAxis(ap=d1i[:, t:t + 1], axis=0)),
                           ['y_d', 'dki'], [f"y1{i}"], sem='d_yg')
                    fw.throttle('pool', 'd_yg', 2)
                    fw.dma('pool', lambda e: e.indirect_dma_start(out=y2[i][:], out_offset=None, in_=y_d,
                                                                  in_offset=bass.IndirectOffsetOnAxis(ap=d2i[:, t:t + 1], axis=0)),
                           ['y_d', 'dki'], [f"y2{i}"], sem='d_yg')
                    V(lambda e: e.tensor_scalar(out=y1[i][:], in0=y1[i][:], scalar1=gates[:, t, 0:1], scalar2=None, op0=ALU.mult),
                      [f"y1{i}", 'gates'], [f"y1{i}"])
                    V(lambda e: e.scalar_tensor_tensor(out=y1[i][:], in0=y2[i][:], scalar=gates[:, t, 1:2], in1=y1[i][:],
                                                       op0=ALU.mult, op1=ALU.add), [f"y1{i}", f"y2{i}", 'gates'], [f"y1{i}"])
                    G(lambda e: e.tensor_tensor(out=y1[i][:], in0=y1[i][:], in1=gt2b[:, b, :], op=ALU.mult), [f"y1{i}", 'gt2b'], [f"y1{i}"])
                    G(lambda e: e.tensor_tensor(out=xt[i][:], in0=xt[i][:], in1=y1[i][:], op=ALU.add), [f"y1{i}", f"xc{i}"], [f"xc{i}"])
                    DMA('sp', out[r0:r0 + 128, :], xt[i][:], [f"xc{i}"], ['xout2'])
                fw.barrier()

    done = False
    if moe_only:
        stage0(0)
        with ExitStack() as cp:
            xc_ = [sb(cp, f"xcp{i}", [128, D], F32) for i in range(2)]
            for t in range(NT):
                DMA('sp', xc_[t % 2][:], dr['x'][t * 128:(t + 1) * 128, :], [], [f"xcp{t % 2}"])
                DMA('sp', out[t * 128:(t + 1) * 128, :], xc_[t % 2][:], [f"xcp{t % 2}"], ['xout'])
            fw.barrier()
        stage_moe(0)
        nlayers = 0
    for l in range(nlayers):
        stage0(l)
        if stop_after == ('stage0', l):
            break
        for b in seqs:
            with ExitStack() as ms:
                oT_mla = sb(ms, "oT_mla", [128, 4, S], BF16)
                oT_dil = sb(ms, "oT_dil", [128, 2, S], BF16)
                oT_win = sb(ms, "oT_win", [128, 4, S], BF16)
                stageA(l, b)
                if stop_after in (('A', l), ('A', l, b)):
                    done = True
                    break
                stage_mla(l, b, oT_mla)
                if stop_after in (('mla', l), ('mla', l, b)):
                    done = True
                    break
                stage_band(l, b, oT_dil, oT_win)
                if stop_after in (('band', l), ('band', l, b)):
                    done = True
                    break
                stage_merge(l, b, oT_mla, oT_dil, oT_win)
                if stop_after in (('merge', l), ('merge', l, b)):
                    done = True
                    break
        if done:
            break
        xsrc[0] = out
        if stop_after == ('mixer', l):
            break
        stage_moe(l)
        if stop_after in (('layer', l), ('slots', l), ('e1tiles', l)):
            break
    fw.barrier()
    top.close()
    build_program.stats = (fw.ninst, fw.nwaits, len(fw.semobj))
    return nc


def kernel(**inputs):
    inputs = {k: np.asarray(v) for k, v in inputs.items()}
    shared = host_shared(inputs)
    in_maps = []
    for core in range(8):
        m = dict(shared)
        m.update(host_layout(inputs, core))
        in_maps.append(m)
    nc = build_program()
    res = run_bass_kernel_spmd(nc, in_maps, core_ids=list(range(8)))
    outs = [np.asarray(r["out"]).reshape(NSEQ, S, D) for r in res.results]
    return np.concatenate(outs, axis=0).astype(np.float32)
```

```python
import math
from contextlib import ExitStack
import numpy as np
import ml_dtypes
import concourse.bass as bass
import concourse.mybir as mybir
from concourse.bass_utils import run_bass_kernel_spmd

F32 = mybir.dt.float32
BF16 = mybir.dt.bfloat16
I32 = mybir.dt.int32
ALU = mybir.AluOpType
AF = mybir.ActivationFunctionType
AX = mybir.AxisListType

D = 1024
S = 2048
NSEQ = 2
T = NSEQ * S
NT = T // 128
L = 2
IN_COLS = 6944
OFF_CQ, OFF_CKV, OFF_KR, OFF_DIL, OFF_WIN, OFF_GATE = 0, 512, 768, 800, 3104, 3872
NE = 32
DE = 384
NBLK = 2 * T // 128 + NE
NSLOT = NBLK * 128
EPS = 1e-6
BIG = 1.0e6
MLA_SCALE = 96 ** -0.5
HD_SCALE = 64 ** -0.5
DIL_R = (1, 2, 8)
DIL_PAT = ((128, 1), (512, 4), (2048, 16))
MASK_BASE = (0, 3, 8, 25)
NMASK = 28


def alibi(n):
    return [2.0 ** (-8.0 * (i + 1) / n) for i in range(n)]


class FW:
    def __init__(self, nc, same_engine_sync=True):
        self.nc = nc
        self.eng = {'pe': nc.tensor, 'act': nc.scalar, 'dve': nc.vector, 'pool': nc.gpsimd, 'sp': nc.sync}
        self.semobj = {}
        self.cnt = {}
        self.dcnt = {}
        for e in ['pe', 'act', 'dve', 'pool']:
            self.semobj[e] = nc.alloc_semaphore("s_" + e)
            self.cnt[e] = 0
        self.pool = [nc.alloc_semaphore(f"s_dma{i}") for i in range(84)]
        self.allsems = [self.semobj[e] for e in ['pe', 'act', 'dve', 'pool']] + list(self.pool)
        self.seen = {e: {} for e in self.eng}
        self.lastw = {}
        self.readers = {}
        self.same = same_engine_sync
        self.nwaits = 0
        self.ninst = 0

    def clear_all(self):
        for sm in self.allsems:
            self.nc.gpsimd.sem_clear(sm)
        self.nc.all_engine_barrier()

    def _wait(self, E, ev):
        key, val = ev
        if key == E and (E == 'pe' or not self.same):
            return
        if key in self.dcnt:
            val = self.dcnt[key]
        if self.seen[E].get(key, 0) >= val:
            return
        self.eng[E].wait_ge(self.semobj[key], val)
        self.seen[E][key] = val
        self.nwaits += 1

    def _deps(self, E, reads, writes):
        for t in reads:
            ev = self.lastw.get(t)
            if ev is not None:
                self._wait(E, ev)
        for t in writes:
            ev = self.lastw.get(t)
            if ev is not None:
                self._wait(E, ev)
            for ev in self.readers.get(t, ()):
                self._wait(E, ev)

    def _commit(self, ev, reads, writes):
        for t in reads:
            lst = self.readers.setdefault(t, [])
            lst.append(ev)
            if len(lst) > 16:
                d = {}
                for k, v in lst:
                    d[k] = max(d.get(k, 0), v)
                self.readers[t] = list(d.items())
        for t in writes:
            self.lastw[t] = ev
            self.readers[t] = []

    def op(self, E, fn, reads=(), writes=()):
        self._deps(E, reads, writes)
        inst = fn(self.eng[E])
        self.cnt[E] += 1
        inst.then_inc(self.semobj[E], 1)
        self.ninst += 1
        self._commit((E, self.cnt[E]), reads, writes)
        return inst

    def dma(self, Q, fn, reads=(), writes=(), sem=None):
        if sem is None:
            sem = 'd_' + (writes[0] if writes else reads[0])
        if sem not in self.semobj:
            self.semobj[sem] = self.pool.pop()
            self.dcnt[sem] = 0
        self._deps(Q, reads, writes)
        if Q == 'pool':
            hist = self.__dict__.setdefault('pool_hist', [])
            if len(hist) >= 2:
                hk, hv = hist[-2]
                if self.seen['pool'].get(hk, 0) < hv:
                    self.eng['pool'].wait_ge(self.semobj[hk], hv)
                    self.seen['pool'][hk] = hv
                    self.nwaits += 1
        inst = fn(self.eng[Q])
        self.dcnt[sem] += 16
        inst.then_inc(self.semobj[sem], 16)
        self.ninst += 1
        self._commit((sem, self.dcnt[sem]), reads, writes)
        if Q == 'pool':
            self.pool_hist.append((sem, self.dcnt[sem]))
            if len(self.pool_hist) > 4:
                self.pool_hist.pop(0)
        return inst

    def throttle(self, Q, sem, lag):
        v = self.dcnt.get(sem, 0) - 16 * lag
        if v > 0 and self.seen[Q].get(sem, 0) < v:
            self.eng[Q].wait_ge(self.semobj[sem], v)
            self.seen[Q][sem] = v
            self.nwaits += 1

    def barrier(self):
        for E in self.eng:
            for k, v in self.cnt.items():
                if v > 0:
                    self._wait(E, (k, v))
            for k, v in self.dcnt.items():
                if v > 0:
                    self._wait(E, (k, v))
        self.lastw = {}
        self.readers = {}


def host_consts():
    c = {}
    c['c_ident_f'] = np.eye(128, dtype=np.float32)
    c['c_ident_b'] = np.eye(128, dtype=np.float32).astype(ml_dtypes.bfloat16)
    c['c_ones_b'] = np.ones((128, 128), np.float32).astype(ml_dtypes.bfloat16)
    blk = np.zeros((128, 128), np.float32)
    blk[0:64, 0:64] = 1
    blk[64:96, 64:96] = 1
    c['c_blk96_b'] = blk.astype(ml_dtypes.bfloat16)
    j = np.arange(128)
    c['c_ustrict_b'] = (j[:, None] < j[None, :]).astype(np.float32).astype(ml_dtypes.bfloat16)
    masks = np.zeros((128, NMASK, 128), np.float32)
    k = np.arange(128)[:, None]
    q = np.arange(128)[None, :]
    for g in range(3):
        win, dil = DIL_PAT[g]
        R = DIL_R[g]
        for dl in range(-R, R + 1):
            delta = 128 * dl + k - q
            valid = (np.abs(delta) <= win // 2) & (delta % dil == 0)
            masks[:, MASK_BASE[g] + dl + R, :] = np.where(valid, 0.0, BIG)
    for dl in range(-1, 2):
        delta = 128 * dl + k - q
        valid = np.abs(delta) <= 128
        masks[:, MASK_BASE[3] + dl + 1, :] = np.where(valid, 0.0, BIG)
    c['c_masks'] = masks
    col = np.zeros((128, 8), np.float32)
    col[:, 0] = 1.0
    col[0:64, 0] = 1.0 / 64
    col[64:96, 0] = 1.0 / 32
    col[:, 1] = EPS
    invf = 10000.0 ** (-np.arange(16, dtype=np.float32) / 16)
    col[64:80, 2] = -invf
    col[80:96, 2] = invf
    col[64:80, 3] = invf
    col[80:96, 3] = invf
    col[:, 4] = 1.0 / 64
    col[:, 5] = np.arange(128)
    col[:, 6] = math.pi / 2
    col[:, 7] = 0.0
    c['c_col'] = col
    c['c_bstart'] = np.tile((np.arange(NBLK, dtype=np.float32) * 128)[None, :], (128, 1))
    return c


CONST_SPECS = [('c_ident_f', [128, 128], F32), ('c_ident_b', [128, 128], BF16), ('c_ones_b', [128, 128], BF16),
               ('c_blk96_b', [128, 128], BF16), ('c_ustrict_b', [128, 128], BF16),
               ('c_masks', [128, NMASK, 128], F32), ('c_col', [128, 8], F32), ('c_bstart', [128, NBLK], F32)]

IN_SPECS = [('x', [T, D], F32), ('cT', [128, 8, 2], F32), ('pos', [NSEQ, S], I32), ('posT', [128, NSEQ, 16], I32),
            ('w_ada', [L, D, 6 * D], F32), ('b_ada', [L, 1, 6 * D], F32), ('g1T', [L, 128, 8], F32),
            ('w_in', [L, D, IN_COLS], F32), ('gcqT', [L, 128, 4], F32), ('w_uq', [L, 512, 768], F32),
            ('gckvT', [L, 128, 2], F32), ('w_ukv', [L, 256, 1024], F32), ('pcol', [L, 128, 8], F32),
            ('sink', [L, 1, 8], F32), ('w_br_mla', [L, 512, D], F32), ('w_br_dil', [L, 256, D], F32),
            ('w_br_win', [L, 512, D], F32), ('w_out', [L, D, D], F32), ('g_norm2', [L, 1, D], F32),
            ('w_r', [L, D, 36], F32), ('b_r', [L, 1, 36], F32),
            ('w1', [L, NE, D, DE], F32), ('w3', [L, NE, D, DE], F32), ('w2', [L, NE, DE, D], F32)]


def host_layout(inp, core):
    b0 = core * NSEQ
    m = {}
    m['x'] = np.ascontiguousarray(inp['x'][b0:b0 + NSEQ].reshape(T, D))
    c = inp['c'][b0:b0 + NSEQ]
    m['cT'] = np.ascontiguousarray(c.reshape(NSEQ, 8, 128).transpose(2, 1, 0))
    pos = np.ascontiguousarray(inp['pos'][b0:b0 + NSEQ]).astype(np.int32)
    m['pos'] = pos
    m['posT'] = np.ascontiguousarray(pos.reshape(NSEQ, 16, 128).transpose(2, 0, 1))
    return m


def host_shared(inp):
    m = {}
    m['w_ada'] = inp['w_ada']
    m['b_ada'] = inp['b_ada'].reshape(L, 1, 6 * D)
    m['g1T'] = np.ascontiguousarray(inp['g_norm1'].reshape(L, 8, 128).transpose(0, 2, 1))
    m['w_in'] = inp['w_in']
    m['gcqT'] = np.ascontiguousarray(inp['g_cq'].reshape(L, 4, 128).transpose(0, 2, 1))
    m['w_uq'] = inp['w_uq']
    m['gckvT'] = np.ascontiguousarray(inp['g_ckv'].reshape(L, 2, 128).transpose(0, 2, 1))
    m['w_ukv'] = inp['w_ukv']
    pcol = np.zeros((L, 128, 8), np.float32)
    for l in range(L):
        gq, gk = inp['g_q_mla'][l], inp['g_k_mla'][l]
        pcol[l, 0:96, 0] = gq
        pcol[l, 64:80, 1] = gq[80:96]
        pcol[l, 80:96, 1] = gq[64:80]
        pcol[l, 0:96, 2] = gk
        pcol[l, 64:80, 3] = gk[80:96]
        pcol[l, 80:96, 3] = gk[64:80]
        pcol[l, 0:64, 4] = inp['g_q_dil'][l]
        pcol[l, 0:64, 5] = inp['g_k_dil'][l]
        pcol[l, 0:64, 6] = inp['g_q_win'][l]
        pcol[l, 0:64, 7] = inp['g_k_win'][l]
    m['pcol'] = pcol
    m['sink'] = inp['sink_win'].reshape(L, 1, 8)
    m['w_br_mla'] = inp['w_br_mla']
    m['w_br_dil'] = inp['w_br_dil']
    m['w_br_win'] = inp['w_br_win']
    m['w_out'] = inp['w_out']
    m['g_norm2'] = inp['g_norm2'].reshape(L, 1, D)
    m['w_r'] = np.ascontiguousarray(np.concatenate([inp['w_gr'], inp['w_er']], axis=2))
    m['b_r'] = np.ascontiguousarray(np.concatenate([inp['b_gr'], inp['b_er']], axis=1)).reshape(L, 1, 36)
    m['w1'] = inp['w1']
    m['w3'] = inp['w3']
    m['w2'] = inp['w2']
    m = {k: np.ascontiguousarray(v.astype(np.float32)) for k, v in m.items()}
    m.update(host_consts())
    return m


def build_program(stop_after=None, nlayers=L, same_engine_sync=True, tiny_moe=False, seqs=(0, 1), moe_only=False):
    nc = bass.Bass("TRN2", target_bir_lowering=False)
    fw = FW(nc, same_engine_sync=same_engine_sync)
    dr = {}
    for name, shape, dt in IN_SPECS + CONST_SPECS:
        if tiny_moe and name in ('w1', 'w3', 'w2'):
            shape = [L, 1] + list(shape[2:])
        dr[name] = nc.dram_tensor(name, list(shape), dt, kind="ExternalInput").ap()
    out = nc.dram_tensor("out", [T, D], F32, kind="ExternalOutput").ap()
    dbg = out[0:128, 0:512]
    hT_d = nc.dram_tensor("hT_d", [NSEQ, 128, 8, S], BF16, kind="Internal").ap()
    bc_d = nc.dram_tensor("bc_d", [3, NSEQ, 128, D], F32, kind="Internal").ap()
    rows_d = nc.dram_tensor("rows_d", [NSLOT, D], BF16, kind="Internal").ap()
    y_d = nc.dram_tensor("y_d", [NSLOT, D], F32, kind="Internal").ap()

    ps = [nc.alloc_psum_tensor(f"ps{i}", [128, 512], F32) for i in range(8)]

    def V(fn, r=(), w=()):
        return fw.op('dve', fn, r, w)

    def A(fn, r=(), w=()):
        return fw.op('act', fn, r, w)

    def G(fn, r=(), w=()):
        return fw.op('pool', fn, r, w)

    def PE(fn, r=(), w=()):
        return fw.op('pe', fn, r, w)

    def MM(o, lhsT, rhs, start, stop, r, w):
        return fw.op('pe', lambda e: e.matmul(o, lhsT=lhsT, rhs=rhs, start=start, stop=stop), r, w)

    def DMA(q, o, i, r, w, sem=None):
        return fw.dma(q, lambda e: e.dma_start(out=o, in_=i), r, w, sem=sem)

    uid = [0]

    def sb(es, name, shape, dt):
        uid[0] += 1
        return es.enter_context(nc.sbuf_tensor(f"{name}_{uid[0]}", list(shape), dt))

    top = ExitStack()
    cs = {}
    for name, shape, dt in CONST_SPECS:
        cs[name] = sb(top, "s_" + name, shape, dt)
        DMA('sp', cs[name][:], dr[name], [], [name])
    ident_f, ident_b, ones_b = cs['c_ident_f'], cs['c_ident_b'], cs['c_ones_b']
    blk96, ustrict, masks, col, bstart = cs['c_blk96_b'], cs['c_ustrict_b'], cs['c_masks'], cs['c_col'], cs['c_bstart']
    CONST_T = [n for n, _, _ in CONST_SPECS]
    ones_f = sb(top, "ones_f", [128, 128], F32)
    V(lambda e: e.memset(ones_f[:], 1.0), [], ['ones_f'])
    condT = sb(top, "condT", [128, 8, 2], F32)
    condrep = sb(top, "condrep", [128, 8, 2, 128], F32)
    modT = sb(top, "modT", [128, 16, 2], F32)
    A1 = sb(top, "A1", [128, 8, 2], F32)
    gt1b = sb(top, "gt1b", [128, NSEQ, D], F32)
    pcol = sb(top, "pcol", [128, 8], F32)
    g1T = sb(top, "g1T", [128, 8], F32)
    gcqT = sb(top, "gcqT", [128, 4], F32)
    gckvT = sb(top, "gckvT", [128, 2], F32)
    esink = sb(top, "esink", [128, 8], F32)
    poskT = sb(top, "poskT", [128, NSEQ, 16], F32)
    poskT_i = sb(top, "poskT_i", [128, NSEQ, 16], I32)
    DMA('sp', condT[:], dr['cT'], [], ['condT'])
    A(lambda e: e.activation(out=condT[:], in_=condT[:], func=AF.Silu), ['condT'], ['condT'])
    for b in range(NSEQ):
        V(lambda e: e.tensor_copy(out=condrep[:, :, b, :], in_=condT[:, :, b:b + 1].to_broadcast([128, 8, 128])),
          ['condT'], ['condrep'])
    DMA('sp', poskT_i[:], dr['posT'], [], ['poskT_i'])
    V(lambda e: e.tensor_copy(out=poskT[:], in_=poskT_i[:]), ['poskT_i'], ['poskT'])

    bank_rr = [0]

    def nb(lo=0, hi=8):
        b = lo + bank_rr[0] % (hi - lo)
        bank_rr[0] += 1
        return b

    xsrc = [dr['x']]

    def stage0(l):
        with ExitStack() as es:
            wblk = [sb(es, f"wada{i}", [128, 8, 512], F32) for i in range(2)]
            brow = sb(es, "brow", [1, 6 * D], F32)
            g2b = sb(es, "g2b", [128, D], F32)
            stg = [sb(es, f"stg{i}", [128, D], F32) for i in range(2)]
            DMA('sp', brow[:], dr['b_ada'][l], [], ['brow'])
            DMA('sp', g2b[:], dr['g_norm2'][l, 0].partition_broadcast(128), [], ['g2b'])
            DMA('sp', pcol[:], dr['pcol'][l], [], ['pcol'])
            DMA('sp', g1T[:], dr['g1T'][l], [], ['g1T'])
            DMA('sp', gcqT[:], dr['gcqT'][l], [], ['gcqT'])
            DMA('sp', gckvT[:], dr['gckvT'][l], [], ['gckvT'])
            DMA('sp', esink[:], dr['sink'][l, 0].partition_broadcast(128), [], ['esink'])
            A(lambda e: e.activation(out=esink[:], in_=esink[:], func=AF.Exp), ['esink'], ['esink'])
            wv = dr['w_ada'][l].rearrange("(kc p) n -> p kc n", p=128)
            for cb in range(12):
                wb = wblk[cb % 2]
                wt = f"wada{cb % 2}"
                DMA('sp', wb[:], wv[:, :, cb * 512:(cb + 1) * 512], [], [wt])
                if cb < 4:
                    for j in range(4):
                        bk = nb()
                        pt = f"ps{bk}"
                        for kc in range(8):
                            MM(ps[bk][:, 0:2], wb[:, kc, j * 128:(j + 1) * 128], condT[:, kc, :], kc == 0, False,
                               [wt, 'condT'], [pt])
                        MM(ps[bk][:, 0:2], brow[0:1, cb * 512 + j * 128: cb * 512 + (j + 1) * 128], ones_f[0:1, 0:2],
                           False, True, ['brow', 'ones_f'], [pt])
                        V(lambda e: e.tensor_copy(out=modT[:, cb * 4 + j, :], in_=ps[bk][:, 0:2]), [], [pt, 'modT'])
                else:
                    which = (cb - 4) // 2
                    nh = (cb - 4) % 2
                    for b in range(NSEQ):
                        bk = nb()
                        pt = f"ps{bk}"
                        for kc in range(8):
                            MM(ps[bk][:, :], condrep[:, kc, b, :], wb[:, kc, :], kc == 0, False, [wt, 'condrep'], [pt])
                        MM(ps[bk][:, :], ones_f[0:1, 0:128], brow[0:1, cb * 512:(cb + 1) * 512], False, True,
                           ['brow', 'ones_f'], [pt])
                        if which == 0:
                            V(lambda e: e.tensor_copy(out=gt1b[:, b, nh * 512:(nh + 1) * 512], in_=ps[bk][:, :]),
                              [], [pt, 'gt1b'])
                        else:
                            st = stg[b]
                            stt = f"stg{b}"
                            if which == 2:
                                V(lambda e: e.scalar_tensor_tensor(out=st[:, nh * 512:(nh + 1) * 512], in0=ps[bk][:, :],
                                                                   scalar=1.0, in1=g2b[:, nh * 512:(nh + 1) * 512],
                                                                   op0=ALU.add, op1=ALU.mult), ['g2b'], [pt, stt])
                            else:
                                V(lambda e: e.tensor_copy(out=st[:, nh * 512:(nh + 1) * 512], in_=ps[bk][:, :]),
                                  [], [pt, stt])
                            if nh == 1:
                                DMA('sp', bc_d[which - 1, b], st[:], [stt], ['bc_d'])
            V(lambda e: e.scalar_tensor_tensor(out=A1[:], in0=modT[:, 8:16, :], scalar=1.0,
                                               in1=g1T[:].unsqueeze(2).to_broadcast([128, 8, 2]),
                                               op0=ALU.add, op1=ALU.mult), ['modT', 'g1T'], ['A1'])
            fw.barrier()

    def stageA(l, b):
        with ExitStack() as es:
            xt = [sb(es, f"xa{i}", [128, D], F32) for i in range(2)]
            xn = [sb(es, f"xn{i}", [128, D], F32) for i in range(2)]
            junk = sb(es, "junkA", [128, D], BF16)
            st = [sb(es, f"stA{i}", [128, 4], F32) for i in range(2)]
            hT = [sb(es, f"hTa{i}", [128, 8, 128], BF16) for i in range(2)]
            for t in range(16):
                i = t % 2
                r0 = b * S + t * 128
                DMA('sp', xt[i][:], xsrc[0][r0:r0 + 128, :], ['xres'], [f'xa{i}'])
                A(lambda e: e.activation(out=junk[:], in_=xt[i][:], func=AF.Square, accum_out=st[i][:, 0:1]),
                  [f'xa{i}'], ['junkA', f'stA{i}'])
                A(lambda e: e.activation(out=st[i][:, 1:2], in_=st[i][:, 0:1], func=AF.Ln, scale=1.0 / D, bias=col[:, 1:2]),
                  [f'stA{i}', 'c_col'], [f'stA{i}'])
                A(lambda e: e.activation(out=st[i][:, 2:3], in_=st[i][:, 1:2], func=AF.Exp, scale=-0.5),
                  [f'stA{i}'], [f'stA{i}'])
                V(lambda e: e.tensor_scalar(out=xn[i][:], in0=xt[i][:], scalar1=st[i][:, 2:3], scalar2=None, op0=ALU.mult),
                  [f'xa{i}', f'stA{i}'], [f'xn{i}'])
                for half in range(2):
                    bk = nb()
                    pt = f"ps{bk}"
                    for q4 in range(4):
                        kc = half * 4 + q4
                        PE(lambda e: e.transpose(ps[bk][:, q4 * 128:(q4 + 1) * 128], xn[i][:, kc * 128:(kc + 1) * 128], ident_f[:]),
                           [f'xn{i}', 'c_ident_f'], [pt])
                    for q4 in range(4):
                        kc = half * 4 + q4
                        V(lambda e: e.tensor_scalar(out=hT[i][:, kc, :], in0=ps[bk][:, q4 * 128:(q4 + 1) * 128],
                                                    scalar1=A1[:, kc, b:b + 1], scalar2=modT[:, kc, b:b + 1],
                                                    op0=ALU.mult, op1=ALU.add), ['A1', 'modT'], [pt, f'hTa{i}'])
                DMA('sp', hT_d[b][:, :, t * 128:(t + 1) * 128], hT[i][:], [f'hTa{i}'], ['hT_d'])
            fw.barrier()

    def headnorm(P, nrows, rs, gcol, out_ap, out_tok, tmp, Psw=None, gsw=None, CC=None, SS=None, P_tok=(), extra_r=()):
        qs, sq, sd, qn, t2 = tmp['qs'], tmp['sq'], tmp['sd'], tmp['qn'], tmp['t2']
        n = nrows
        if rs is not None:
            V(lambda e: e.tensor_tensor(out=qs[0:n, :], in0=P, in1=rs, op=ALU.mult), list(extra_r), list(P_tok) + ['hn_qs'])
            src = qs[0:n, :]
            src_r, src_w = ['hn_qs'], []
        else:
            src = P
            src_r, src_w = [], list(P_tok)
        A(lambda e: e.activation(out=sq[0:n, :], in_=src, func=AF.Square), src_r, src_w + ['hn_sq'])
        bk = nb(6, 8)
        pt = f"ps{bk}"
        blk = blk96 if n == 96 else ones_b
        MM(ps[bk][0:n, :], blk[0:n, 0:n], sq[0:n, :], True, True, ['hn_sq', 'c_blk96_b', 'c_ones_b'], [pt])
        sc = col[0:n, 0:1] if n == 96 else col[0:n, 4:5]
        A(lambda e: e.activation(out=sd[0:n, :], in_=ps[bk][0:n, :], func=AF.Ln, scale=sc, bias=col[0:n, 1:2]),
          ['c_col'], [pt, 'hn_sd'])
        A(lambda e: e.activation(out=sd[0:n, :], in_=sd[0:n, :], func=AF.Exp, scale=-0.5), ['hn_sd'], ['hn_sd'])
        if Psw is None:
            V(lambda e: e.scalar_tensor_tensor(out=out_ap, in0=src, scalar=gcol, in1=sd[0:n, :], op0=ALU.mult, op1=ALU.mult),
              src_r + ['hn_sd', 'pcol'], src_w + [out_tok])
            return
        V(lambda e: e.scalar_tensor_tensor(out=qn[0:n, :], in0=src, scalar=gcol, in1=sd[0:n, :], op0=ALU.mult, op1=ALU.mult),
          src_r + ['hn_sd', 'pcol'], src_w + ['hn_qn'])
        Psw_ap, Psw_tok = Psw
        if rs is not None:
            V(lambda e: e.scalar_tensor_tensor(out=t2[0:n, :], in0=Psw_ap, scalar=gsw, in1=rs, op0=ALU.mult, op1=ALU.mult),
              ['pcol'] + list(extra_r), list(Psw_tok) + ['hn_t2'])
        else:
            V(lambda e: e.tensor_scalar(out=t2[0:n, :], in0=Psw_ap, scalar1=gsw, scalar2=None, op0=ALU.mult),
              ['pcol'], list(Psw_tok) + ['hn_t2'])
        G(lambda e: e.tensor_tensor(out=t2[0:n, :], in0=t2[0:n, :], in1=sd[0:n, :], op=ALU.mult), ['hn_sd', 'hn_t2'], ['hn_t2'])
        G(lambda e: e.tensor_tensor(out=t2[0:n, :], in0=t2[0:n, :], in1=SS, op=ALU.mult), ['hn_t2', 'ropeT'], ['hn_t2'])
        G(lambda e: e.tensor_tensor(out=qn[0:n, :], in0=qn[0:n, :], in1=CC, op=ALU.mult), ['hn_qn', 'ropeT'], ['hn_qn'])
        V(lambda e: e.tensor_tensor(out=out_ap, in0=qn[0:n, :], in1=t2[0:n, :], op=ALU.add), ['hn_qn', 'hn_t2'], [out_tok])

    def mk_hn_tmp(es):
        return {'qs': sb(es, "hn_qs", [128, 512], F32), 'sq': sb(es, "hn_sq", [128, 512], BF16),
                'sd': sb(es, "hn_sd", [128, 512], F32), 'qn': sb(es, "hn_qn", [128, 512], F32),
                't2': sb(es, "hn_t2", [128, 512], F32)}

    def load_w_cast(dst_ap, src_ap, tok):
        fw.dma('pool', lambda e: e.dma_start(out=dst_ap, in_=src_ap), [], [tok])

    def proj_fm(bk, lhs_list, rhs_list, n, reads, rows=None):
        pt = f"ps{bk}"
        o = ps[bk][0:n, :] if rows is None else ps[bk][rows[0]:rows[1], :]
        K = len(lhs_list)
        for k in range(K):
            MM(o, lhs_list[k], rhs_list[k], k == 0, k == K - 1, reads, [pt])

    def stage_mla(l, b, oT_mla):
        TWO_PI = 2.0 * math.pi
        C1 = 6.28125
        C2 = TWO_PI - C1
        with ExitStack() as es:
            tmp = mk_hn_tmp(es)
            CC = sb(es, "CC", [128, S], F32)
            SS = sb(es, "SS", [128, S], F32)
            ropei = sb(es, "ropei", [128, 512], I32)
            for ch in range(4):
                cs_ = slice(ch * 512, (ch + 1) * 512)
                DMA('sp', ropei[0:96, :], dr['pos'][b, ch * 512:(ch + 1) * 512].partition_broadcast(96), ['hn_qs'], ['ropei'])
                V(lambda e: e.tensor_copy(out=tmp['qs'][0:96, :], in_=ropei[0:96, :]), ['ropei'], ['hn_qs'])
                for tab, ci, phase in ((CC, 3, math.pi / 2), (SS, 2, 0.0)):
                    ang, kf, r_ = tmp['sd'], tmp['qn'], tmp['t2']
                    V(lambda e: e.tensor_scalar(out=ang[0:96, :], in0=tmp['qs'][0:96, :], scalar1=col[0:96, ci:ci + 1],
                                                scalar2=phase, op0=ALU.mult, op1=ALU.add), ['hn_qs', 'c_col'], ['hn_sd'])
                    V(lambda e: e.tensor_scalar(out=ropei[0:96, :], in0=ang[0:96, :], scalar1=1.0 / TWO_PI, scalar2=None,
                                                op0=ALU.mult), ['hn_sd'], ['ropei'])
                    V(lambda e: e.tensor_copy(out=kf[0:96, :], in_=ropei[0:96, :]), ['ropei'], ['hn_qn'])
                    V(lambda e: e.scalar_tensor_tensor(out=r_[0:96, :], in0=kf[0:96, :], scalar=-C1, in1=ang[0:96, :],
                                                       op0=ALU.mult, op1=ALU.add), ['hn_qn', 'hn_sd'], ['hn_t2'])
                    V(lambda e: e.scalar_tensor_tensor(out=r_[0:96, :], in0=kf[0:96, :], scalar=-C2, in1=r_[0:96, :],
                                                       op0=ALU.mult, op1=ALU.add), ['hn_qn', 'hn_t2'], ['hn_t2'])
                    V(lambda e: e.tensor_scalar(out=kf[0:96, :], in0=r_[0:96, :], scalar1=math.pi, scalar2=-TWO_PI,
                                                op0=ALU.is_gt, op1=ALU.mult), ['hn_t2'], ['hn_qn'])
                    V(lambda e: e.tensor_tensor(out=r_[0:96, :], in0=r_[0:96, :], in1=kf[0:96, :], op=ALU.add),
                      ['hn_qn', 'hn_t2'], ['hn_t2'])
                    V(lambda e: e.tensor_scalar(out=kf[0:96, :], in0=r_[0:96, :], scalar1=-math.pi, scalar2=TWO_PI,
                                                op0=ALU.is_lt, op1=ALU.mult), ['hn_t2'], ['hn_qn'])
                    V(lambda e: e.tensor_tensor(out=r_[0:96, :], in0=r_[0:96, :], in1=kf[0:96, :], op=ALU.add),
                      ['hn_qn', 'hn_t2'], ['hn_t2'])
                    A(lambda e: e.activation(out=tab[0:96, cs_], in_=r_[0:96, :], func=AF.Sin), ['hn_t2'], ['ropeT'])
            wA = sb(es, "wA", [128, 8, 768], BF16)
            wkr = sb(es, "wkr", [128, 8, 96], BF16)
            wkrs = sb(es, "wkrs", [128, 8, 96], BF16)
            win_v = dr['w_in'][l].rearrange("(kc p) n -> p kc n", p=128)
            load_w_cast(wA[:], win_v[:, :, 0:768], 'wA')
            G(lambda e: e.memset(wkr[:, :, 0:64], 0.0), [], ['wkr'])
            G(lambda e: e.memset(wkrs[:, :, 0:64], 0.0), [], ['wkrs'])
            load_w_cast(wkr[:, :, 64:96], win_v[:, :, 768:800], 'wkr')
            load_w_cast(wkrs[:, :, 64:80], win_v[:, :, 784:800], 'wkrs')
            load_w_cast(wkrs[:, :, 80:96], win_v[:, :, 768:784], 'wkrs')
            QT = sb(es, "QT", [128, 4, S], BF16)
            KT = sb(es, "KT", [128, 4, S], BF16)
            KR = sb(es, "KR", [128, S], BF16)
            Vm = sb(es, "Vm", [128, 16, 256], BF16)
            wuq = sb(es, "wuq", [128, 4, 384], BF16)
            wuqs = sb(es, "wuqs", [128, 4, 384], BF16)
            wukv = sb(es, "wukv", [128, 2, 512], BF16)
            hc = [sb(es, f"hc{i}", [128, 8, 512], BF16) for i in range(2)]
            cqg = sb(es, "cqg", [128, 4, 512], BF16)
            ckvg = sb(es, "ckvg", [128, 2, 512], BF16)
            sqc = sb(es, "sqc", [128, 4, 512], BF16)
            sqk = sb(es, "sqk", [128, 2, 512], BF16)
            rs_cq = sb(es, "rs_cq", [128, 512], F32)
            rs_ckv = sb(es, "rs_ckv", [128, 512], F32)
            rs_col = sb(es, "rs_col", [128, 4], F32)
            pT = [sb(es, f"pT{i}", [128, 512], BF16) for i in range(2)]
            rden = sb(es, "rden", [128, 512], F32)
            uq_v = dr['w_uq'][l].rearrange("(kc p) n -> p kc n", p=128)
            ukv_v = dr['w_ukv'][l].rearrange("(kc p) n -> p kc n", p=128)
            for half in range(2):
                hh0 = half * 4
                load_w_cast(wuq[:], uq_v[:, :, hh0 * 96:(hh0 + 4) * 96], 'wuq')
                load_w_cast(wukv[:], ukv_v[:, :, hh0 * 128:(hh0 + 4) * 128], 'wukv')
                wq4 = wuq[:].rearrange("p k (h c) -> p k h c", c=96)
                ws4 = wuqs[:].rearrange("p k (h c) -> p k h c", c=96)
                G(lambda e: e.memset(wuqs[:], 0.0), [], ['wuqs'])
                for kc in range(4):
                    G(lambda e: e.tensor_copy(out=ws4[:, kc, :, 64:80], in_=wq4[:, kc, :, 80:96]), ['wuq'], ['wuqs'])
                    G(lambda e: e.tensor_copy(out=ws4[:, kc, :, 80:96], in_=wq4[:, kc, :, 64:80]), ['wuq'], ['wuqs'])
                for ch in range(4):
                    cs_ = slice(ch * 512, (ch + 1) * 512)
                    hcb = hc[ch % 2]
                    hct = f"hc{ch % 2}"
                    DMA('sp', hcb[:], hT_d[b][:, :, cs_], ['hT_d'], [hct])
                    for ft in range(6):
                        bk = nb(0, 6)
                        proj_fm(bk, [wA[:, kc, ft * 128:(ft + 1) * 128] for kc in range(8)], [hcb[:, kc, :] for kc in range(8)],
                                128, ['wA', hct])
                        pt = f"ps{bk}"
                        if ft < 4:
                            V(lambda e: e.tensor_scalar(out=cqg[:, ft, :], in0=ps[bk][:, :], scalar1=gcqT[:, ft:ft + 1],
                                                        scalar2=None, op0=ALU.mult), ['gcqT'], [pt, 'cqg'])
                            A(lambda e: e.activation(out=sqc[:, ft, :], in_=ps[bk][:, :], func=AF.Square), [], [pt, 'sqc'])
                        else:
                            f2 = ft - 4
                            V(lambda e: e.tensor_scalar(out=ckvg[:, f2, :], in0=ps[bk][:, :], scalar1=gckvT[:, f2:f2 + 1],
                                                        scalar2=None, op0=ALU.mult), ['gckvT'], [pt, 'ckvg'])
                            A(lambda e: e.activation(out=sqk[:, f2, :], in_=ps[bk][:, :], func=AF.Square), [], [pt, 'sqk'])
                    for (sq_, nf, rs_, rtok, sqtok, dim) in ((sqc, 4, rs_cq, 'rs_cq', 'sqc', 512), (sqk, 2, rs_ckv, 'rs_ckv', 'sqk', 256)):
                        bk = nb(6, 8)
                        pt = f"ps{bk}"
                        for ft in range(nf):
                            MM(ps[bk][:, :], ones_b[:, :], sq_[:, ft, :], ft == 0, ft == nf - 1, [sqtok, 'c_ones_b'], [pt])
                        A(lambda e: e.activation(out=rs_[:, :], in_=ps[bk][:, :], func=AF.Ln, scale=1.0 / dim, bias=col[:, 1:2]),
                          ['c_col'], [pt, rtok])
                        A(lambda e: e.activation(out=rs_[:, :], in_=rs_[:, :], func=AF.Exp, scale=-0.5), [rtok], [rtok])
                    bk = nb(6, 8)
                    pt = f"ps{bk}"
                    for tt in range(4):
                        for ft in range(2):
                            MM(ps[bk][:, tt:tt + 1], sqk[:, ft, tt * 128:(tt + 1) * 128], ones_b[:, 0:1], ft == 0, ft == 1,
                               ['sqk', 'c_ones_b'], [pt])
                    A(lambda e: e.activation(out=rs_col[:, :], in_=ps[bk][:, 0:4], func=AF.Ln, scale=1.0 / 256, bias=col[:, 1:2]),
                      ['c_col'], [pt, 'rs_col'])
                    A(lambda e: e.activation(out=rs_col[:, :], in_=rs_col[:, :], func=AF.Exp, scale=-0.5), ['rs_col'], ['rs_col'])
                    bkA, bkB = nb(0, 6), nb(0, 6)
                    proj_fm(bkA, [wkr[:, kc, :] for kc in range(8)], [hcb[:, kc, :] for kc in range(8)], 96, ['wkr', hct])
                    proj_fm(bkB, [wkrs[:, kc, :] for kc in range(8)], [hcb[:, kc, :] for kc in range(8)], 96, ['wkrs', hct])
                    headnorm(ps[bkA][0:96, :], 96, None, pcol[0:96, 2:3], KR[0:96, cs_], 'KR', tmp,
                             Psw=(ps[bkB][0:96, :], [f"ps{bkB}"]), gsw=pcol[0:96, 3:4], CC=CC[0:96, cs_], SS=SS[0:96, cs_],
                             P_tok=[f"ps{bkA}"])
                    for h in range(4):
                        bkA, bkB = nb(0, 6), nb(0, 6)
                        proj_fm(bkA, [wuq[:, kc, h * 96:(h + 1) * 96] for kc in range(4)], [cqg[:, kc, :] for kc in range(4)],
                                96, ['wuq', 'cqg'])
                        proj_fm(bkB, [wuqs[:, kc, h * 96:(h + 1) * 96] for kc in range(4)], [cqg[:, kc, :] for kc in range(4)],
                                96, ['wuqs', 'cqg'])
                        headnorm(ps[bkA][0:96, :], 96, rs_cq[0:96, :], pcol[0:96, 0:1], QT[0:96, h, cs_], 'QT', tmp,
                                 Psw=(ps[bkB][0:96, :], [f"ps{bkB}"]), gsw=pcol[0:96, 1:2], CC=CC[0:96, cs_], SS=SS[0:96, cs_],
                                 P_tok=[f"ps{bkA}"], extra_r=['rs_cq'])
                        bkA = nb(0, 6)
                        proj_fm(bkA, [wukv[:, kc, h * 128:h * 128 + 64] for kc in range(2)], [ckvg[:, kc, :] for kc in range(2)],
                                64, ['wukv', 'ckvg'])
                        headnorm(ps[bkA][0:64, :], 64, rs_ckv[0:64, :], pcol[0:64, 2:3], KT[0:64, h, cs_], 'KT', tmp,
                                 P_tok=[f"ps{bkA}"], extra_r=['rs_ckv'])
                        G(lambda e: e.tensor_copy(out=KT[64:96, h, cs_], in_=KR[64:96, cs_]), ['KR'], ['KT'])
                    wv4 = wukv[:].rearrange("p k (h c) -> p k h c", c=128)
                    for tt in range(4):
                        bk = nb(0, 6)
                        pt = f"ps{bk}"
                        for kc in range(2):
                            MM(ps[bk][:, 0:256].rearrange("p (h c) -> p h c", c=64), ckvg[:, kc, tt * 128:(tt + 1) * 128],
                               wv4[:, kc, :, 64:128], kc == 0, kc == 1, ['ckvg', 'wukv'], [pt])
                        V(lambda e: e.tensor_scalar(out=Vm[:, ch * 4 + tt, :], in0=ps[bk][:, 0:256], scalar1=rs_col[:, tt:tt + 1],
                                                    scalar2=None, op0=ALU.mult), ['rs_col'], [pt, 'Vm'])
                it = 0
                for h in range(4):
                    hg = hh0 + h
                    prow = (hg % 2) * 64
                    for qc in range(4):
                        bo, bd = 2 + it % 2, 4 + it % 2
                        it += 1
                        for kt in range(16):
                            bs = kt % 2
                            MM(ps[bs][:, :], KT[0:96, h, kt * 128:(kt + 1) * 128], QT[0:96, h, qc * 512:(qc + 1) * 512], True, True,
                               ['KT', 'QT'], [f"ps{bs}"])
                            A(lambda e: e.activation(out=pT[bs][:, :], in_=ps[bs][:, :], func=AF.Exp, scale=MLA_SCALE),
                              [], [f"ps{bs}", f"pT{bs}"])
                            MM(ps[bo][prow:prow + 64, :], Vm[:, kt, h * 64:(h + 1) * 64], pT[bs][:, :], kt == 0, kt == 15,
                               ['Vm', f"pT{bs}"], [f"ps{bo}"])
                            MM(ps[bd][prow:prow + 64, :], ones_b[:, 0:64], pT[bs][:, :], kt == 0, kt == 15,
                               ['c_ones_b', f"pT{bs}"], [f"ps{bd}"])
                        A(lambda e: e.activation(out=rden[prow:prow + 64, :], in_=ps[bd][prow:prow + 64, :], func=AF.Ln),
                          [], [f"ps{bd}", 'rden'])
                        A(lambda e: e.activation(out=rden[prow:prow + 64, :], in_=rden[prow:prow + 64, :], func=AF.Exp, scale=-1.0),
                          ['rden'], ['rden'])
                        V(lambda e: e.tensor_tensor(out=oT_mla[prow:prow + 64, hg // 2, qc * 512:(qc + 1) * 512],
                                                    in0=ps[bo][prow:prow + 64, :], in1=rden[prow:prow + 64, :], op=ALU.mult),
                          ['rden'], [f"ps{bo}", 'oT_mla'])
            fw.barrier()

    def band_group(l, b, es, tmp, wv_cols, n_k, gq_col, gk_col, R, mask_base, slopes, epilogue, posq, hcbufs, tag):
        with ExitStack() as gs:
            wG = sb(gs, "wG", [128, 8, 256 + 2 * 64 * n_k], BF16)
            QT = sb(gs, "QTb", [64, 4, S], BF16)
            KT = sb(gs, "KTb", [64, n_k, S], BF16)
            Vt = sb(gs, "Vtb", [128, 16, n_k, 65], BF16)
            tS = [sb(gs, f"tS{i}", [128, 4, 128], F32) for i in range(2)]
            pB = [sb(gs, f"pB{i}", [128, 4, 128], BF16) for i in range(2)]
            Dt = [sb(gs, f"Dt{i}", [128, 128], F32) for i in range(2)]
            win_v = dr['w_in'][l].rearrange("(kc p) n -> p kc n", p=128)
            q0, k0, v0 = wv_cols
            nkc = 64 * n_k
            load_w_cast(wG[:, :, 0:256], win_v[:, :, q0:q0 + 256], 'wG')
            load_w_cast(wG[:, :, 256:256 + nkc], win_v[:, :, k0:k0 + nkc], 'wG')
            load_w_cast(wG[:, :, 256 + nkc:256 + 2 * nkc], win_v[:, :, v0:v0 + nkc], 'wG')
            G(lambda e: e.memset(Vt[:, :, :, 64:65], 1.0), [], ['Vtb'])
            for ch in range(4):
                cs_ = slice(ch * 512, (ch + 1) * 512)
                hcb = hcbufs[ch % 2]
                hct = f"hc{ch % 2}"
                DMA('sp', hcb[:], hT_d[b][:, :, cs_], ['hT_d'], [hct])
                for hh in range(4):
                    bk = nb(0, 4)
                    proj_fm(bk, [wG[:, kc, hh * 64:(hh + 1) * 64] for kc in range(8)], [hcb[:, kc, :] for kc in range(8)],
                            64, ['wG', hct])
                    headnorm(ps[bk][0:64, :], 64, None, gq_col, QT[0:64, hh, cs_], 'QTb', tmp, P_tok=[f"ps{bk}"])
                for hk in range(n_k):
                    bk = nb(0, 4)
                    proj_fm(bk, [wG[:, kc, 256 + hk * 64:256 + (hk + 1) * 64] for kc in range(8)],
                            [hcb[:, kc, :] for kc in range(8)], 64, ['wG', hct])
                    headnorm(ps[bk][0:64, :], 64, None, gk_col, KT[0:64, hk, cs_], 'KTb', tmp, P_tok=[f"ps{bk}"])
                for tt in range(4):
                    bk = nb(0, 4)
                    pt = f"ps{bk}"
                    for kc in range(8):
                        MM(ps[bk][:, 0:nkc], hcb[:, kc, tt * 128:(tt + 1) * 128], wG[:, kc, 256 + nkc:256 + 2 * nkc],
                           kc == 0, kc == 7, ['wG', hct], [pt])
                    V(lambda e: e.tensor_copy(out=Vt[:, ch * 4 + tt, :, 0:64],
                                              in_=ps[bk][:, 0:nkc].rearrange("p (h c) -> p h c", c=64)), [], [pt, 'Vtb'])
            it = 0
            for blk in range(16):
                kts = list(range(max(0, blk - R), min(15, blk + R) + 1))
                for j, kt in enumerate(kts):
                    dl = kt - blk
                    i2 = it % 2
                    it += 1
                    d_ = Dt[i2]
                    dtok = f"Dt{i2}"
                    V(lambda e: e.scalar_tensor_tensor(out=d_[:, :], in0=posq[:, blk * 128:(blk + 1) * 128],
                                                       scalar=poskT[:, b, kt:kt + 1], in1=masks[:, mask_base + dl + R, :],
                                                       op0=ALU.subtract, op1=ALU.add), ['posq', 'poskT', 'c_masks'], [dtok])
                    V(lambda e: e.scalar_tensor_tensor(out=d_[:, :], in0=d_[:, :], scalar=-1.0, in1=d_[:, :],
                                                       op0=ALU.mult, op1=ALU.max), [dtok], [dtok])
                    bs = i2
                    for hh in range(4):
                        hk = hh if n_k == 4 else 0
                        MM(ps[bs][:, hh * 128:(hh + 1) * 128], KT[0:64, hk, kt * 128:(kt + 1) * 128],
                           QT[0:64, hh, blk * 128:(blk + 1) * 128], True, True, ['KTb', 'QTb'], [f"ps{bs}"])
                    for hh in range(4):
                        V(lambda e: e.scalar_tensor_tensor(out=tS[i2][:, hh, :], in0=d_[:, :], scalar=-slopes[hh] / HD_SCALE,
                                                           in1=ps[bs][:, hh * 128:(hh + 1) * 128], op0=ALU.mult, op1=ALU.add),
                          [dtok], [f"ps{bs}", f"tS{i2}"])
                    A(lambda e: e.activation(out=pB[i2][:], in_=tS[i2][:], func=AF.Exp, scale=HD_SCALE),
                      [f"tS{i2}"], [f"pB{i2}"])
                    for hh in range(4):
                        hk = hh if n_k == 4 else 0
                        MM(ps[4 + hh][:, 0:65], pB[i2][:, hh, :], Vt[:, kt, hk, :], j == 0, j == len(kts) - 1,
                           [f"pB{i2}", 'Vtb'], [f"ps{4 + hh}"])
                epilogue(blk)
            fw.barrier()

    def stage_band(l, b, oT_dil, oT_win):
        sl12 = alibi(12)
        sl8 = alibi(8)
        with ExitStack() as es:
            tmp = mk_hn_tmp(es)
            posq = sb(es, "posq", [128, S], F32)
            posq_i = sb(es, "posq_i", [128, S], I32)
            hcbufs = [sb(es, f"hc{i}", [128, 8, 512], BF16) for i in range(2)]
            acc = sb(es, "acc", [128, 16, 4, 65], F32)
            osb = sb(es, "osb", [128, 4, 64], BF16)
            rdn = sb(es, "rdn", [128, 4], F32)
            DMA('sp', posq_i[:], dr['pos'][b].partition_broadcast(128), [], ['posq_i'])
            V(lambda e: e.tensor_copy(out=posq[:], in_=posq_i[:]), ['posq_i'], ['posq'])

            def transpose_out(blk, oT, ft0):
                for i in range(2):
                    bk = nb(0, 4)
                    pt = f"ps{bk}"
                    pbf = ps[bk][:].bitcast(BF16)
                    PE(lambda e: e.transpose(pbf[:, 0:128], osb[:, 2 * i:2 * i + 2, :].rearrange("p h c -> p (h c)"), ident_b[:]),
                       ['osb', 'c_ident_b'], [pt])
                    V(lambda e: e.tensor_copy(out=oT[:, ft0 + i, blk * 128:(blk + 1) * 128], in_=pbf[:, 0:128]),
                      [], [pt, 'oT_band'])

            for g in range(3):
                def epi_dil(blk, g=g):
                    for hh in range(4):
                        if g == 0:
                            V(lambda e: e.tensor_copy(out=acc[:, blk, hh, :], in_=ps[4 + hh][:, 0:65]), [], [f"ps{4 + hh}", 'acc'])
                        else:
                            V(lambda e: e.tensor_tensor(out=acc[:, blk, hh, :], in0=acc[:, blk, hh, :], in1=ps[4 + hh][:, 0:65],
                                                        op=ALU.add), [], [f"ps{4 + hh}", 'acc'])
                    if g == 2:
                        V(lambda e: e.reciprocal(out=rdn[:, :], in_=acc[:, blk, :, 64]), ['acc'], ['rdn'])
                        V(lambda e: e.tensor_tensor(out=osb[:], in0=acc[:, blk, :, 0:64],
                                                    in1=rdn[:, :].unsqueeze(2).to_broadcast([128, 4, 64]), op=ALU.mult),
                          ['acc', 'rdn'], ['osb'])
                        transpose_out(blk, oT_dil, 0)
                q0 = OFF_DIL + g * 256
                band_group(l, b, es, tmp, (q0, q0 + 768, q0 + 1536), 4, pcol[0:64, 4:5], pcol[0:64, 5:6], DIL_R[g],
                           MASK_BASE[g], sl12[g * 4:(g + 1) * 4], epi_dil, posq, hcbufs, f"d{g}")
            for kvg in range(2):
                def epi_win(blk, kvg=kvg):
                    for hh in range(4):
                        V(lambda e: e.tensor_tensor(out=rdn[:, hh:hh + 1], in0=ps[4 + hh][:, 64:65],
                                                    in1=esink[:, kvg * 4 + hh:kvg * 4 + hh + 1], op=ALU.add),
                          ['esink'], [f"ps{4 + hh}", 'rdn'])
                    V(lambda e: e.reciprocal(out=rdn[:, :], in_=rdn[:, :]), ['rdn'], ['rdn'])
                    for hh in range(4):
                        V(lambda e: e.tensor_scalar(out=osb[:, hh, :], in0=ps[4 + hh][:, 0:64], scalar1=rdn[:, hh:hh + 1],
                                                    scalar2=None, op0=ALU.mult), ['rdn'], [f"ps{4 + hh}", 'osb'])
                    transpose_out(blk, oT_win, kvg * 2)
                band_group(l, b, es, tmp, (OFF_WIN + kvg * 256, OFF_WIN + 512 + kvg * 64, OFF_WIN + 640 + kvg * 64), 1,
                           pcol[0:64, 6:7], pcol[0:64, 7:8], 1, MASK_BASE[3], sl8[kvg * 4:(kvg + 1) * 4], epi_win,
                           posq, hcbufs, f"w{kvg}")
            fw.barrier()

    def stage_merge(l, b, oT_mla, oT_dil, oT_win):
        with ExitStack() as es:
            wg = [sb(es, f"wg{i}", [128, 8, D], BF16) for i in range(2)]
            wbr_m = sb(es, "wbr_m", [128, 4, D], BF16)
            wbr_d = sb(es, "wbr_d", [128, 2, D], BF16)
            wbr_w = sb(es, "wbr_w", [128, 4, D], BF16)
            wout = sb(es, "wout", [128, 8, D], BF16)
            hcb = sb(es, "hcm", [128, 8, 512], BF16)
            mgT = sb(es, "mgT", [128, 8, 512], BF16)
            mg = sb(es, "mg", [128, 8, 512], F32)
            sig = [sb(es, f"sig{i}", [128, 512], F32) for i in range(2)]
            tmpm = sb(es, "tmpm", [128, 512], F32)
            xt = [sb(es, f"xm{i}", [128, D], F32) for i in range(2)]
            win_v = dr['w_in'][l].rearrange("(kc p) n -> p kc n", p=128)
            load_w_cast(wbr_m[:], dr['w_br_mla'][l].rearrange("(kc p) n -> p kc n", p=128), 'wbr_m')
            load_w_cast(wbr_d[:], dr['w_br_dil'][l].rearrange("(kc p) n -> p kc n", p=128), 'wbr_d')
            load_w_cast(wbr_w[:], dr['w_br_win'][l].rearrange("(kc p) n -> p kc n", p=128), 'wbr_w')
            load_w_cast(wout[:], dr['w_out'][l].rearrange("(kc p) n -> p kc n", p=128), 'wout')
            srcs = ((oT_mla, 4, wbr_m, 'wbr_m', 'oT_mla'), (oT_dil, 2, wbr_d, 'wbr_d', 'oT_band'),
                    (oT_win, 4, wbr_w, 'wbr_w', 'oT_band'))
            it = 0
            for ch in range(4):
                cs_ = slice(ch * 512, (ch + 1) * 512)
                DMA('sp', hcb[:], hT_d[b][:, :, cs_], ['hT_d'], ['hcm'])
                for m in range(3):
                    wgb = wg[(ch * 3 + m) % 2]
                    wgt = f"wg{(ch * 3 + m) % 2}"
                    load_w_cast(wgb[:], win_v[:, :, OFF_GATE + m * D:OFF_GATE + (m + 1) * D], wgt)
                    oT, nk, wbr, wbrt, otok = srcs[m]
                    for ft in range(8):
                        i2 = it % 2
                        it += 1
                        bg, by = i2, 2 + i2
                        for kc in range(8):
                            MM(ps[bg][:, :], wgb[:, kc, ft * 128:(ft + 1) * 128], hcb[:, kc, :], kc == 0, kc == 7,
                               [wgt, 'hcm'], [f"ps{bg}"])
                        A(lambda e: e.activation(out=sig[i2][:], in_=ps[bg][:, :], func=AF.Sigmoid), [], [f"ps{bg}", f"sig{i2}"])
                        for k in range(nk):
                            MM(ps[by][:, :], wbr[:, k, ft * 128:(ft + 1) * 128], oT[:, k, cs_], k == 0, k == nk - 1,
                               [wbrt, otok], [f"ps{by}"])
                        if m == 0:
                            V(lambda e: e.tensor_tensor(out=mg[:, ft, :], in0=ps[by][:, :], in1=sig[i2][:], op=ALU.mult),
                              [f"sig{i2}"], [f"ps{by}", 'mg'])
                        else:
                            V(lambda e: e.tensor_tensor(out=tmpm[:], in0=ps[by][:, :], in1=sig[i2][:], op=ALU.mult),
                              [f"sig{i2}"], [f"ps{by}", 'tmpm'])
                            if m == 1:
                                G(lambda e: e.tensor_tensor(out=mg[:, ft, :], in0=mg[:, ft, :], in1=tmpm[:], op=ALU.add),
                                  ['tmpm', 'mg'], ['mg'])
                            else:
                                G(lambda e: e.tensor_tensor(out=mgT[:, ft, :], in0=mg[:, ft, :], in1=tmpm[:], op=ALU.add),
                                  ['tmpm', 'mg'], ['mgT'])
                for tt in range(4):
                    i2 = tt % 2
                    r0 = b * S + ch * 512 + tt * 128
                    DMA('sp', xt[i2][:], xsrc[0][r0:r0 + 128, :], ['xres'], [f"xm{i2}"])
                    for nh in range(2):
                        bk = 4 + (tt * 2 + nh) % 4
                        for ft in range(8):
                            MM(ps[bk][:, :], mgT[:, ft, tt * 128:(tt + 1) * 128], wout[:, ft, nh * 512:(nh + 1) * 512],
                               ft == 0, ft == 7, ['mgT', 'wout'], [f"ps{bk}"])
                        V(lambda e: e.tensor_tensor(out=tmpm[:], in0=ps[bk][:, :], in1=gt1b[:, b, nh * 512:(nh + 1) * 512],
                                                    op=ALU.mult), ['gt1b'], [f"ps{bk}", 'tmpm'])
                        G(lambda e: e.tensor_tensor(out=xt[i2][:, nh * 512:(nh + 1) * 512], in0=xt[i2][:, nh * 512:(nh + 1) * 512],
                                                    in1=tmpm[:], op=ALU.add), ['tmpm', f"xm{i2}"], [f"xm{i2}"])
                    DMA('sp', out[r0:r0 + 128, :], xt[i2][:], [f"xm{i2}"], ['xout'])
            fw.barrier()

    def stage_moe(l):
        w1v = dr['w1'].rearrange("l e (p k) n -> (l e p k) n", k=8)
        w3v = dr['w3'].rearrange("l e (p k) n -> (l e p k) n", k=8)
        w2v = dr['w2'].rearrange("l e (p c) n -> (l e p c) n", c=3)
        with ExitStack() as es:
            gates = sb(es, "gates", [128, NT, 2], F32)
            d1i = sb(es, "d1i", [128, NT], I32)
            d2i = sb(es, "d2i", [128, NT], I32)
            widx = sb(es, "widx", [128, NBLK], I32)
            widx4 = sb(es, "widx8", [128, 8, NBLK], I32)
            widx3 = sb(es, "widx3", [128, 3, NBLK], I32)
            gt2b = sb(es, "gt2b", [128, NSEQ, D], F32)
            DMA('sp', gt2b[:], bc_d[2].rearrange("b p n -> p b n"), ['bc_d'], ['gt2b'])
            with ExitStack() as e1:
                h2bf = sb(e1, "h2bf", [128, NT, D], BF16)
                A2b = sb(e1, "A2b", [128, NSEQ, D], F32)
                B2b = sb(e1, "B2b", [128, NSEQ, D], F32)
                DMA('sp', A2b[:], bc_d[1].rearrange("b p n -> p b n"), ['bc_d'], ['A2b'])
                DMA('sp', B2b[:], bc_d[0].rearrange("b p n -> p b n"), ['bc_d'], ['B2b'])
                wr = sb(e1, "wr", [128, 8, 36], F32)
                brow = sb(e1, "browr", [1, 36], F32)
                DMA('sp', wr[:], dr['w_r'][l].rearrange("(kc p) n -> p kc n", p=128), [], ['wr'])
                DMA('sp', brow[:], dr['b_r'][l], [], ['browr'])
                M1a = sb(e1, "M1a", [128, NT, 32], F32)
                M2a = sb(e1, "M2a", [128, NT, 32], F32)
                xt = [sb(e1, f"xe{i}", [128, D], F32) for i in range(2)]
                h2 = [sb(e1, f"h2{i}", [128, D], F32) for i in range(2)]
                h2T = [sb(e1, f"h2T{i}", [128, 8, 128], F32) for i in range(2)]
                junk = sb(e1, "junkE", [128, D], BF16)
                st = [sb(e1, f"stE{i}", [128, 4], F32) for i in range(2)]
                rt = sb(e1, "rt", [128, 64], F32)
                r2 = sb(e1, "r2", [128, 64], F32)
                for t in range(NT):
                    i = t % 2
                    b = t // 16
                    r0 = t * 128
                    DMA('sp', xt[i][:], out[r0:r0 + 128, :], ['xout'], [f'xe{i}'])
                    A(lambda e: e.activation(out=junk[:], in_=xt[i][:], func=AF.Square, accum_out=st[i][:, 0:1]),
                      [f'xe{i}'], ['junkE', f'stE{i}'])
                    A(lambda e: e.activation(out=st[i][:, 1:2], in_=st[i][:, 0:1], func=AF.Ln, scale=1.0 / D, bias=col[:, 1:2]),
                      [f'stE{i}', 'c_col'], [f'stE{i}'])
                    A(lambda e: e.activation(out=st[i][:, 2:3], in_=st[i][:, 1:2], func=AF.Exp, scale=-0.5),
                      [f'stE{i}'], [f'stE{i}'])
                    V(lambda e: e.scalar_tensor_tensor(out=h2[i][:], in0=xt[i][:], scalar=st[i][:, 2:3], in1=A2b[:, b, :],
                                                       op0=ALU.mult, op1=ALU.mult), [f'xe{i}', f'stE{i}', 'A2b'], [f'h2{i}'])
                    G(lambda e: e.tensor_tensor(out=h2[i][:], in0=h2[i][:], in1=B2b[:, b, :], op=ALU.add), [f'h2{i}', 'B2b'], [f'h2{i}'])
                    A(lambda e: e.copy(out=h2bf[:, t, :], in_=h2[i][:]), [f'h2{i}'], ['h2bf'])
                    for half in range(2):
                        bk = nb(0, 4)
                        pt = f"ps{bk}"
                        for q4 in range(4):
                            kc = half * 4 + q4
                            PE(lambda e: e.transpose(ps[bk][:, q4 * 128:(q4 + 1) * 128], h2[i][:, kc * 128:(kc + 1) * 128], ident_f[:]),
                               [f'h2{i}', 'c_ident_f'], [pt])
                        V(lambda e: e.tensor_copy(out=h2T[i][:, half * 4:half * 4 + 4, :],
                                                  in_=ps[bk][:, :].rearrange("p (q c) -> p q c", c=128)), [], [pt, f'h2T{i}'])
                    bk = nb(4, 8)
                    pt = f"ps{bk}"
                    for kc in range(8):
                        MM(ps[bk][:, 0:36], h2T[i][:, kc, :], wr[:, kc, :], kc == 0, False, [f'h2T{i}', 'wr'], [pt])
                    MM(ps[bk][:, 0:36], ones_f[0:1, 0:128], brow[0:1, :], False, True, ['ones_f', 'browr'], [pt])
                    lg = rt[:, 0:36]
                    gmax, ngmax, gsum, gw = rt[:, 36:37], rt[:, 37:38], rt[:, 38:39], rt[:, 39:40]
                    ohg, ge = rt[:, 40:44], rt[:, 44:48]
                    m1, m2, dd, ed, rr = rt[:, 48:49], rt[:, 49:50], rt[:, 50:51], rt[:, 51:52], rt[:, 52:53]
                    sel, mask1, sel2, mask2, t48 = r2[:, 0:8], r2[:, 8:16], r2[:, 16:24], r2[:, 24:32], r2[:, 32:64]
                    t48v = t48.rearrange("p (g e) -> p g e", e=8)
                    RT = ['rt']
                    V(lambda e: e.tensor_copy(out=lg, in_=ps[bk][:, 0:36]), [], [pt, 'rt'])
                    V(lambda e: e.tensor_reduce(out=gmax, in_=rt[:, 0:4], axis=AX.X, op=ALU.max), RT, RT)
                    V(lambda e: e.tensor_scalar(out=ohg, in0=rt[:, 0:4], scalar1=gmax, scalar2=None, op0=ALU.is_equal), RT, RT)
                    V(lambda e: e.tensor_scalar(out=ngmax, in0=gmax, scalar1=-1.0, scalar2=None, op0=ALU.mult), RT, RT)
                    A(lambda e: e.activation(out=ge, in_=rt[:, 0:4], func=AF.Exp, bias=ngmax, scale=1.0, accum_out=gsum), RT, RT)
                    V(lambda e: e.reciprocal(out=gw, in_=gsum), RT, RT)
                    V(lambda e: e.tensor_tensor(out=t48v, in0=rt[:, 4:36].rearrange("p (g e) -> p g e", e=8),
                                                in1=ohg.unsqueeze(2).to_broadcast([128, 4, 8]), op=ALU.mult), RT, RT)
                    V(lambda e: e.tensor_reduce(out=sel, in_=t48v.rearrange("p g e -> p e g"), axis=AX.X, op=ALU.add), RT, RT)
                    V(lambda e: e.tensor_reduce(out=m1, in_=sel, axis=AX.X, op=ALU.max), RT, RT)
                    V(lambda e: e.tensor_scalar(out=mask1, in0=sel, scalar1=m1, scalar2=None, op0=ALU.is_equal), RT, RT)
                    V(lambda e: e.scalar_tensor_tensor(out=sel2, in0=mask1, scalar=-1.0e30, in1=sel, op0=ALU.mult, op1=ALU.add), RT, RT)
                    V(lambda e: e.tensor_reduce(out=m2, in_=sel2, axis=AX.X, op=ALU.max), RT, RT)
                    V(lambda e: e.tensor_scalar(out=mask2, in0=sel2, scalar1=m2, scalar2=None, op0=ALU.is_equal), RT, RT)
                    V(lambda e: e.tensor_tensor(out=dd, in0=m2, in1=m1, op=ALU.subtract), RT, RT)
                    A(lambda e: e.activation(out=ed, in_=dd, func=AF.Exp), RT, RT)
                    V(lambda e: e.tensor_scalar(out=rr, in0=ed, scalar1=1.0, scalar2=None, op0=ALU.add), RT, RT)
                    V(lambda e: e.reciprocal(out=rr, in_=rr), RT, RT)
                    V(lambda e: e.tensor_tensor(out=gates[:, t, 0:1], in0=gw, in1=rr, op=ALU.mult), RT, ['gates'])
                    V(lambda e: e.tensor_tensor(out=gates[:, t, 1:2], in0=gw, in1=gates[:, t, 0:1], op=ALU.subtract),
                      RT + ['gates'], ['gates'])
                    V(lambda e: e.tensor_tensor(out=M1a[:, t, :].rearrange("p (g e) -> p g e", e=8),
                                                in0=ohg.unsqueeze(2).to_broadcast([128, 4, 8]),
                                                in1=mask1.unsqueeze(1).to_broadcast([128, 4, 8]), op=ALU.mult), RT, ['M1a'])
                    V(lambda e: e.tensor_tensor(out=M2a[:, t, :].rearrange("p (g e) -> p g e", e=8),
                                                in0=ohg.unsqueeze(2).to_broadcast([128, 4, 8]),
                                                in1=mask2.unsqueeze(1).to_broadcast([128, 4, 8]), op=ALU.mult), RT, ['M2a'])
                if stop_after in (('e1tiles', l), ('moe_e1tiles', l)):
                    dsb = sb(e1, "dsb", [128, 512], F32)
                    V(lambda e: e.memset(dsb[:], 0.0), [], ['dsb'])
                    V(lambda e: e.tensor_copy(out=dsb[:, 160:224], in_=gates[:].rearrange("p t k -> p (t k)")), ['gates'], ['dsb'])
                    V(lambda e: e.tensor_copy(out=dsb[:, 288:320], in_=M1a[:, 0, :]), ['M1a'], ['dsb'])
                    V(lambda e: e.tensor_copy(out=dsb[:, 320:352], in_=M2a[:, 0, :]), ['M2a'], ['dsb'])
                    V(lambda e: e.tensor_copy(out=dsb[:, 352:405], in_=rt[:, 0:53]), ['rt'], ['dsb'])
                    DMA('sp', dbg, dsb[:], ['dsb'], ['dbg'])
                    fw.barrier()
                    return
                Mt_bf = sb(e1, "Mt_bf", [128, NT * 32], BF16)
                CSs = sb(e1, "CSs", [128, NT, 32], F32)
                pref = sb(e1, "pref", [128, NT, 32], F32)
                destf = sb(e1, "destf", [128, NT, 32], F32)
                tmpd = sb(e1, "tmpd", [128, NT, 32], F32)
                cmp2 = sb(e1, "cmp2", [128, NBLK, 32], F32)
                sc = sb(e1, "scn", [128, 8, 32], F32)
                dkf = sb(e1, "dkf", [128, 2, NT], F32)
                bef = sb(e1, "bef", [128, NBLK], F32)
                SL = ['slot']
                V(lambda e: e.tensor_tensor(out=Mt_bf[:], in0=M1a[:].rearrange("p t e -> p (t e)"),
                                            in1=M2a[:].rearrange("p t e -> p (t e)"), op=ALU.add), ['M1a', 'M2a'], ['Mt_bf'])
                for hf in range(2):
                    MM(ps[hf][:, :], ustrict[:, :], Mt_bf[:, hf * 512:(hf + 1) * 512], True, True, ['Mt_bf', 'c_ustrict_b'], [f"ps{hf}"])
                    MM(ps[2 + hf][:, :], ones_b[:, :], Mt_bf[:, hf * 512:(hf + 1) * 512], True, True, ['Mt_bf', 'c_ones_b'], [f"ps{2 + hf}"])
                    V(lambda e: e.tensor_copy(out=CSs[:, hf * 16:(hf + 1) * 16, :].rearrange("p t e -> p (t e)"), in_=ps[2 + hf][:, :]),
                      [], [f"ps{2 + hf}"] + SL)
                V(lambda e: e.memset(pref[:, 0, :], 0.0), SL, SL)
                for t in range(1, NT):
                    V(lambda e: e.tensor_tensor(out=pref[:, t, :], in0=pref[:, t - 1, :], in1=CSs[:, t - 1, :], op=ALU.add), SL, SL)
                cnts, nblk_, padd, incA, incB, startp = sc[:, 0, :], sc[:, 1, :], sc[:, 2, :], sc[:, 3, :], sc[:, 4, :], sc[:, 5, :]
                V(lambda e: e.tensor_tensor(out=cnts, in0=pref[:, NT - 1, :], in1=CSs[:, NT - 1, :], op=ALU.add), SL, SL)
                c3 = cmp2[:, 0:32, :]
                V(lambda e: e.tensor_tensor(out=c3, in0=cnts.unsqueeze(2).to_broadcast([128, 32, 32]),
                                            in1=bstart[:, 0:32].unsqueeze(1).to_broadcast([128, 32, 32]), op=ALU.is_gt),
                  SL + ['c_bstart'], SL)
                V(lambda e: e.tensor_reduce(out=nblk_, in_=c3, axis=AX.X, op=ALU.add), SL, SL)
                V(lambda e: e.tensor_scalar(out=padd, in0=nblk_, scalar1=128.0, scalar2=None, op0=ALU.mult), SL, SL)
                V(lambda e: e.tensor_copy(out=incA, in_=padd), SL, SL)
                cur, nxt = incA, incB
                for s_ in (1, 2, 4, 8, 16):
                    V(lambda e: e.tensor_copy(out=nxt[:, 0:s_], in_=cur[:, 0:s_]), SL, SL)
                    V(lambda e: e.tensor_tensor(out=nxt[:, s_:32], in0=cur[:, s_:32], in1=cur[:, 0:32 - s_], op=ALU.add), SL, SL)
                    cur, nxt = nxt, cur
                endp = cur
                V(lambda e: e.tensor_tensor(out=startp, in0=endp, in1=padd, op=ALU.subtract), SL, SL)
                V(lambda e: e.tensor_tensor(out=tmpd[:], in0=pref[:], in1=startp.unsqueeze(1).to_broadcast([128, NT, 32]),
                                            op=ALU.add), SL, SL)
                for hf in range(2):
                    V(lambda e: e.tensor_tensor(out=destf[:, hf * 16:(hf + 1) * 16, :].rearrange("p t e -> p (t e)"),
                                                in0=tmpd[:, hf * 16:(hf + 1) * 16, :].rearrange("p t e -> p (t e)"),
                                                in1=ps[hf][:, :], op=ALU.add), SL, [f"ps{hf}"] + SL)
                for k_, (Ma, Mtok, dki) in enumerate(((M1a, 'M1a', d1i), (M2a, 'M2a', d2i))):
                    V(lambda e: e.tensor_tensor(out=tmpd[:], in0=destf[:], in1=Ma[:], op=ALU.mult), SL + [Mtok], SL)
                    V(lambda e: e.tensor_reduce(out=dkf[:, k_, :], in_=tmpd[:], axis=AX.X, op=ALU.add), SL, SL)
                    V(lambda e: e.tensor_copy(out=dki[:], in_=dkf[:, k_, :]), SL, ['dki'])
                V(lambda e: e.tensor_tensor(out=cmp2[:], in0=endp.unsqueeze(1).to_broadcast([128, NBLK, 32]),
                                            in1=bstart[:, :].unsqueeze(2).to_broadcast([128, NBLK, 32]), op=ALU.is_le),
                  SL + ['c_bstart'], SL)
                V(lambda e: e.tensor_reduce(out=bef[:], in_=cmp2[:], axis=AX.X, op=ALU.add), SL, SL)
                V(lambda e: e.tensor_scalar(out=bef[:], in0=bef[:], scalar1=31.0, scalar2=128.0, op0=ALU.min, op1=ALU.mult), SL, SL)
                V(lambda e: e.tensor_scalar(out=bef[:], in0=bef[:], scalar1=col[:, 5:6], scalar2=float(l * NE * 128),
                                            op0=ALU.add, op1=ALU.add), SL + ['c_col'], SL)
                V(lambda e: e.tensor_copy(out=widx[:], in_=bef[:]), SL, ['widx'])
                for q_ in range(8):
                    V(lambda e: e.tensor_scalar(out=widx4[:, q_, :], in0=bef[:], scalar1=8.0, scalar2=float(q_),
                                                op0=ALU.mult, op1=ALU.add), SL, ['widx'])
                for c_ in range(3):
                    V(lambda e: e.tensor_scalar(out=widx3[:, c_, :], in0=bef[:], scalar1=3.0, scalar2=float(c_),
                                                op0=ALU.mult, op1=ALU.add), SL, ['widx'])
                if stop_after in (('slots', l), ('moe_slots', l)):
                    dsb = sb(e1, "dsb", [128, 512], F32)
                    V(lambda e: e.memset(dsb[:], 0.0), [], ['dsb'])
                    V(lambda e: e.tensor_copy(out=dsb[:, 0:32], in_=d1i[:]), ['dki'], ['dsb'])
                    V(lambda e: e.tensor_copy(out=dsb[:, 32:64], in_=d2i[:]), ['dki'], ['dsb'])
                    V(lambda e: e.tensor_copy(out=dsb[:, 64:160], in_=widx[:]), ['widx'], ['dsb'])
                    V(lambda e: e.tensor_copy(out=dsb[:, 160:224], in_=gates[:].rearrange("p t k -> p (t k)")), ['gates'], ['dsb'])
                    V(lambda e: e.tensor_copy(out=dsb[:, 224:256], in_=cnts), SL, ['dsb'])
                    V(lambda e: e.tensor_copy(out=dsb[:, 256:288], in_=startp), SL, ['dsb'])
                    V(lambda e: e.tensor_copy(out=dsb[:, 288:320], in_=M1a[:, 0, :]), ['M1a'], ['dsb'])
                    V(lambda e: e.tensor_copy(out=dsb[:, 320:352], in_=M2a[:, 0, :]), ['M2a'], ['dsb'])
                    V(lambda e: e.tensor_copy(out=dsb[:, 352:405], in_=rt[:, 0:53]), ['rt'], ['dsb'])
                    DMA('sp', dbg, dsb[:], ['dsb'], ['dbg'])
                    fw.barrier()
                    return
                for t in range(NT):
                    for dki in (d1i, d2i):
                        fw.throttle('pool', 'd_rows_d', 2)
                        fw.dma('pool', lambda e: e.indirect_dma_start(
                            out=rows_d, out_offset=bass.IndirectOffsetOnAxis(ap=dki[:, t:t + 1], axis=0),
                            in_=h2bf[:, t, :], in_offset=None), ['h2bf', 'dki'], ['rows_d'])
                fw.barrier()
            with ExitStack() as e2:
                Xb = [sb(e2, f"Xb{i}", [128, D], BF16) for i in range(2)]
                XT = [sb(e2, f"XT{i}", [128, 8, 128], BF16) for i in range(2)]
                w1b = [sb(e2, f"w1b{i}", [128, 8, DE], BF16) for i in range(2)]
                w3b = [sb(e2, f"w3b{i}", [128, 8, DE], BF16) for i in range(2)]
                w2b = [sb(e2, f"w2b{i}", [128, 3, D], BF16) for i in range(2)]
                s1 = sb(e2, "s1", [128, 384], F32)
                actT = sb(e2, "actT", [128, 3, 128], BF16)
                ysb = [sb(e2, f"ysb{i}", [128, D], F32) for i in range(2)]

                def issue_loads(bi):
                    i = bi % 2
                    DMA('sp', Xb[i][:], rows_d[bi * 128:(bi + 1) * 128, :], ['rows_d'], [f"Xb{i}"])
                    wsem = f'd_wg{i}'
                    fw.throttle('pool', wsem, 0)
                    for q_ in range(8):
                        off = bass.IndirectOffsetOnAxis(ap=widx4[:, q_, bi:bi + 1], axis=0)
                        for (wb_, wv_, wt_) in ((w1b[i], w1v, f"w1b{i}"), (w3b[i], w3v, f"w3b{i}")):
                            fw.throttle('pool', wsem, 2)
                            fw.dma('pool', lambda e: e.indirect_dma_start(out=wb_[:, q_, :], out_offset=None,
                                                                          in_=wv_, in_offset=off), ['widx'], [wt_], sem=wsem)
                    fw.throttle('pool', wsem, 0)
                    for c_ in range(3):
                        off = bass.IndirectOffsetOnAxis(ap=widx3[:, c_, bi:bi + 1], axis=0)
                        fw.throttle('pool', wsem, 2)
                        fw.dma('pool', lambda e: e.indirect_dma_start(out=w2b[i][:, c_, :], out_offset=None, in_=w2v,
                                                                      in_offset=off), ['widx'], [f"w2b{i}"], sem=wsem)
                    fw.throttle('pool', wsem, 0)

                issue_loads(0)
                for bi in range(NBLK):
                    i = bi % 2
                    if bi + 1 < NBLK:
                        issue_loads(bi + 1)
                    xv = Xb[i][:].rearrange("s (p k) -> s p k", k=8)
                    for half in range(2):
                        bk = nb(0, 2)
                        pt = f"ps{bk}"
                        pbf = ps[bk][:].bitcast(BF16)
                        for q4 in range(4):
                            kc = half * 4 + q4
                            PE(lambda e: e.transpose(pbf[:, q4 * 128:(q4 + 1) * 128], xv[:, :, kc], ident_b[:]),
                               [f"Xb{i}", 'c_ident_b'], [pt])
                        V(lambda e: e.tensor_copy(out=XT[i][:, half * 4:half * 4 + 4, :],
                                                  in_=pbf[:, 0:512].rearrange("p (q c) -> p q c", c=128)), [], [pt, f"XT{i}"])
                    b1, b3 = 2 + (bi % 2) * 2, 3 + (bi % 2) * 2
                    for (wb_, wt_, bk) in ((w1b[i], f"w1b{i}", b1), (w3b[i], f"w3b{i}", b3)):
                        for c in range(3):
                            for kc in range(8):
                                MM(ps[bk][:, c * 128:(c + 1) * 128], wb_[:, kc, :].rearrange("p (q c) -> p q c", c=3)[:, :, c],
                                   XT[i][:, kc, :], kc == 0, kc == 7, [wt_, f"XT{i}"], [f"ps{bk}"])
                    A(lambda e: e.activation(out=s1[:], in_=ps[b1][:, 0:384], func=AF.Silu), [], [f"ps{b1}", 's1'])
                    V(lambda e: e.tensor_tensor(out=actT[:].rearrange("p c s -> p (c s)"), in0=ps[b3][:, 0:384], in1=s1[:], op=ALU.mult),
                      ['s1'], [f"ps{b3}", 'actT'])
                    for nh in range(2):
                        bk = 6 + nh
                        for c in range(3):
                            MM(ps[bk][:, :], actT[:, c, :], w2b[i][:, c, nh * 512:(nh + 1) * 512], c == 0, c == 2,
                               ['actT', f"w2b{i}"], [f"ps{bk}"])
                        if nh == 0:
                            A(lambda e: e.copy(out=ysb[i][:, 0:512], in_=ps[bk][:, :]), [], [f"ps{bk}", f"ysb{i}"])
                        else:
                            V(lambda e: e.tensor_copy(out=ysb[i][:, 512:1024], in_=ps[bk][:, :]), [], [f"ps{bk}", f"ysb{i}"])
                    DMA('sp', y_d[bi * 128:(bi + 1) * 128, :], ysb[i][:], [f"ysb{i}"], ['y_d'])
                    if stop_after == ('moe_blk0', l) and bi == 0:
                        dd_ = sb(e2, "dd_", [128, D], F32)
                        V(lambda e: e.tensor_copy(out=dd_[:], in_=Xb[0][:]), ['Xb0'], ['dd_'])
                        DMA('sp', out[0:128, :], dd_[:], ['dd_'], ['dbgo'])
                        V(lambda e: e.memset(dd_[:], 0.0), [], ['dd_'])
                        V(lambda e: e.tensor_copy(out=dd_[:, 0:768], in_=w1b[0][:, 0:2, :].rearrange("p a b -> p (a b)")), ['w1b0'], ['dd_'])
                        DMA('sp', out[128:256, :], dd_[:], ['dd_'], ['dbgo'])
                        V(lambda e: e.tensor_copy(out=dd_[:], in_=w2b[0][:, 0, :]), ['w2b0'], ['dd_'])
                        DMA('sp', out[256:384, :], dd_[:], ['dd_'], ['dbgo'])
                        DMA('sp', out[384:512, :], ysb[0][:], ['ysb0'], ['dbgo'])
                        V(lambda e: e.memset(dd_[:], 0.0), [], ['dd_'])
                        V(lambda e: e.tensor_copy(out=dd_[:, 0:32], in_=d1i[:]), ['dki'], ['dd_'])
                        V(lambda e: e.tensor_copy(out=dd_[:, 32:64], in_=d2i[:]), ['dki'], ['dd_'])
                        V(lambda e: e.tensor_copy(out=dd_[:, 64:160], in_=widx[:]), ['widx'], ['dd_'])
                        V(lambda e: e.tensor_copy(out=dd_[:, 160:256], in_=widx4[:, 1, :]), ['widx'], ['dd_'])
                        V(lambda e: e.tensor_copy(out=dd_[:, 256:640], in_=actT[:].rearrange("p c s -> p (c s)")), ['actT'], ['dd_'])
                        V(lambda e: e.tensor_copy(out=dd_[:, 640:768], in_=XT[0][:, 3, :]), ['XT0'], ['dd_'])
                        DMA('sp', out[640:768, :], dd_[:], ['dd_'], ['dbgo'])
                        fw.barrier()
                        return
                fw.barrier()
            with ExitStack() as e3:
                xt = [sb(e3, f"xc{i}", [128, D], F32) for i in range(2)]
                y1 = [sb(e3, f"y1{i}", [128, D], F32) for i in range(2)]
                y2 = [sb(e3, f"y2{i}", [128, D], F32) for i in range(2)]
                for t in range(NT):
                    i = t % 2
                    b = t // 16
                    r0 = t * 128
                    DMA('sp', xt[i][:], out[r0:r0 + 128, :], ['xout'], [f"xc{i}"])
                    fw.throttle('pool', 'd_yg', 2)
                    fw.dma('pool', lambda e: e.indirect_dma_start(out=y1[i][:], out_offset=None, in_=y_d,
                                                                  in_offset=bass.IndirectOffsetOnAxis(ap=d1i[:, t:t + 1], axis=0)),
                           ['y_d', 'dki'], [f"y1{i}"], sem='d_yg')
                    fw.throttle('pool', 'd_yg', 2)
                    fw.dma('pool', lambda e: e.indirect_dma_start(out=y2[i][:], out_offset=None, in_=y_d,
                                                                  in_offset=bass.IndirectOffsetOnAxis(ap=d2i[:, t:t + 1], axis=0)),
                           ['y_d', 'dki'], [f"y2{i}"], sem='d_yg')
                    V(lambda e: e.tensor_scalar(out=y1[i][:], in0=y1[i][:], scalar1=gates[:, t, 0:1], scalar2=None, op0=ALU.mult),
                      [f"y1{i}", 'gates'], [f"y1{i}"])
                    V(lambda e: e.scalar_tensor_tensor(out=y1[i][:], in0=y2[i][:], scalar=gates[:, t, 1:2], in1=y1[i][:],
                                                       op0=ALU.mult, op1=ALU.add), [f"y1{i}", f"y2{i}", 'gates'], [f"y1{i}"])
                    G(lambda e: e.tensor_tensor(out=y1[i][:], in0=y1[i][:], in1=gt2b[:, b, :], op=ALU.mult), [f"y1{i}", 'gt2b'], [f"y1{i}"])
                    G(lambda e: e.tensor_tensor(out=xt[i][:], in0=xt[i][:], in1=y1[i][:], op=ALU.add), [f"y1{i}", f"xc{i}"], [f"xc{i}"])
                    DMA('sp', out[r0:r0 + 128, :], xt[i][:], [f"xc{i}"], ['xout2'])
                fw.barrier()

    done = False
    if moe_only:
        stage0(0)
        with ExitStack() as cp:
            xc_ = [sb(cp, f"xcp{i}", [128, D], F32) for i in range(2)]
            for t in range(NT):
                DMA('sp', xc_[t % 2][:], dr['x'][t * 128:(t + 1) * 128, :], [], [f"xcp{t % 2}"])
                DMA('sp', out[t * 128:(t + 1) * 128, :], xc_[t % 2][:], [f"xcp{t % 2}"], ['xout'])
            fw.barrier()
        stage_moe(0)
        nlayers = 0
    for l in range(nlayers):
        stage0(l)
        if stop_after == ('stage0', l):
            break
        for b in seqs:
            with ExitStack() as ms:
                oT_mla = sb(ms, "oT_mla", [128, 4, S], BF16)
                oT_dil = sb(ms, "oT_dil", [128, 2, S], BF16)
                oT_win = sb(ms, "oT_win", [128, 4, S], BF16)
                stageA(l, b)
                if stop_after in (('A', l), ('A', l, b)):
                    done = True
                    break
                stage_mla(l, b, oT_mla)
                if stop_after in (('mla', l), ('mla', l, b)):
                    done = True
                    break
                stage_band(l, b, oT_dil, oT_win)
                if stop_after in (('band', l), ('band', l, b)):
                    done = True
                    break
                stage_merge(l, b, oT_mla, oT_dil, oT_win)
                if stop_after in (('merge', l), ('merge', l, b)):
                    done = True
                    break
        if done:
            break
        xsrc[0] = out
        if stop_after == ('mixer', l):
            break
        stage_moe(l)
        if stop_after in (('layer', l), ('slots', l), ('e1tiles', l)):
            break
    fw.barrier()
    top.close()
    build_program.stats = (fw.ninst, fw.nwaits, len(fw.semobj))
    return nc


def kernel(**inputs):
    inputs = {k: np.asarray(v) for k, v in inputs.items()}
    shared = host_shared(inputs)
    in_maps = []
    for core in range(8):
        m = dict(shared)
        m.update(host_layout(inputs, core))
        in_maps.append(m)
    nc = build_program()
    res = run_bass_kernel_spmd(nc, in_maps, core_ids=list(range(8)))
    outs = [np.asarray(r["out"]).reshape(NSEQ, S, D) for r in res.results]
    return np.concatenate(outs, axis=0).astype(np.float32)
```

```python
import math
from contextlib import ExitStack
import numpy as np
import ml_dtypes
import concourse.bass as bass
import concourse.mybir as mybir
from concourse.bass_utils import run_bass_kernel_spmd

F32 = mybir.dt.float32
BF16 = mybir.dt.bfloat16
I32 = mybir.dt.int32
ALU = mybir.AluOpType
AF = mybir.ActivationFunctionType
AX = mybir.AxisListType

D = 1024
S = 2048
NSEQ = 2
T = NSEQ * S
NT = T // 128
L = 2
IN_COLS = 6944
OFF_CQ, OFF_CKV, OFF_KR, OFF_DIL, OFF_WIN, OFF_GATE = 0, 512, 768, 800, 3104, 3872
NE = 32
DE = 384
NBLK = 2 * T // 128 + NE
NSLOT = NBLK * 128
EPS = 1e-6
BIG = 1.0e6
MLA_SCALE = 96 ** -0.5
HD_SCALE = 64 ** -0.5
DIL_R = (1, 2, 8)
DIL_PAT = ((128, 1), (512, 4), (2048, 16))
MASK_BASE = (0, 3, 8, 25)
NMASK = 28


def alibi(n):
    return [2.0 ** (-8.0 * (i + 1) / n) for i in range(n)]


class FW:
    def __init__(self, nc, same_engine_sync=True):
        self.nc = nc
        self.eng = {'pe': nc.tensor, 'act': nc.scalar, 'dve': nc.vector, 'pool': nc.gpsimd, 'sp': nc.sync}
        self.semobj = {}
        self.cnt = {}
        self.dcnt = {}
        for e in ['pe', 'act', 'dve', 'pool']:
            self.semobj[e] = nc.alloc_semaphore("s_" + e)
            self.cnt[e] = 0
        self.pool = [nc.alloc_semaphore(f"s_dma{i}") for i in range(84)]
        self.allsems = [self.semobj[e] for e in ['pe', 'act', 'dve', 'pool']] + list(self.pool)
        self.seen = {e: {} for e in self.eng}
        self.lastw = {}
        self.readers = {}
        self.same = same_engine_sync
        self.nwaits = 0
        self.ninst = 0

    def clear_all(self):
        for sm in self.allsems:
            self.nc.gpsimd.sem_clear(sm)
        self.nc.all_engine_barrier()

    def _wait(self, E, ev):
        key, val = ev
        if key == E and (E == 'pe' or not self.same):
            return
        if key in self.dcnt:
            val = self.dcnt[key]
        if self.seen[E].get(key, 0) >= val:
            return
        self.eng[E].wait_ge(self.semobj[key], val)
        self.seen[E][key] = val
        self.nwaits += 1

    def _deps(self, E, reads, writes):
        for t in reads:
            ev = self.lastw.get(t)
            if ev is not None:
                self._wait(E, ev)
        for t in writes:
            ev = self.lastw.get(t)
            if ev is not None:
                self._wait(E, ev)
            for ev in self.readers.get(t, ()):
                self._wait(E, ev)

    def _commit(self, ev, reads, writes):
        for t in reads:
            lst = self.readers.setdefault(t, [])
            lst.append(ev)
            if len(lst) > 16:
                d = {}
                for k, v in lst:
                    d[k] = max(d.get(k, 0), v)
                self.readers[t] = list(d.items())
        for t in writes:
            self.lastw[t] = ev
            self.readers[t] = []

    def op(self, E, fn, reads=(), writes=()):
        self._deps(E, reads, writes)
        inst = fn(self.eng[E])
        self.cnt[E] += 1
        inst.then_inc(self.semobj[E], 1)
        self.ninst += 1
        self._commit((E, self.cnt[E]), reads, writes)
        return inst

    def dma(self, Q, fn, reads=(), writes=(), sem=None):
        if sem is None:
            sem = 'd_' + (writes[0] if writes else reads[0])
        if sem not in self.semobj:
            self.semobj[sem] = self.pool.pop()
            self.dcnt[sem] = 0
        self._deps(Q, reads, writes)
        if Q == 'pool':
            hist = self.__dict__.setdefault('pool_hist', [])
            if len(hist) >= 2:
                hk, hv = hist[-2]
                if self.seen['pool'].get(hk, 0) < hv:
                    self.eng['pool'].wait_ge(self.semobj[hk], hv)
                    self.seen['pool'][hk] = hv
                    self.nwaits += 1
        inst = fn(self.eng[Q])
        self.dcnt[sem] += 16
        inst.then_inc(self.semobj[sem], 16)
        self.ninst += 1
        self._commit((sem, self.dcnt[sem]), reads, writes)
        if Q == 'pool':
            self.pool_hist.append((sem, self.dcnt[sem]))
            if len(self.pool_hist) > 4:
                self.pool_hist.pop(0)
        return inst

    def throttle(self, Q, sem, lag):
        v = self.dcnt.get(sem, 0) - 16 * lag
        if v > 0 and self.seen[Q].get(sem, 0) < v:
            self.eng[Q].wait_ge(self.semobj[sem], v)
            self.seen[Q][sem] = v
            self.nwaits += 1

    def barrier(self):
        for E in self.eng:
            for k, v in self.cnt.items():
                if v > 0:
                    self._wait(E, (k, v))
            for k, v in self.dcnt.items():
                if v > 0:
                    self._wait(E, (k, v))
        self.lastw = {}
        self.readers = {}


def host_consts():
    c = {}
    c['c_ident_f'] = np.eye(128, dtype=np.float32)
    c['c_ident_b'] = np.eye(128, dtype=np.float32).astype(ml_dtypes.bfloat16)
    c['c_ones_b'] = np.ones((128, 128), np.float32).astype(ml_dtypes.bfloat16)
    blk = np.zeros((128, 128), np.float32)
    blk[0:64, 0:64] = 1
    blk[64:96, 64:96] = 1
    c['c_blk96_b'] = blk.astype(ml_dtypes.bfloat16)
    j = np.arange(128)
    c['c_ustrict_b'] = (j[:, None] < j[None, :]).astype(np.float32).astype(ml_dtypes.bfloat16)
    masks = np.zeros((128, NMASK, 128), np.float32)
    k = np.arange(128)[:, None]
    q = np.arange(128)[None, :]
    for g in range(3):
        win, dil = DIL_PAT[g]
        R = DIL_R[g]
        for dl in range(-R, R + 1):
            delta = 128 * dl + k - q
            valid = (np.abs(delta) <= win // 2) & (delta % dil == 0)
            masks[:, MASK_BASE[g] + dl + R, :] = np.where(valid, 0.0, BIG)
    for dl in range(-1, 2):
        delta = 128 * dl + k - q
        valid = np.abs(delta) <= 128
        masks[:, MASK_BASE[3] + dl + 1, :] = np.where(valid, 0.0, BIG)
    c['c_masks'] = masks
    col = np.zeros((128, 8), np.float32)
    col[:, 0] = 1.0
    col[0:64, 0] = 1.0 / 64
    col[64:96, 0] = 1.0 / 32
    col[:, 1] = EPS
    invf = 10000.0 ** (-np.arange(16, dtype=np.float32) / 16)
    col[64:80, 2] = -invf
    col[80:96, 2] = invf
    col[64:80, 3] = invf
    col[80:96, 3] = invf
    col[:, 4] = 1.0 / 64
    col[:, 5] = np.arange(128)
    col[:, 6] = math.pi / 2
    col[:, 7] = 0.0
    c['c_col'] = col
    c['c_bstart'] = np.tile((np.arange(NBLK, dtype=np.float32) * 128)[None, :], (128, 1))
    return c


CONST_SPECS = [('c_ident_f', [128, 128], F32), ('c_ident_b', [128, 128], BF16), ('c_ones_b', [128, 128], BF16),
               ('c_blk96_b', [128, 128], BF16), ('c_ustrict_b', [128, 128], BF16),
               ('c_masks', [128, NMASK, 128], F32), ('c_col', [128, 8], F32), ('c_bstart', [128, NBLK], F32)]

IN_SPECS = [('x', [T, D], F32), ('cT', [128, 8, 2], F32), ('pos', [NSEQ, S], I32), ('posT', [128, NSEQ, 16], I32),
            ('w_ada', [L, D, 6 * D], F32), ('b_ada', [L, 1, 6 * D], F32), ('g1T', [L, 128, 8], F32),
            ('w_in', [L, D, IN_COLS], F32), ('gcqT', [L, 128, 4], F32), ('w_uq', [L, 512, 768], F32),
            ('gckvT', [L, 128, 2], F32), ('w_ukv', [L, 256, 1024], F32), ('pcol', [L, 128, 8], F32),
            ('sink', [L, 1, 8], F32), ('w_br_mla', [L, 512, D], F32), ('w_br_dil', [L, 256, D], F32),
            ('w_br_win', [L, 512, D], F32), ('w_out', [L, D, D], F32), ('g_norm2', [L, 1, D], F32),
            ('w_r', [L, D, 36], F32), ('b_r', [L, 1, 36], F32),
            ('w1', [L, NE, D, DE], F32), ('w3', [L, NE, D, DE], F32), ('w2', [L, NE, DE, D], F32)]


def host_layout(inp, core):
    b0 = core * NSEQ
    m = {}
    m['x'] = np.ascontiguousarray(inp['x'][b0:b0 + NSEQ].reshape(T, D))
    c = inp['c'][b0:b0 + NSEQ]
    m['cT'] = np.ascontiguousarray(c.reshape(NSEQ, 8, 128).transpose(2, 1, 0))
    pos = np.ascontiguousarray(inp['pos'][b0:b0 + NSEQ]).astype(np.int32)
    m['pos'] = pos
    m['posT'] = np.ascontiguousarray(pos.reshape(NSEQ, 16, 128).transpose(2, 0, 1))
    return m


def host_shared(inp):
    m = {}
    m['w_ada'] = inp['w_ada']
    m['b_ada'] = inp['b_ada'].reshape(L, 1, 6 * D)
    m['g1T'] = np.ascontiguousarray(inp['g_norm1'].reshape(L, 8, 128).transpose(0, 2, 1))
    m['w_in'] = inp['w_in']
    m['gcqT'] = np.ascontiguousarray(inp['g_cq'].reshape(L, 4, 128).transpose(0, 2, 1))
    m['w_uq'] = inp['w_uq']
    m['gckvT'] = np.ascontiguousarray(inp['g_ckv'].reshape(L, 2, 128).transpose(0, 2, 1))
    m['w_ukv'] = inp['w_ukv']
    pcol = np.zeros((L, 128, 8), np.float32)
    for l in range(L):
        gq, gk = inp['g_q_mla'][l], inp['g_k_mla'][l]
        pcol[l, 0:96, 0] = gq
        pcol[l, 64:80, 1] = gq[80:96]
        pcol[l, 80:96, 1] = gq[64:80]
        pcol[l, 0:96, 2] = gk
        pcol[l, 64:80, 3] = gk[80:96]
        pcol[l, 80:96, 3] = gk[64:80]
        pcol[l, 0:64, 4] = inp['g_q_dil'][l]
        pcol[l, 0:64, 5] = inp['g_k_dil'][l]
        pcol[l, 0:64, 6] = inp['g_q_win'][l]
        pcol[l, 0:64, 7] = inp['g_k_win'][l]
    m['pcol'] = pcol
    m['sink'] = inp['sink_win'].reshape(L, 1, 8)
    m['w_br_mla'] = inp['w_br_mla']
    m['w_br_dil'] = inp['w_br_dil']
    m['w_br_win'] = inp['w_br_win']
    m['w_out'] = inp['w_out']
    m['g_norm2'] = inp['g_norm2'].reshape(L, 1, D)
    m['w_r'] = np.ascontiguousarray(np.concatenate([inp['w_gr'], inp['w_er']], axis=2))
    m['b_r'] = np.ascontiguousarray(np.concatenate([inp['b_gr'], inp['b_er']], axis=1)).reshape(L, 1, 36)
    m['w1'] = inp['w1']
    m['w3'] = inp['w3']
    m['w2'] = inp['w2']
    m = {k: np.ascontiguousarray(v.astype(np.float32)) for k, v in m.items()}
    m.update(host_consts())
    return m


def build_program(stop_after=None, nlayers=L, same_engine_sync=True, tiny_moe=False, seqs=(0, 1), moe_only=False):
    nc = bass.Bass("TRN2", target_bir_lowering=False)
    fw = FW(nc, same_engine_sync=same_engine_sync)
    dr = {}
    for name, shape, dt in IN_SPECS + CONST_SPECS:
        if tiny_moe and name in ('w1', 'w3', 'w2'):
            shape = [L, 1] + list(shape[2:])
        dr[name] = nc.dram_tensor(name, list(shape), dt, kind="ExternalInput").ap()
    out = nc.dram_tensor("out", [T, D], F32, kind="ExternalOutput").ap()
    dbg = out[0:128, 0:512]
    hT_d = nc.dram_tensor("hT_d", [NSEQ, 128, 8, S], BF16, kind="Internal").ap()
    bc_d = nc.dram_tensor("bc_d", [3, NSEQ, 128, D], F32, kind="Internal").ap()
    rows_d = nc.dram_tensor("rows_d", [NSLOT, D], BF16, kind="Internal").ap()
    y_d = nc.dram_tensor("y_d", [NSLOT, D], F32, kind="Internal").ap()

    ps = [nc.alloc_psum_tensor(f"ps{i}", [128, 512], F32) for i in range(8)]

    def V(fn, r=(), w=()):
        return fw.op('dve', fn, r, w)

    def A(fn, r=(), w=()):
        return fw.op('act', fn, r, w)

    def G(fn, r=(), w=()):
        return fw.op('pool', fn, r, w)

    def PE(fn, r=(), w=()):
        return fw.op('pe', fn, r, w)

    def MM(o, lhsT, rhs, start, stop, r, w):
        return fw.op('pe', lambda e: e.matmul(o, lhsT=lhsT, rhs=rhs, start=start, stop=stop), r, w)

    def DMA(q, o, i, r, w, sem=None):
        return fw.dma(q, lambda e: e.dma_start(out=o, in_=i), r, w, sem=sem)

    uid = [0]

    def sb(es, name, shape, dt):
        uid[0] += 1
        return es.enter_context(nc.sbuf_tensor(f"{name}_{uid[0]}", list(shape), dt))

    top = ExitStack()
    cs = {}
    for name, shape, dt in CONST_SPECS:
        cs[name] = sb(top, "s_" + name, shape, dt)
        DMA('sp', cs[name][:], dr[name], [], [name])
    ident_f, ident_b, ones_b = cs['c_ident_f'], cs['c_ident_b'], cs['c_ones_b']
    blk96, ustrict, masks, col, bstart = cs['c_blk96_b'], cs['c_ustrict_b'], cs['c_masks'], cs['c_col'], cs['c_bstart']
    CONST_T = [n for n, _, _ in CONST_SPECS]
    ones_f = sb(top, "ones_f", [128, 128], F32)
    V(lambda e: e.memset(ones_f[:], 1.0), [], ['ones_f'])
    condT = sb(top, "condT", [128, 8, 2], F32)
    condrep = sb(top, "condrep", [128, 8, 2, 128], F32)
    modT = sb(top, "modT", [128, 16, 2], F32)
    A1 = sb(top, "A1", [128, 8, 2], F32)
    gt1b = sb(top, "gt1b", [128, NSEQ, D], F32)
    pcol = sb(top, "pcol", [128, 8], F32)
    g1T = sb(top, "g1T", [128, 8], F32)
    gcqT = sb(top, "gcqT", [128, 4], F32)
    gckvT = sb(top, "gckvT", [128, 2], F32)
    esink = sb(top, "esink", [128, 8], F32)
    poskT = sb(top, "poskT", [128, NSEQ, 16], F32)
    poskT_i = sb(top, "poskT_i", [128, NSEQ, 16], I32)
    DMA('sp', condT[:], dr['cT'], [], ['condT'])
    A(lambda e: e.activation(out=condT[:], in_=condT[:], func=AF.Silu), ['condT'], ['condT'])
    for b in range(NSEQ):
        V(lambda e: e.tensor_copy(out=condrep[:, :, b, :], in_=condT[:, :, b:b + 1].to_broadcast([128, 8, 128])),
          ['condT'], ['condrep'])
    DMA('sp', poskT_i[:], dr['posT'], [], ['poskT_i'])
    V(lambda e: e.tensor_copy(out=poskT[:], in_=poskT_i[:]), ['poskT_i'], ['poskT'])

    bank_rr = [0]

    def nb(lo=0, hi=8):
        b = lo + bank_rr[0] % (hi - lo)
        bank_rr[0] += 1
        return b

    xsrc = [dr['x']]

    def stage0(l):
        with ExitStack() as es:
            wblk = [sb(es, f"wada{i}", [128, 8, 512], F32) for i in range(2)]
            brow = sb(es, "brow", [1, 6 * D], F32)
            g2b = sb(es, "g2b", [128, D], F32)
            stg = [sb(es, f"stg{i}", [128, D], F32) for i in range(2)]
            DMA('sp', brow[:], dr['b_ada'][l], [], ['brow'])
            DMA('sp', g2b[:], dr['g_norm2'][l, 0].partition_broadcast(128), [], ['g2b'])
            DMA('sp', pcol[:], dr['pcol'][l], [], ['pcol'])
            DMA('sp', g1T[:], dr['g1T'][l], [], ['g1T'])
            DMA('sp', gcqT[:], dr['gcqT'][l], [], ['gcqT'])
            DMA('sp', gckvT[:], dr['gckvT'][l], [], ['gckvT'])
            DMA('sp', esink[:], dr['sink'][l, 0].partition_broadcast(128), [], ['esink'])
            A(lambda e: e.activation(out=esink[:], in_=esink[:], func=AF.Exp), ['esink'], ['esink'])
            wv = dr['w_ada'][l].rearrange("(kc p) n -> p kc n", p=128)
            for cb in range(12):
                wb = wblk[cb % 2]
                wt = f"wada{cb % 2}"
                DMA('sp', wb[:], wv[:, :, cb * 512:(cb + 1) * 512], [], [wt])
                if cb < 4:
                    for j in range(4):
                        bk = nb()
                        pt = f"ps{bk}"
                        for kc in range(8):
                            MM(ps[bk][:, 0:2], wb[:, kc, j * 128:(j + 1) * 128], condT[:, kc, :], kc == 0, False,
                               [wt, 'condT'], [pt])
                        MM(ps[bk][:, 0:2], brow[0:1, cb * 512 + j * 128: cb * 512 + (j + 1) * 128], ones_f[0:1, 0:2],
                           False, True, ['brow', 'ones_f'], [pt])
                        V(lambda e: e.tensor_copy(out=modT[:, cb * 4 + j, :], in_=ps[bk][:, 0:2]), [], [pt, 'modT'])
                else:
                    which = (cb - 4) // 2
                    nh = (cb - 4) % 2
                    for b in range(NSEQ):
                        bk = nb()
                        pt = f"ps{bk}"
                        for kc in range(8):
                            MM(ps[bk][:, :], condrep[:, kc, b, :], wb[:, kc, :], kc == 0, False, [wt, 'condrep'], [pt])
                        MM(ps[bk][:, :], ones_f[0:1, 0:128], brow[0:1, cb * 512:(cb + 1) * 512], False, True,
                           ['brow', 'ones_f'], [pt])
                        if which == 0:
                            V(lambda e: e.tensor_copy(out=gt1b[:, b, nh * 512:(nh + 1) * 512], in_=ps[bk][:, :]),
                              [], [pt, 'gt1b'])
                        else:
                            st = stg[b]
                            stt = f"stg{b}"
                            if which == 2:
                                V(lambda e: e.scalar_tensor_tensor(out=st[:, nh * 512:(nh + 1) * 512], in0=ps[bk][:, :],
                                                                   scalar=1.0, in1=g2b[:, nh * 512:(nh + 1) * 512],
                                                                   op0=ALU.add, op1=ALU.mult), ['g2b'], [pt, stt])
                            else:
                                V(lambda e: e.tensor_copy(out=st[:, nh * 512:(nh + 1) * 512], in_=ps[bk][:, :]),
                                  [], [pt, stt])
                            if nh == 1:
                                DMA('sp', bc_d[which - 1, b], st[:], [stt], ['bc_d'])
            V(lambda e: e.scalar_tensor_tensor(out=A1[:], in0=modT[:, 8:16, :], scalar=1.0,
                                               in1=g1T[:].unsqueeze(2).to_broadcast([128, 8, 2]),
                                               op0=ALU.add, op1=ALU.mult), ['modT', 'g1T'], ['A1'])
            fw.barrier()

    def stageA(l, b):
        with ExitStack() as es:
            xt = [sb(es, f"xa{i}", [128, D], F32) for i in range(2)]
            xn = [sb(es, f"xn{i}", [128, D], F32) for i in range(2)]
            junk = sb(es, "junkA", [128, D], BF16)
            st = [sb(es, f"stA{i}", [128, 4], F32) for i in range(2)]
            hT = [sb(es, f"hTa{i}", [128, 8, 128], BF16) for i in range(2)]
            for t in range(16):
                i = t % 2
                r0 = b * S + t * 128
                DMA('sp', xt[i][:], xsrc[0][r0:r0 + 128, :], ['xres'], [f'xa{i}'])
                A(lambda e: e.activation(out=junk[:], in_=xt[i][:], func=AF.Square, accum_out=st[i][:, 0:1]),
                  [f'xa{i}'], ['junkA', f'stA{i}'])
                A(lambda e: e.activation(out=st[i][:, 1:2], in_=st[i][:, 0:1], func=AF.Ln, scale=1.0 / D, bias=col[:, 1:2]),
                  [f'stA{i}', 'c_col'], [f'stA{i}'])
                A(lambda e: e.activation(out=st[i][:, 2:3], in_=st[i][:, 1:2], func=AF.Exp, scale=-0.5),
                  [f'stA{i}'], [f'stA{i}'])
                V(lambda e: e.tensor_scalar(out=xn[i][:], in0=xt[i][:], scalar1=st[i][:, 2:3], scalar2=None, op0=ALU.mult),
                  [f'xa{i}', f'stA{i}'], [f'xn{i}'])
                for half in range(2):
                    bk = nb()
                    pt = f"ps{bk}"
                    for q4 in range(4):
                        kc = half * 4 + q4
                        PE(lambda e: e.transpose(ps[bk][:, q4 * 128:(q4 + 1) * 128], xn[i][:, kc * 128:(kc + 1) * 128], ident_f[:]),
                           [f'xn{i}', 'c_ident_f'], [pt])
                    for q4 in range(4):
                        kc = half * 4 + q4
                        V(lambda e: e.tensor_scalar(out=hT[i][:, kc, :], in0=ps[bk][:, q4 * 128:(q4 + 1) * 128],
                                                    scalar1=A1[:, kc, b:b + 1], scalar2=modT[:, kc, b:b + 1],
                                                    op0=ALU.mult, op1=ALU.add), ['A1', 'modT'], [pt, f'hTa{i}'])
                DMA('sp', hT_d[b][:, :, t * 128:(t + 1) * 128], hT[i][:], [f'hTa{i}'], ['hT_d'])
            fw.barrier()

    def headnorm(P, nrows, rs, gcol, out_ap, out_tok, tmp, Psw=None, gsw=None, CC=None, SS=None, P_tok=(), extra_r=()):
        qs, sq, sd, qn, t2 = tmp['qs'], tmp['sq'], tmp['sd'], tmp['qn'], tmp['t2']
        n = nrows
        if rs is not None:
            V(lambda e: e.tensor_tensor(out=qs[0:n, :], in0=P, in1=rs, op=ALU.mult), list(extra_r), list(P_tok) + ['hn_qs'])
            src = qs[0:n, :]
            src_r, src_w = ['hn_qs'], []
        else:
            src = P
            src_r, src_w = [], list(P_tok)
        A(lambda e: e.activation(out=sq[0:n, :], in_=src, func=AF.Square), src_r, src_w + ['hn_sq'])
        bk = nb(6, 8)
        pt = f"ps{bk}"
        blk = blk96 if n == 96 else ones_b
        MM(ps[bk][0:n, :], blk[0:n, 0:n], sq[0:n, :], True, True, ['hn_sq', 'c_blk96_b', 'c_ones_b'], [pt])
        sc = col[0:n, 0:1] if n == 96 else col[0:n, 4:5]
        A(lambda e: e.activation(out=sd[0:n, :], in_=ps[bk][0:n, :], func=AF.Ln, scale=sc, bias=col[0:n, 1:2]),
          ['c_col'], [pt, 'hn_sd'])
        A(lambda e: e.activation(out=sd[0:n, :], in_=sd[0:n, :], func=AF.Exp, scale=-0.5), ['hn_sd'], ['hn_sd'])
        if Psw is None:
            V(lambda e: e.scalar_tensor_tensor(out=out_ap, in0=src, scalar=gcol, in1=sd[0:n, :], op0=ALU.mult, op1=ALU.mult),
              src_r + ['hn_sd', 'pcol'], src_w + [out_tok])
            return
        V(lambda e: e.scalar_tensor_tensor(out=qn[0:n, :], in0=src, scalar=gcol, in1=sd[0:n, :], op0=ALU.mult, op1=ALU.mult),
          src_r + ['hn_sd', 'pcol'], src_w + ['hn_qn'])
        Psw_ap, Psw_tok = Psw
        if rs is not None:
            V(lambda e: e.scalar_tensor_tensor(out=t2[0:n, :], in0=Psw_ap, scalar=gsw, in1=rs, op0=ALU.mult, op1=ALU.mult),
              ['pcol'] + list(extra_r), list(Psw_tok) + ['hn_t2'])
        else:
            V(lambda e: e.tensor_scalar(out=t2[0:n, :], in0=Psw_ap, scalar1=gsw, scalar2=None, op0=ALU.mult),
              ['pcol'], list(Psw_tok) + ['hn_t2'])
        G(lambda e: e.tensor_tensor(out=t2[0:n, :], in0=t2[0:n, :], in1=sd[0:n, :], op=ALU.mult), ['hn_sd', 'hn_t2'], ['hn_t2'])
        G(lambda e: e.tensor_tensor(out=t2[0:n, :], in0=t2[0:n, :], in1=SS, op=ALU.mult), ['hn_t2', 'ropeT'], ['hn_t2'])
        G(lambda e: e.tensor_tensor(out=qn[0:n, :], in0=qn[0:n, :], in1=CC, op=ALU.mult), ['hn_qn', 'ropeT'], ['hn_qn'])
        V(lambda e: e.tensor_tensor(out=out_ap, in0=qn[0:n, :], in1=t2[0:n, :], op=ALU.add), ['hn_qn', 'hn_t2'], [out_tok])

    def mk_hn_tmp(es):
        return {'qs': sb(es, "hn_qs", [128, 512], F32), 'sq': sb(es, "hn_sq", [128, 512], BF16),
                'sd': sb(es, "hn_sd", [128, 512], F32), 'qn': sb(es, "hn_qn", [128, 512], F32),
                't2': sb(es, "hn_t2", [128, 512], F32)}

    def load_w_cast(dst_ap, src_ap, tok):
        fw.dma('pool', lambda e: e.dma_start(out=dst_ap, in_=src_ap), [], [tok])

    def proj_fm(bk, lhs_list, rhs_list, n, reads, rows=None):
        pt = f"ps{bk}"
        o = ps[bk][0:n, :] if rows is None else ps[bk][rows[0]:rows[1], :]
        K = len(lhs_list)
        for k in range(K):
            MM(o, lhs_list[k], rhs_list[k], k == 0, k == K - 1, reads, [pt])

    def stage_mla(l, b, oT_mla):
        TWO_PI = 2.0 * math.pi
        C1 = 6.28125
        C2 = TWO_PI - C1
        with ExitStack() as es:
            tmp = mk_hn_tmp(es)
            CC = sb(es, "CC", [128, S], F32)
            SS = sb(es, "SS", [128, S], F32)
            ropei = sb(es, "ropei", [128, 512], I32)
            for ch in range(4):
                cs_ = slice(ch * 512, (ch + 1) * 512)
                DMA('sp', ropei[0:96, :], dr['pos'][b, ch * 512:(ch + 1) * 512].partition_broadcast(96), ['hn_qs'], ['ropei'])
                V(lambda e: e.tensor_copy(out=tmp['qs'][0:96, :], in_=ropei[0:96, :]), ['ropei'], ['hn_qs'])
                for tab, ci, phase in ((CC, 3, math.pi / 2), (SS, 2, 0.0)):
                    ang, kf, r_ = tmp['sd'], tmp['qn'], tmp['t2']
                    V(lambda e: e.tensor_scalar(out=ang[0:96, :], in0=tmp['qs'][0:96, :], scalar1=col[0:96, ci:ci + 1],
                                                scalar2=phase, op0=ALU.mult, op1=ALU.add), ['hn_qs', 'c_col'], ['hn_sd'])
                    V(lambda e: e.tensor_scalar(out=ropei[0:96, :], in0=ang[0:96, :], scalar1=1.0 / TWO_PI, scalar2=None,
                                                op0=ALU.mult), ['hn_sd'], ['ropei'])
                    V(lambda e: e.tensor_copy(out=kf[0:96, :], in_=ropei[0:96, :]), ['ropei'], ['hn_qn'])
                    V(lambda e: e.scalar_tensor_tensor(out=r_[0:96, :], in0=kf[0:96, :], scalar=-C1, in1=ang[0:96, :],
                                                       op0=ALU.mult, op1=ALU.add), ['hn_qn', 'hn_sd'], ['hn_t2'])
                    V(lambda e: e.scalar_tensor_tensor(out=r_[0:96, :], in0=kf[0:96, :], scalar=-C2, in1=r_[0:96, :],
                                                       op0=ALU.mult, op1=ALU.add), ['hn_qn', 'hn_t2'], ['hn_t2'])
                    V(lambda e: e.tensor_scalar(out=kf[0:96, :], in0=r_[0:96, :], scalar1=math.pi, scalar2=-TWO_PI,
                                                op0=ALU.is_gt, op1=ALU.mult), ['hn_t2'], ['hn_qn'])
                    V(lambda e: e.tensor_tensor(out=r_[0:96, :], in0=r_[0:96, :], in1=kf[0:96, :], op=ALU.add),
                      ['hn_qn', 'hn_t2'], ['hn_t2'])
                    V(lambda e: e.tensor_scalar(out=kf[0:96, :], in0=r_[0:96, :], scalar1=-math.pi, scalar2=TWO_PI,
                                                op0=ALU.is_lt, op1=ALU.mult), ['hn_t2'], ['hn_qn'])
                    V(lambda e: e.tensor_tensor(out=r_[0:96, :], in0=r_[0:96, :], in1=kf[0:96, :], op=ALU.add),
                      ['hn_qn', 'hn_t2'], ['hn_t2'])
                    A(lambda e: e.activation(out=tab[0:96, cs_], in_=r_[0:96, :], func=AF.Sin), ['hn_t2'], ['ropeT'])
            wA = sb(es, "wA", [128, 8, 768], BF16)
            wkr = sb(es, "wkr", [128, 8, 96], BF16)
            wkrs = sb(es, "wkrs", [128, 8, 96], BF16)
            win_v = dr['w_in'][l].rearrange("(kc p) n -> p kc n", p=128)
            load_w_cast(wA[:], win_v[:, :, 0:768], 'wA')
            G(lambda e: e.memset(wkr[:, :, 0:64], 0.0), [], ['wkr'])
            G(lambda e: e.memset(wkrs[:, :, 0:64], 0.0), [], ['wkrs'])
            load_w_cast(wkr[:, :, 64:96], win_v[:, :, 768:800], 'wkr')
            load_w_cast(wkrs[:, :, 64:80], win_v[:, :, 784:800], 'wkrs')
            load_w_cast(wkrs[:, :, 80:96], win_v[:, :, 768:784], 'wkrs')
            QT = sb(es, "QT", [128, 4, S], BF16)
            KT = sb(es, "KT", [128, 4, S], BF16)
            KR = sb(es, "KR", [128, S], BF16)
            Vm = sb(es, "Vm", [128, 16, 256], BF16)
            wuq = sb(es, "wuq", [128, 4, 384], BF16)
            wuqs = sb(es, "wuqs", [128, 4, 384], BF16)
            wukv = sb(es, "wukv", [128, 2, 512], BF16)
            hc = [sb(es, f"hc{i}", [128, 8, 512], BF16) for i in range(2)]
            cqg = sb(es, "cqg", [128, 4, 512], BF16)
            ckvg = sb(es, "ckvg", [128, 2, 512], BF16)
            sqc = sb(es, "sqc", [128, 4, 512], BF16)
            sqk = sb(es, "sqk", [128, 2, 512], BF16)
            rs_cq = sb(es, "rs_cq", [128, 512], F32)
            rs_ckv = sb(es, "rs_ckv", [128, 512], F32)
            rs_col = sb(es, "rs_col", [128, 4], F32)
            pT = [sb(es, f"pT{i}", [128, 512], BF16) for i in range(2)]
            rden = sb(es, "rden", [128, 512], F32)
            uq_v = dr['w_uq'][l].rearrange("(kc p) n -> p kc n", p=128)
            ukv_v = dr['w_ukv'][l].rearrange("(kc p) n -> p kc n", p=128)
            for half in range(2):
                hh0 = half * 4
                load_w_cast(wuq[:], uq_v[:, :, hh0 * 96:(hh0 + 4) * 96], 'wuq')
                load_w_cast(wukv[:], ukv_v[:, :, hh0 * 128:(hh0 + 4) * 128], 'wukv')
                wq4 = wuq[:].rearrange("p k (h c) -> p k h c", c=96)
                ws4 = wuqs[:].rearrange("p k (h c) -> p k h c", c=96)
                G(lambda e: e.memset(wuqs[:], 0.0), [], ['wuqs'])
                for kc in range(4):
                    G(lambda e: e.tensor_copy(out=ws4[:, kc, :, 64:80], in_=wq4[:, kc, :, 80:96]), ['wuq'], ['wuqs'])
                    G(lambda e: e.tensor_copy(out=ws4[:, kc, :, 80:96], in_=wq4[:, kc, :, 64:80]), ['wuq'], ['wuqs'])
                for ch in range(4):
                    cs_ = slice(ch * 512, (ch + 1) * 512)
                    hcb = hc[ch % 2]
                    hct = f"hc{ch % 2}"
                    DMA('sp', hcb[:], hT_d[b][:, :, cs_], ['hT_d'], [hct])
                    for ft in range(6):
                        bk = nb(0, 6)
                        proj_fm(bk, [wA[:, kc, ft * 128:(ft + 1) * 128] for kc in range(8)], [hcb[:, kc, :] for kc in range(8)],
                                128, ['wA', hct])
                        pt = f"ps{bk}"
                        if ft < 4:
                            V(lambda e: e.tensor_scalar(out=cqg[:, ft, :], in0=ps[bk][:, :], scalar1=gcqT[:, ft:ft + 1],
                                                        scalar2=None, op0=ALU.mult), ['gcqT'], [pt, 'cqg'])
                            A(lambda e: e.activation(out=sqc[:, ft, :], in_=ps[bk][:, :], func=AF.Square), [], [pt, 'sqc'])
                        else:
                            f2 = ft - 4
                            V(lambda e: e.tensor_scalar(out=ckvg[:, f2, :], in0=ps[bk][:, :], scalar1=gckvT[:, f2:f2 + 1],
                                                        scalar2=None, op0=ALU.mult), ['gckvT'], [pt, 'ckvg'])
                            A(lambda e: e.activation(out=sqk[:, f2, :], in_=ps[bk][:, :], func=AF.Square), [], [pt, 'sqk'])
                    for (sq_, nf, rs_, rtok, sqtok, dim) in ((sqc, 4, rs_cq, 'rs_cq', 'sqc', 512), (sqk, 2, rs_ckv, 'rs_ckv', 'sqk', 256)):
                        bk = nb(6, 8)
                        pt = f"ps{bk}"
                        for ft in range(nf):
                            MM(ps[bk][:, :], ones_b[:, :], sq_[:, ft, :], ft == 0, ft == nf - 1, [sqtok, 'c_ones_b'], [pt])
                        A(lambda e: e.activation(out=rs_[:, :], in_=ps[bk][:, :], func=AF.Ln, scale=1.0 / dim, bias=col[:, 1:2]),
                          ['c_col'], [pt, rtok])
                        A(lambda e: e.activation(out=rs_[:, :], in_=rs_[:, :], func=AF.Exp, scale=-0.5), [rtok], [rtok])
                    bk = nb(6, 8)
                    pt = f"ps{bk}"
                    for tt in range(4):
                        for ft in range(2):
                            MM(ps[bk][:, tt:tt + 1], sqk[:, ft, tt * 128:(tt + 1) * 128], ones_b[:, 0:1], ft == 0, ft == 1,
                               ['sqk', 'c_ones_b'], [pt])
                    A(lambda e: e.activation(out=rs_col[:, :], in_=ps[bk][:, 0:4], func=AF.Ln, scale=1.0 / 256, bias=col[:, 1:2]),
                      ['c_col'], [pt, 'rs_col'])
                    A(lambda e: e.activation(out=rs_col[:, :], in_=rs_col[:, :], func=AF.Exp, scale=-0.5), ['rs_col'], ['rs_col'])
                    bkA, bkB = nb(0, 6), nb(0, 6)
                    proj_fm(bkA, [wkr[:, kc, :] for kc in range(8)], [hcb[:, kc, :] for kc in range(8)], 96, ['wkr', hct])
                    proj_fm(bkB, [wkrs[:, kc, :] for kc in range(8)], [hcb[:, kc, :] for kc in range(8)], 96, ['wkrs', hct])
                    headnorm(ps[bkA][0:96, :], 96, None, pcol[0:96, 2:3], KR[0:96, cs_], 'KR', tmp,
                             Psw=(ps[bkB][0:96, :], [f"ps{bkB}"]), gsw=pcol[0:96, 3:4], CC=CC[0:96, cs_], SS=SS[0:96, cs_],
                             P_tok=[f"ps{bkA}"])
                    for h in range(4):
                        bkA, bkB = nb(0, 6), nb(0, 6)
                        proj_fm(bkA, [wuq[:, kc, h * 96:(h + 1) * 96] for kc in range(4)], [cqg[:, kc, :] for kc in range(4)],
                                96, ['wuq', 'cqg'])
                        proj_fm(bkB, [wuqs[:, kc, h * 96:(h + 1) * 96] for kc in range(4)], [cqg[:, kc, :] for kc in range(4)],
                                96, ['wuqs', 'cqg'])
                        headnorm(ps[bkA][0:96, :], 96, rs_cq[0:96, :], pcol[0:96, 0:1], QT[0:96, h, cs_], 'QT', tmp,
                                 Psw=(ps[bkB][0:96, :], [f"ps{bkB}"]), gsw=pcol[0:96, 1:2], CC=CC[0:96, cs_], SS=SS[0:96, cs_],
                                 P_tok=[f"ps{bkA}"], extra_r=['rs_cq'])
                        bkA = nb(0, 6)
                        proj_fm(bkA, [wukv[:, kc, h * 128:h * 128 + 64] for kc in range(2)], [ckvg[:, kc, :] for kc in range(2)],
                                64, ['wukv', 'ckvg'])
                        headnorm(ps[bkA][0:64, :], 64, rs_ckv[0:64, :], pcol[0:64, 2:3], KT[0:64, h, cs_], 'KT', tmp,
                                 P_tok=[f"ps{bkA}"], extra_r=['rs_ckv'])
                        G(lambda e: e.tensor_copy(out=KT[64:96, h, cs_], in_=KR[64:96, cs_]), ['KR'], ['KT'])
                    wv4 = wukv[:].rearrange("p k (h c) -> p k h c", c=128)
                    for tt in range(4):
                        bk = nb(0, 6)
                        pt = f"ps{bk}"
                        for kc in range(2):
                            MM(ps[bk][:, 0:256].rearrange("p (h c) -> p h c", c=64), ckvg[:, kc, tt * 128:(tt + 1) * 128],
                               wv4[:, kc, :, 64:128], kc == 0, kc == 1, ['ckvg', 'wukv'], [pt])
                        V(lambda e: e.tensor_scalar(out=Vm[:, ch * 4 + tt, :], in0=ps[bk][:, 0:256], scalar1=rs_col[:, tt:tt + 1],
                                                    scalar2=None, op0=ALU.mult), ['rs_col'], [pt, 'Vm'])
                it = 0
                for h in range(4):
                    hg = hh0 + h
                    prow = (hg % 2) * 64
                    for qc in range(4):
                        bo, bd = 2 + it % 2, 4 + it % 2
                        it += 1
                        for kt in range(16):
                            bs = kt % 2
                            MM(ps[bs][:, :], KT[0:96, h, kt * 128:(kt + 1) * 128], QT[0:96, h, qc * 512:(qc + 1) * 512], True, True,
                               ['KT', 'QT'], [f"ps{bs}"])
                            A(lambda e: e.activation(out=pT[bs][:, :], in_=ps[bs][:, :], func=AF.Exp, scale=MLA_SCALE),
                              [], [f"ps{bs}", f"pT{bs}"])
                            MM(ps[bo][prow:prow + 64, :], Vm[:, kt, h * 64:(h + 1) * 64], pT[bs][:, :], kt == 0, kt == 15,
                               ['Vm', f"pT{bs}"], [f"ps{bo}"])
                            MM(ps[bd][prow:prow + 64, :], ones_b[:, 0:64], pT[bs][:, :], kt == 0, kt == 15,
                               ['c_ones_b', f"pT{bs}"], [f"ps{bd}"])
                        A(lambda e: e.activation(out=rden[prow:prow + 64, :], in_=ps[bd][prow:prow + 64, :], func=AF.Ln),
                          [], [f"ps{bd}", 'rden'])
                        A(lambda e: e.activation(out=rden[prow:prow + 64, :], in_=rden[prow:prow + 64, :], func=AF.Exp, scale=-1.0),
                          ['rden'], ['rden'])
                        V(lambda e: e.tensor_tensor(out=oT_mla[prow:prow + 64, hg // 2, qc * 512:(qc + 1) * 512],
                                                    in0=ps[bo][prow:prow + 64, :], in1=rden[prow:prow + 64, :], op=ALU.mult),
                          ['rden'], [f"ps{bo}", 'oT_mla'])
            fw.barrier()

    def band_group(l, b, es, tmp, wv_cols, n_k, gq_col, gk_col, R, mask_base, slopes, epilogue, posq, hcbufs, tag):
        with ExitStack() as gs:
            wG = sb(gs, "wG", [128, 8, 256 + 2 * 64 * n_k], BF16)
            QT = sb(gs, "QTb", [64, 4, S], BF16)
            KT = sb(gs, "KTb", [64, n_k, S], BF16)
            Vt = sb(gs, "Vtb", [128, 16, n_k, 65], BF16)
            tS = [sb(gs, f"tS{i}", [128, 4, 128], F32) for i in range(2)]
            pB = [sb(gs, f"pB{i}", [128, 4, 128], BF16) for i in range(2)]
            Dt = [sb(gs, f"Dt{i}", [128, 128], F32) for i in range(2)]
            win_v = dr['w_in'][l].rearrange("(kc p) n -> p kc n", p=128)
            q0, k0, v0 = wv_cols
            nkc = 64 * n_k
            load_w_cast(wG[:, :, 0:256], win_v[:, :, q0:q0 + 256], 'wG')
            load_w_cast(wG[:, :, 256:256 + nkc], win_v[:, :, k0:k0 + nkc], 'wG')
            load_w_cast(wG[:, :, 256 + nkc:256 + 2 * nkc], win_v[:, :, v0:v0 + nkc], 'wG')
            G(lambda e: e.memset(Vt[:, :, :, 64:65], 1.0), [], ['Vtb'])
            for ch in range(4):
                cs_ = slice(ch * 512, (ch + 1) * 512)
                hcb = hcbufs[ch % 2]
                hct = f"hc{ch % 2}"
                DMA('sp', hcb[:], hT_d[b][:, :, cs_], ['hT_d'], [hct])
                for hh in range(4):
                    bk = nb(0, 4)
                    proj_fm(bk, [wG[:, kc, hh * 64:(hh + 1) * 64] for kc in range(8)], [hcb[:, kc, :] for kc in range(8)],
                            64, ['wG', hct])
                    headnorm(ps[bk][0:64, :], 64, None, gq_col, QT[0:64, hh, cs_], 'QTb', tmp, P_tok=[f"ps{bk}"])
                for hk in range(n_k):
                    bk = nb(0, 4)
                    proj_fm(bk, [wG[:, kc, 256 + hk * 64:256 + (hk + 1) * 64] for kc in range(8)],
                            [hcb[:, kc, :] for kc in range(8)], 64, ['wG', hct])
                    headnorm(ps[bk][0:64, :], 64, None, gk_col, KT[0:64, hk, cs_], 'KTb', tmp, P_tok=[f"ps{bk}"])
                for tt in range(4):
                    bk = nb(0, 4)
                    pt = f"ps{bk}"
                    for kc in range(8):
                        MM(ps[bk][:, 0:nkc], hcb[:, kc, tt * 128:(tt + 1) * 128], wG[:, kc, 256 + nkc:256 + 2 * nkc],
                           kc == 0, kc == 7, ['wG', hct], [pt])
                    V(lambda e: e.tensor_copy(out=Vt[:, ch * 4 + tt, :, 0:64],
                                              in_=ps[bk][:, 0:nkc].rearrange("p (h c) -> p h c", c=64)), [], [pt, 'Vtb'])
            it = 0
            for blk in range(16):
                kts = list(range(max(0, blk - R), min(15, blk + R) + 1))
                for j, kt in enumerate(kts):
                    dl = kt - blk
                    i2 = it % 2
                    it += 1
                    d_ = Dt[i2]
                    dtok = f"Dt{i2}"
                    V(lambda e: e.scalar_tensor_tensor(out=d_[:, :], in0=posq[:, blk * 128:(blk + 1) * 128],
                                                       scalar=poskT[:, b, kt:kt + 1], in1=masks[:, mask_base + dl + R, :],
                                                       op0=ALU.subtract, op1=ALU.add), ['posq', 'poskT', 'c_masks'], [dtok])
                    V(lambda e: e.scalar_tensor_tensor(out=d_[:, :], in0=d_[:, :], scalar=-1.0, in1=d_[:, :],
                                                       op0=ALU.mult, op1=ALU.max), [dtok], [dtok])
                    bs = i2
                    for hh in range(4):
                        hk = hh if n_k == 4 else 0
                        MM(ps[bs][:, hh * 128:(hh + 1) * 128], KT[0:64, hk, kt * 128:(kt + 1) * 128],
                           QT[0:64, hh, blk * 128:(blk + 1) * 128], True, True, ['KTb', 'QTb'], [f"ps{bs}"])
                    for hh in range(4):
                        V(lambda e: e.scalar_tensor_tensor(out=tS[i2][:, hh, :], in0=d_[:, :], scalar=-slopes[hh] / HD_SCALE,
                                                           in1=ps[bs][:, hh * 128:(hh + 1) * 128], op0=ALU.mult, op1=ALU.add),
                          [dtok], [f"ps{bs}", f"tS{i2}"])
                    A(lambda e: e.activation(out=pB[i2][:], in_=tS[i2][:], func=AF.Exp, scale=HD_SCALE),
                      [f"tS{i2}"], [f"pB{i2}"])
                    for hh in range(4):
                        hk = hh if n_k == 4 else 0
                        MM(ps[4 + hh][:, 0:65], pB[i2][:, hh, :], Vt[:, kt, hk, :], j == 0, j == len(kts) - 1,
                           [f"pB{i2}", 'Vtb'], [f"ps{4 + hh}"])
                epilogue(blk)
            fw.barrier()

    def stage_band(l, b, oT_dil, oT_win):
        sl12 = alibi(12)
        sl8 = alibi(8)
        with ExitStack() as es:
            tmp = mk_hn_tmp(es)
            posq = sb(es, "posq", [128, S], F32)
            posq_i = sb(es, "posq_i", [128, S], I32)
            hcbufs = [sb(es, f"hc{i}", [128, 8, 512], BF16) for i in range(2)]
            acc = sb(es, "acc", [128, 16, 4, 65], F32)
            osb = sb(es, "osb", [128, 4, 64], BF16)
            rdn = sb(es, "rdn", [128, 4], F32)
            DMA('sp', posq_i[:], dr['pos'][b].partition_broadcast(128), [], ['posq_i'])
            V(lambda e: e.tensor_copy(out=posq[:], in_=posq_i[:]), ['posq_i'], ['posq'])

            def transpose_out(blk, oT, ft0):
                for i in range(2):
                    bk = nb(0, 4)
                    pt = f"ps{bk}"
                    pbf = ps[bk][:].bitcast(BF16)
                    PE(lambda e: e.transpose(pbf[:, 0:128], osb[:, 2 * i:2 * i + 2, :].rearrange("p h c -> p (h c)"), ident_b[:]),
                       ['osb', 'c_ident_b'], [pt])
                    V(lambda e: e.tensor_copy(out=oT[:, ft0 + i, blk * 128:(blk + 1) * 128], in_=pbf[:, 0:128]),
                      [], [pt, 'oT_band'])

            for g in range(3):
                def epi_dil(blk, g=g):
                    for hh in range(4):
                        if g == 0:
                            V(lambda e: e.tensor_copy(out=acc[:, blk, hh, :], in_=ps[4 + hh][:, 0:65]), [], [f"ps{4 + hh}", 'acc'])
                        else:
                            V(lambda e: e.tensor_tensor(out=acc[:, blk, hh, :], in0=acc[:, blk, hh, :], in1=ps[4 + hh][:, 0:65],
                                                        op=ALU.add), [], [f"ps{4 + hh}", 'acc'])
                    if g == 2:
                        V(lambda e: e.reciprocal(out=rdn[:, :], in_=acc[:, blk, :, 64]), ['acc'], ['rdn'])
                        V(lambda e: e.tensor_tensor(out=osb[:], in0=acc[:, blk, :, 0:64],
                                                    in1=rdn[:, :].unsqueeze(2).to_broadcast([128, 4, 64]), op=ALU.mult),
                          ['acc', 'rdn'], ['osb'])
                        transpose_out(blk, oT_dil, 0)
                q0 = OFF_DIL + g * 256
                band_group(l, b, es, tmp, (q0, q0 + 768, q0 + 1536), 4, pcol[0:64, 4:5], pcol[0:64, 5:6], DIL_R[g],
                           MASK_BASE[g], sl12[g * 4:(g + 1) * 4], epi_dil, posq, hcbufs, f"d{g}")
            for kvg in range(2):
                def epi_win(blk, kvg=kvg):
                    for hh in range(4):
                        V(lambda e: e.tensor_tensor(out=rdn[:, hh:hh + 1], in0=ps[4 + hh][:, 64:65],
                                                    in1=esink[:, kvg * 4 + hh:kvg * 4 + hh + 1], op=ALU.add),
                          ['esink'], [f"ps{4 + hh}", 'rdn'])
                    V(lambda e: e.reciprocal(out=rdn[:, :], in_=rdn[:, :]), ['rdn'], ['rdn'])
                    for hh in range(4):
                        V(lambda e: e.tensor_scalar(out=osb[:, hh, :], in0=ps[4 + hh][:, 0:64], scalar1=rdn[:, hh:hh + 1],
                                                    scalar2=None, op0=ALU.mult), ['rdn'], [f"ps{4 + hh}", 'osb'])
                    transpose_out(blk, oT_win, kvg * 2)
                band_group(l, b, es, tmp, (OFF_WIN + kvg * 256, OFF_WIN + 512 + kvg * 64, OFF_WIN + 640 + kvg * 64), 1,
                           pcol[0:64, 6:7], pcol[0:64, 7:8], 1, MASK_BASE[3], sl8[kvg * 4:(kvg + 1) * 4], epi_win,
                           posq, hcbufs, f"w{kvg}")
            fw.barrier()

    def stage_merge(l, b, oT_mla, oT_dil, oT_win):
        with ExitStack() as es:
            wg = [sb(es, f"wg{i}", [128, 8, D], BF16) for i in range(2)]
            wbr_m = sb(es, "wbr_m", [128, 4, D], BF16)
            wbr_d = sb(es, "wbr_d", [128, 2, D], BF16)
            wbr_w = sb(es, "wbr_w", [128, 4, D], BF16)
            wout = sb(es, "wout", [128, 8, D], BF16)
            hcb = sb(es, "hcm", [128, 8, 512], BF16)
            mgT = sb(es, "mgT", [128, 8, 512], BF16)
            mg = sb(es, "mg", [128, 8, 512], F32)
            sig = [sb(es, f"sig{i}", [128, 512], F32) for i in range(2)]
            tmpm = sb(es, "tmpm", [128, 512], F32)
            xt = [sb(es, f"xm{i}", [128, D], F32) for i in range(2)]
            win_v = dr['w_in'][l].rearrange("(kc p) n -> p kc n", p=128)
            load_w_cast(wbr_m[:], dr['w_br_mla'][l].rearrange("(kc p) n -> p kc n", p=128), 'wbr_m')
            load_w_cast(wbr_d[:], dr['w_br_dil'][l].rearrange("(kc p) n -> p kc n", p=128), 'wbr_d')
            load_w_cast(wbr_w[:], dr['w_br_win'][l].rearrange("(kc p) n -> p kc n", p=128), 'wbr_w')
            load_w_cast(wout[:], dr['w_out'][l].rearrange("(kc p) n -> p kc n", p=128), 'wout')
            srcs = ((oT_mla, 4, wbr_m, 'wbr_m', 'oT_mla'), (oT_dil, 2, wbr_d, 'wbr_d', 'oT_band'),
                    (oT_win, 4, wbr_w, 'wbr_w', 'oT_band'))
            it = 0
            for ch in range(4):
                cs_ = slice(ch * 512, (ch + 1) * 512)
                DMA('sp', hcb[:], hT_d[b][:, :, cs_], ['hT_d'], ['hcm'])
                for m in range(3):
                    wgb = wg[(ch * 3 + m) % 2]
                    wgt = f"wg{(ch * 3 + m) % 2}"
                    load_w_cast(wgb[:], win_v[:, :, OFF_GATE + m * D:OFF_GATE + (m + 1) * D], wgt)
                    oT, nk, wbr, wbrt, otok = srcs[m]
                    for ft in range(8):
                        i2 = it % 2
                        it += 1
                        bg, by = i2, 2 + i2
                        for kc in range(8):
                            MM(ps[bg][:, :], wgb[:, kc, ft * 128:(ft + 1) * 128], hcb[:, kc, :], kc == 0, kc == 7,
                               [wgt, 'hcm'], [f"ps{bg}"])
                        A(lambda e: e.activation(out=sig[i2][:], in_=ps[bg][:, :], func=AF.Sigmoid), [], [f"ps{bg}", f"sig{i2}"])
                        for k in range(nk):
                            MM(ps[by][:, :], wbr[:, k, ft * 128:(ft + 1) * 128], oT[:, k, cs_], k == 0, k == nk - 1,
                               [wbrt, otok], [f"ps{by}"])
                        if m == 0:
                            V(lambda e: e.tensor_tensor(out=mg[:, ft, :], in0=ps[by][:, :], in1=sig[i2][:], op=ALU.mult),
                              [f"sig{i2}"], [f"ps{by}", 'mg'])
                        else:
                            V(lambda e: e.tensor_tensor(out=tmpm[:], in0=ps[by][:, :], in1=sig[i2][:], op=ALU.mult),
                              [f"sig{i2}"], [f"ps{by}", 'tmpm'])
                            if m == 1:
                                G(lambda e: e.tensor_tensor(out=mg[:, ft, :], in0=mg[:, ft, :], in1=tmpm[:], op=ALU.add),
                                  ['tmpm', 'mg'], ['mg'])
                            else:
                                G(lambda e: e.tensor_tensor(out=mgT[:, ft, :], in0=mg[:, ft, :], in1=tmpm[:], op=ALU.add),
                                  ['tmpm', 'mg'], ['mgT'])
                for tt in range(4):
                    i2 = tt % 2
                    r0 = b * S + ch * 512 + tt * 128
                    DMA('sp', xt[i2][:], xsrc[0][r0:r0 + 128, :], ['xres'], [f"xm{i2}"])
                    for nh in range(2):
                        bk = 4 + (tt * 2 + nh) % 4
                        for ft in range(8):
                            MM(ps[bk][:, :], mgT[:, ft, tt * 128:(tt + 1) * 128], wout[:, ft, nh * 512:(nh + 1) * 512],
                               ft == 0, ft == 7, ['mgT', 'wout'], [f"ps{bk}"])
                        V(lambda e: e.tensor_tensor(out=tmpm[:], in0=ps[bk][:, :], in1=gt1b[:, b, nh * 512:(nh + 1) * 512],
                                                    op=ALU.mult), ['gt1b'], [f"ps{bk}", 'tmpm'])
                        G(lambda e: e.tensor_tensor(out=xt[i2][:, nh * 512:(nh + 1) * 512], in0=xt[i2][:, nh * 512:(nh + 1) * 512],
                                                    in1=tmpm[:], op=ALU.add), ['tmpm', f"xm{i2}"], [f"xm{i2}"])
                    DMA('sp', out[r0:r0 + 128, :], xt[i2][:], [f"xm{i2}"], ['xout'])
            fw.barrier()

    def stage_moe(l):
        w1v = dr['w1'].rearrange("l e (p k) n -> (l e p) (k n)", k=8)
        w3v = dr['w3'].rearrange("l e (p k) n -> (l e p) (k n)", k=8)
        w2v = dr['w2'].rearrange("l e (p c) n -> (l e p) (c n)", c=3)
        with ExitStack() as es:
            gates = sb(es, "gates", [128, NT, 2], F32)
            d1i = sb(es, "d1i", [128, NT], I32)
            d2i = sb(es, "d2i", [128, NT], I32)
            widx = sb(es, "widx", [128, NBLK], I32)
            widx4 = sb(es, "widx8", [128, 8, NBLK], I32)
            widx3 = sb(es, "widx3", [128, 3, NBLK], I32)
            gt2b = sb(es, "gt2b", [128, NSEQ, D], F32)
            DMA('sp', gt2b[:], bc_d[2].rearrange("b p n -> p b n"), ['bc_d'], ['gt2b'])
            with ExitStack() as e1:
                h2bf = sb(e1, "h2bf", [128, NT, D], BF16)
                A2b = sb(e1, "A2b", [128, NSEQ, D], F32)
                B2b = sb(e1, "B2b", [128, NSEQ, D], F32)
                DMA('sp', A2b[:], bc_d[1].rearrange("b p n -> p b n"), ['bc_d'], ['A2b'])
                DMA('sp', B2b[:], bc_d[0].rearrange("b p n -> p b n"), ['bc_d'], ['B2b'])
                wr = sb(e1, "wr", [128, 8, 36], F32)
                brow = sb(e1, "browr", [1, 36], F32)
                DMA('sp', wr[:], dr['w_r'][l].rearrange("(kc p) n -> p kc n", p=128), [], ['wr'])
                DMA('sp', brow[:], dr['b_r'][l], [], ['browr'])
                M1a = sb(e1, "M1a", [128, NT, 32], F32)
                M2a = sb(e1, "M2a", [128, NT, 32], F32)
                xt = [sb(e1, f"xe{i}", [128, D], F32) for i in range(2)]
                h2 = [sb(e1, f"h2{i}", [128, D], F32) for i in range(2)]
                h2T = [sb(e1, f"h2T{i}", [128, 8, 128], F32) for i in range(2)]
                junk = sb(e1, "junkE", [128, D], BF16)
                st = [sb(e1, f"stE{i}", [128, 4], F32) for i in range(2)]
                rt = sb(e1, "rt", [128, 64], F32)
                r2 = sb(e1, "r2", [128, 64], F32)
                for t in range(NT):
                    i = t % 2
                    b = t // 16
                    r0 = t * 128
                    DMA('sp', xt[i][:], out[r0:r0 + 128, :], ['xout'], [f'xe{i}'])
                    A(lambda e: e.activation(out=junk[:], in_=xt[i][:], func=AF.Square, accum_out=st[i][:, 0:1]),
                      [f'xe{i}'], ['junkE', f'stE{i}'])
                    A(lambda e: e.activation(out=st[i][:, 1:2], in_=st[i][:, 0:1], func=AF.Ln, scale=1.0 / D, bias=col[:, 1:2]),
                      [f'stE{i}', 'c_col'], [f'stE{i}'])
                    A(lambda e: e.activation(out=st[i][:, 2:3], in_=st[i][:, 1:2], func=AF.Exp, scale=-0.5),
                      [f'stE{i}'], [f'stE{i}'])
                    V(lambda e: e.scalar_tensor_tensor(out=h2[i][:], in0=xt[i][:], scalar=st[i][:, 2:3], in1=A2b[:, b, :],
                                                       op0=ALU.mult, op1=ALU.mult), [f'xe{i}', f'stE{i}', 'A2b'], [f'h2{i}'])
                    G(lambda e: e.tensor_tensor(out=h2[i][:], in0=h2[i][:], in1=B2b[:, b, :], op=ALU.add), [f'h2{i}', 'B2b'], [f'h2{i}'])
                    A(lambda e: e.copy(out=h2bf[:, t, :], in_=h2[i][:]), [f'h2{i}'], ['h2bf'])
                    for half in range(2):
                        bk = nb(0, 4)
                        pt = f"ps{bk}"
                        for q4 in range(4):
                            kc = half * 4 + q4
                            PE(lambda e: e.transpose(ps[bk][:, q4 * 128:(q4 + 1) * 128], h2[i][:, kc * 128:(kc + 1) * 128], ident_f[:]),
                               [f'h2{i}', 'c_ident_f'], [pt])
                        V(lambda e: e.tensor_copy(out=h2T[i][:, half * 4:half * 4 + 4, :],
                                                  in_=ps[bk][:, :].rearrange("p (q c) -> p q c", c=128)), [], [pt, f'h2T{i}'])
                    bk = nb(4, 8)
                    pt = f"ps{bk}"
                    for kc in range(8):
                        MM(ps[bk][:, 0:36], h2T[i][:, kc, :], wr[:, kc, :], kc == 0, False, [f'h2T{i}', 'wr'], [pt])
                    MM(ps[bk][:, 0:36], ones_f[0:1, 0:128], brow[0:1, :], False, True, ['ones_f', 'browr'], [pt])
                    lg = rt[:, 0:36]
                    gmax, ngmax, gsum, gw = rt[:, 36:37], rt[:, 37:38], rt[:, 38:39], rt[:, 39:40]
                    ohg, ge = rt[:, 40:44], rt[:, 44:48]
                    m1, m2, dd, ed, rr = rt[:, 48:49], rt[:, 49:50], rt[:, 50:51], rt[:, 51:52], rt[:, 52:53]
                    sel, mask1, sel2, mask2, t48 = r2[:, 0:8], r2[:, 8:16], r2[:, 16:24], r2[:, 24:32], r2[:, 32:64]
                    t48v = t48.rearrange("p (g e) -> p g e", e=8)
                    RT = ['rt']
                    V(lambda e: e.tensor_copy(out=lg, in_=ps[bk][:, 0:36]), [], [pt, 'rt'])
                    V(lambda e: e.tensor_reduce(out=gmax, in_=rt[:, 0:4], axis=AX.X, op=ALU.max), RT, RT)
                    V(lambda e: e.tensor_scalar(out=ohg, in0=rt[:, 0:4], scalar1=gmax, scalar2=None, op0=ALU.is_equal), RT, RT)
                    V(lambda e: e.tensor_scalar(out=ngmax, in0=gmax, scalar1=-1.0, scalar2=None, op0=ALU.mult), RT, RT)
                    A(lambda e: e.activation(out=ge, in_=rt[:, 0:4], func=AF.Exp, bias=ngmax, scale=1.0, accum_out=gsum), RT, RT)
                    V(lambda e: e.reciprocal(out=gw, in_=gsum), RT, RT)
                    V(lambda e: e.tensor_tensor(out=t48v, in0=rt[:, 4:36].rearrange("p (g e) -> p g e", e=8),
                                                in1=ohg.unsqueeze(2).to_broadcast([128, 4, 8]), op=ALU.mult), RT, RT)
                    V(lambda e: e.tensor_reduce(out=sel, in_=t48v.rearrange("p g e -> p e g"), axis=AX.X, op=ALU.add), RT, RT)
                    V(lambda e: e.tensor_reduce(out=m1, in_=sel, axis=AX.X, op=ALU.max), RT, RT)
                    V(lambda e: e.tensor_scalar(out=mask1, in0=sel, scalar1=m1, scalar2=None, op0=ALU.is_equal), RT, RT)
                    V(lambda e: e.scalar_tensor_tensor(out=sel2, in0=mask1, scalar=-1.0e30, in1=sel, op0=ALU.mult, op1=ALU.add), RT, RT)
                    V(lambda e: e.tensor_reduce(out=m2, in_=sel2, axis=AX.X, op=ALU.max), RT, RT)
                    V(lambda e: e.tensor_scalar(out=mask2, in0=sel2, scalar1=m2, scalar2=None, op0=ALU.is_equal), RT, RT)
                    V(lambda e: e.tensor_tensor(out=dd, in0=m2, in1=m1, op=ALU.subtract), RT, RT)
                    A(lambda e: e.activation(out=ed, in_=dd, func=AF.Exp), RT, RT)
                    V(lambda e: e.tensor_scalar(out=rr, in0=ed, scalar1=1.0, scalar2=None, op0=ALU.add), RT, RT)
                    V(lambda e: e.reciprocal(out=rr, in_=rr), RT, RT)
                    V(lambda e: e.tensor_tensor(out=gates[:, t, 0:1], in0=gw, in1=rr, op=ALU.mult), RT, ['gates'])
                    V(lambda e: e.tensor_tensor(out=gates[:, t, 1:2], in0=gw, in1=gates[:, t, 0:1], op=ALU.subtract),
                      RT + ['gates'], ['gates'])
                    V(lambda e: e.tensor_tensor(out=M1a[:, t, :].rearrange("p (g e) -> p g e", e=8),
                                                in0=ohg.unsqueeze(2).to_broadcast([128, 4, 8]),
                                                in1=mask1.unsqueeze(1).to_broadcast([128, 4, 8]), op=ALU.mult), RT, ['M1a'])
                    V(lambda e: e.tensor_tensor(out=M2a[:, t, :].rearrange("p (g e) -> p g e", e=8),
                                                in0=ohg.unsqueeze(2).to_broadcast([128, 4, 8]),
                                                in1=mask2.unsqueeze(1).to_broadcast([128, 4, 8]), op=ALU.mult), RT, ['M2a'])
                if stop_after in (('e1tiles', l), ('moe_e1tiles', l)):
                    dsb = sb(e1, "dsb", [128, 512], F32)
                    V(lambda e: e.memset(dsb[:], 0.0), [], ['dsb'])
                    V(lambda e: e.tensor_copy(out=dsb[:, 160:224], in_=gates[:].rearrange("p t k -> p (t k)")), ['gates'], ['dsb'])
                    V(lambda e: e.tensor_copy(out=dsb[:, 288:320], in_=M1a[:, 0, :]), ['M1a'], ['dsb'])
                    V(lambda e: e.tensor_copy(out=dsb[:, 320:352], in_=M2a[:, 0, :]), ['M2a'], ['dsb'])
                    V(lambda e: e.tensor_copy(out=dsb[:, 352:405], in_=rt[:, 0:53]), ['rt'], ['dsb'])
                    DMA('sp', dbg, dsb[:], ['dsb'], ['dbg'])
                    fw.barrier()
                    return
                Mt_bf = sb(e1, "Mt_bf", [128, NT * 32], BF16)
                CSs = sb(e1, "CSs", [128, NT, 32], F32)
                pref = sb(e1, "pref", [128, NT, 32], F32)
                destf = sb(e1, "destf", [128, NT, 32], F32)
                tmpd = sb(e1, "tmpd", [128, NT, 32], F32)
                cmp2 = sb(e1, "cmp2", [128, NBLK, 32], F32)
                sc = sb(e1, "scn", [128, 8, 32], F32)
                dkf = sb(e1, "dkf", [128, 2, NT], F32)
                bef = sb(e1, "bef", [128, NBLK], F32)
                SL = ['slot']
                V(lambda e: e.tensor_tensor(out=Mt_bf[:], in0=M1a[:].rearrange("p t e -> p (t e)"),
                                            in1=M2a[:].rearrange("p t e -> p (t e)"), op=ALU.add), ['M1a', 'M2a'], ['Mt_bf'])
                for hf in range(2):
                    MM(ps[hf][:, :], ustrict[:, :], Mt_bf[:, hf * 512:(hf + 1) * 512], True, True, ['Mt_bf', 'c_ustrict_b'], [f"ps{hf}"])
                    MM(ps[2 + hf][:, :], ones_b[:, :], Mt_bf[:, hf * 512:(hf + 1) * 512], True, True, ['Mt_bf', 'c_ones_b'], [f"ps{2 + hf}"])
                    V(lambda e: e.tensor_copy(out=CSs[:, hf * 16:(hf + 1) * 16, :].rearrange("p t e -> p (t e)"), in_=ps[2 + hf][:, :]),
                      [], [f"ps{2 + hf}"] + SL)
                V(lambda e: e.memset(pref[:, 0, :], 0.0), SL, SL)
                for t in range(1, NT):
                    V(lambda e: e.tensor_tensor(out=pref[:, t, :], in0=pref[:, t - 1, :], in1=CSs[:, t - 1, :], op=ALU.add), SL, SL)
                cnts, nblk_, padd, incA, incB, startp = sc[:, 0, :], sc[:, 1, :], sc[:, 2, :], sc[:, 3, :], sc[:, 4, :], sc[:, 5, :]
                V(lambda e: e.tensor_tensor(out=cnts, in0=pref[:, NT - 1, :], in1=CSs[:, NT - 1, :], op=ALU.add), SL, SL)
                c3 = cmp2[:, 0:32, :]
                V(lambda e: e.tensor_tensor(out=c3, in0=cnts.unsqueeze(2).to_broadcast([128, 32, 32]),
                                            in1=bstart[:, 0:32].unsqueeze(1).to_broadcast([128, 32, 32]), op=ALU.is_gt),
                  SL + ['c_bstart'], SL)
                V(lambda e: e.tensor_reduce(out=nblk_, in_=c3, axis=AX.X, op=ALU.add), SL, SL)
                V(lambda e: e.tensor_scalar(out=padd, in0=nblk_, scalar1=128.0, scalar2=None, op0=ALU.mult), SL, SL)
                V(lambda e: e.tensor_copy(out=incA, in_=padd), SL, SL)
                cur, nxt = incA, incB
                for s_ in (1, 2, 4, 8, 16):
                    V(lambda e: e.tensor_copy(out=nxt[:, 0:s_], in_=cur[:, 0:s_]), SL, SL)
                    V(lambda e: e.tensor_tensor(out=nxt[:, s_:32], in0=cur[:, s_:32], in1=cur[:, 0:32 - s_], op=ALU.add), SL, SL)
                    cur, nxt = nxt, cur
                endp = cur
                V(lambda e: e.tensor_tensor(out=startp, in0=endp, in1=padd, op=ALU.subtract), SL, SL)
                V(lambda e: e.tensor_tensor(out=tmpd[:], in0=pref[:], in1=startp.unsqueeze(1).to_broadcast([128, NT, 32]),
                                            op=ALU.add), SL, SL)
                for hf in range(2):
                    V(lambda e: e.tensor_tensor(out=destf[:, hf * 16:(hf + 1) * 16, :].rearrange("p t e -> p (t e)"),
                                                in0=tmpd[:, hf * 16:(hf + 1) * 16, :].rearrange("p t e -> p (t e)"),
                                                in1=ps[hf][:, :], op=ALU.add), SL, [f"ps{hf}"] + SL)
                for k_, (Ma, Mtok, dki) in enumerate(((M1a, 'M1a', d1i), (M2a, 'M2a', d2i))):
                    V(lambda e: e.tensor_tensor(out=tmpd[:], in0=destf[:], in1=Ma[:], op=ALU.mult), SL + [Mtok], SL)
                    V(lambda e: e.tensor_reduce(out=dkf[:, k_, :], in_=tmpd[:], axis=AX.X, op=ALU.add), SL, SL)
                    V(lambda e: e.tensor_copy(out=dki[:], in_=dkf[:, k_, :]), SL, ['dki'])
                V(lambda e: e.tensor_tensor(out=cmp2[:], in0=endp.unsqueeze(1).to_broadcast([128, NBLK, 32]),
                                            in1=bstart[:, :].unsqueeze(2).to_broadcast([128, NBLK, 32]), op=ALU.is_le),
                  SL + ['c_bstart'], SL)
                V(lambda e: e.tensor_reduce(out=bef[:], in_=cmp2[:], axis=AX.X, op=ALU.add), SL, SL)
                V(lambda e: e.tensor_scalar(out=bef[:], in0=bef[:], scalar1=31.0, scalar2=128.0, op0=ALU.min, op1=ALU.mult), SL, SL)
                V(lambda e: e.tensor_scalar(out=bef[:], in0=bef[:], scalar1=col[:, 5:6], scalar2=float(l * NE * 128),
                                            op0=ALU.add, op1=ALU.add), SL + ['c_col'], SL)
                V(lambda e: e.tensor_copy(out=widx[:], in_=bef[:]), SL, ['widx'])
                for q_ in range(8):
                    V(lambda e: e.tensor_scalar(out=widx4[:, q_, :], in0=bef[:], scalar1=8.0, scalar2=float(q_),
                                                op0=ALU.mult, op1=ALU.add), SL, ['widx'])
                for c_ in range(3):
                    V(lambda e: e.tensor_scalar(out=widx3[:, c_, :], in0=bef[:], scalar1=3.0, scalar2=float(c_),
                                                op0=ALU.mult, op1=ALU.add), SL, ['widx'])
                if stop_after in (('slots', l), ('moe_slots', l)):
                    dsb = sb(e1, "dsb", [128, 512], F32)
                    V(lambda e: e.memset(dsb[:], 0.0), [], ['dsb'])
                    V(lambda e: e.tensor_copy(out=dsb[:, 0:32], in_=d1i[:]), ['dki'], ['dsb'])
                    V(lambda e: e.tensor_copy(out=dsb[:, 32:64], in_=d2i[:]), ['dki'], ['dsb'])
                    V(lambda e: e.tensor_copy(out=dsb[:, 64:160], in_=widx[:]), ['widx'], ['dsb'])
                    V(lambda e: e.tensor_copy(out=dsb[:, 160:224], in_=gates[:].rearrange("p t k -> p (t k)")), ['gates'], ['dsb'])
                    V(lambda e: e.tensor_copy(out=dsb[:, 224:256], in_=cnts), SL, ['dsb'])
                    V(lambda e: e.tensor_copy(out=dsb[:, 256:288], in_=startp), SL, ['dsb'])
                    V(lambda e: e.tensor_copy(out=dsb[:, 288:320], in_=M1a[:, 0, :]), ['M1a'], ['dsb'])
                    V(lambda e: e.tensor_copy(out=dsb[:, 320:352], in_=M2a[:, 0, :]), ['M2a'], ['dsb'])
                    V(lambda e: e.tensor_copy(out=dsb[:, 352:405], in_=rt[:, 0:53]), ['rt'], ['dsb'])
                    DMA('sp', dbg, dsb[:], ['dsb'], ['dbg'])
                    fw.barrier()
                    return
                for t in range(NT):
                    for dki in (d1i, d2i):
                        fw.throttle('pool', 'd_rows_d', 2)
                        fw.dma('pool', lambda e: e.indirect_dma_start(
                            out=rows_d, out_offset=bass.IndirectOffsetOnAxis(ap=dki[:, t:t + 1], axis=0),
                            in_=h2bf[:, t, :], in_offset=None), ['h2bf', 'dki'], ['rows_d'])
                fw.barrier()
            with ExitStack() as e2:
                Xb = [sb(e2, f"Xb{i}", [128, D], BF16) for i in range(2)]
                XT = [sb(e2, f"XT{i}", [128, 8, 128], BF16) for i in range(2)]
                w1b = [sb(e2, f"w1b{i}", [128, 8, DE], BF16) for i in range(2)]
                w3b = [sb(e2, f"w3b{i}", [128, 8, DE], BF16) for i in range(2)]
                w2b = [sb(e2, f"w2b{i}", [128, 3, D], BF16) for i in range(2)]
                stg = [[sb(e2, f"stg{j}_{i}", [128, 3072], F32) for j in range(3)] for i in range(2)]
                s1 = sb(e2, "s1", [128, 384], F32)
                actT = sb(e2, "actT", [128, 3, 128], BF16)
                ysb = [sb(e2, f"ysb{i}", [128, D], F32) for i in range(2)]

                def issue_loads(bi):
                    i = bi % 2
                    DMA('sp', Xb[i][:], rows_d[bi * 128:(bi + 1) * 128, :], ['rows_d'], [f"Xb{i}"])
                    wsem = f'd_wg{i}'
                    off = bass.IndirectOffsetOnAxis(ap=widx[:, bi:bi + 1], axis=0)
                    for j, wv_ in enumerate((w1v, w3v, w2v)):
                        fw.dma('pool', lambda e: e.indirect_dma_start(out=stg[i][j][:], out_offset=None, in_=wv_, in_offset=off),
                               ['widx'], [f"stg{j}_{i}"], sem=wsem)

                issue_loads(0)
                for bi in range(NBLK):
                    i = bi % 2
                    if bi + 1 < NBLK:
                        issue_loads(bi + 1)
                    A(lambda e: e.copy(out=w1b[i][:].rearrange("p k n -> p (k n)"), in_=stg[i][0][:]), [f"stg0_{i}"], [f"w1b{i}"])
                    V(lambda e: e.tensor_copy(out=w3b[i][:].rearrange("p k n -> p (k n)"), in_=stg[i][1][:]), [f"stg1_{i}"], [f"w3b{i}"])
                    V(lambda e: e.tensor_copy(out=w2b[i][:].rearrange("p c n -> p (c n)"), in_=stg[i][2][:]), [f"stg2_{i}"], [f"w2b{i}"])
                    xv = Xb[i][:].rearrange("s (p k) -> s p k", k=8)
                    for half in range(2):
                        bk = nb(0, 2)
                        pt = f"ps{bk}"
                        pbf = ps[bk][:].bitcast(BF16)
                        for q4 in range(4):
                            kc = half * 4 + q4
                            PE(lambda e: e.transpose(pbf[:, q4 * 128:(q4 + 1) * 128], xv[:, :, kc], ident_b[:]),
                               [f"Xb{i}", 'c_ident_b'], [pt])
                        V(lambda e: e.tensor_copy(out=XT[i][:, half * 4:half * 4 + 4, :],
                                                  in_=pbf[:, 0:512].rearrange("p (q c) -> p q c", c=128)), [], [pt, f"XT{i}"])
                    b1, b3 = 2 + (bi % 2) * 2, 3 + (bi % 2) * 2
                    for (wb_, wt_, bk) in ((w1b[i], f"w1b{i}", b1), (w3b[i], f"w3b{i}", b3)):
                        for c in range(3):
                            for kc in range(8):
                                MM(ps[bk][:, c * 128:(c + 1) * 128], wb_[:, kc, :].rearrange("p (q c) -> p q c", c=3)[:, :, c],
                                   XT[i][:, kc, :], kc == 0, kc == 7, [wt_, f"XT{i}"], [f"ps{bk}"])
                    A(lambda e: e.activation(out=s1[:], in_=ps[b1][:, 0:384], func=AF.Silu), [], [f"ps{b1}", 's1'])
                    V(lambda e: e.tensor_tensor(out=actT[:].rearrange("p c s -> p (c s)"), in0=ps[b3][:, 0:384], in1=s1[:], op=ALU.mult),
                      ['s1'], [f"ps{b3}", 'actT'])
                    for nh in range(2):
                        bk = 6 + nh
                        for c in range(3):
                            MM(ps[bk][:, :], actT[:, c, :], w2b[i][:, c, nh * 512:(nh + 1) * 512], c == 0, c == 2,
                               ['actT', f"w2b{i}"], [f"ps{bk}"])
                        if nh == 0:
                            A(lambda e: e.copy(out=ysb[i][:, 0:512], in_=ps[bk][:, :]), [], [f"ps{bk}", f"ysb{i}"])
                        else:
                            V(lambda e: e.tensor_copy(out=ysb[i][:, 512:1024], in_=ps[bk][:, :]), [], [f"ps{bk}", f"ysb{i}"])
                    DMA('sp', y_d[bi * 128:(bi + 1) * 128, :], ysb[i][:], [f"ysb{i}"], ['y_d'])
                    if stop_after == ('moe_blk0', l) and bi == 0:
                        dd_ = sb(e2, "dd_", [128, D], F32)
                        V(lambda e: e.tensor_copy(out=dd_[:], in_=Xb[0][:]), ['Xb0'], ['dd_'])
                        DMA('sp', out[0:128, :], dd_[:], ['dd_'], ['dbgo'])
                        V(lambda e: e.memset(dd_[:], 0.0), [], ['dd_'])
                        V(lambda e: e.tensor_copy(out=dd_[:, 0:768], in_=w1b[0][:, 0:2, :].rearrange("p a b -> p (a b)")), ['w1b0'], ['dd_'])
                        DMA('sp', out[128:256, :], dd_[:], ['dd_'], ['dbgo'])
                        V(lambda e: e.tensor_copy(out=dd_[:], in_=w2b[0][:, 0, :]), ['w2b0'], ['dd_'])
                        DMA('sp', out[256:384, :], dd_[:], ['dd_'], ['dbgo'])
                        DMA('sp', out[384:512, :], ysb[0][:], ['ysb0'], ['dbgo'])
                        V(lambda e: e.memset(dd_[:], 0.0), [], ['dd_'])
                        V(lambda e: e.tensor_copy(out=dd_[:, 0:32], in_=d1i[:]), ['dki'], ['dd_'])
                        V(lambda e: e.tensor_copy(out=dd_[:, 32:64], in_=d2i[:]), ['dki'], ['dd_'])
                        V(lambda e: e.tensor_copy(out=dd_[:, 64:160], in_=widx[:]), ['widx'], ['dd_'])
                        V(lambda e: e.tensor_copy(out=dd_[:, 160:256], in_=widx4[:, 1, :]), ['widx'], ['dd_'])
                        V(lambda e: e.tensor_copy(out=dd_[:, 256:640], in_=actT[:].rearrange("p c s -> p (c s)")), ['actT'], ['dd_'])
                        V(lambda e: e.tensor_copy(out=dd_[:, 640:768], in_=XT[0][:, 3, :]), ['XT0'], ['dd_'])
                        DMA('sp', out[640:768, :], dd_[:], ['dd_'], ['dbgo'])
                        fw.barrier()
                        return
                fw.barrier()
            with ExitStack() as e3:
                xt = [sb(e3, f"xc{i}", [128, D], F32) for i in range(2)]
                y1 = [sb(e3, f"y1{i}", [128, D], F32) for i in range(2)]
                y2 = [sb(e3, f"y2{i}", [128, D], F32) for i in range(2)]
                for t in range(NT):
                    i = t % 2
                    b = t // 16
                    r0 = t * 128
                    DMA('sp', xt[i][:], out[r0:r0 + 128, :], ['xout'], [f"xc{i}"])
                    fw.throttle('pool', 'd_yg', 2)
                    fw.dma('pool', lambda e: e.indirect_dma_start(out=y1[i][:], out_offset=None, in_=y_d,
                                                                  in_offset=bass.IndirectOffsetOnAxis(ap=d1i[:, t:t + 1], axis=0)),
                           ['y_d', 'dki'], [f"y1{i}"], sem='d_yg')
                    fw.throttle('pool', 'd_yg', 2)
                    fw.dma('pool', lambda e: e.indirect_dma_start(out=y2[i][:], out_offset=None, in_=y_d,
                                                                  in_offset=bass.IndirectOffsetOnAxis(ap=d2i[:, t:t + 1], axis=0)),
                           ['y_d', 'dki'], [f"y2{i}"], sem='d_yg')
                    V(lambda e: e.tensor_scalar(out=y1[i][:], in0=y1[i][:], scalar1=gates[:, t, 0:1], scalar2=None, op0=ALU.mult),
                      [f"y1{i}", 'gates'], [f"y1{i}"])
                    V(lambda e: e.scalar_tensor_tensor(out=y1[i][:], in0=y2[i][:], scalar=gates[:, t, 1:2], in1=y1[i][:],
                                                       op0=ALU.mult, op1=ALU.add), [f"y1{i}", f"y2{i}", 'gates'], [f"y1{i}"])
                    G(lambda e: e.tensor_tensor(out=y1[i][:], in0=y1[i][:], in1=gt2b[:, b, :], op=ALU.mult), [f"y1{i}", 'gt2b'], [f"y1{i}"])
                    G(lambda e: e.tensor_tensor(out=xt[i][:], in0=xt[i][:], in1=y1[i][:], op=ALU.add), [f"y1{i}", f"xc{i}"], [f"xc{i}"])
                    DMA('sp', out[r0:r0 + 128, :], xt[i][:], [f"xc{i}"], ['xout2'])
                fw.barrier()

    done = False
    if moe_only:
        stage0(0)
        with ExitStack() as cp:
            xc_ = [sb(cp, f"xcp{i}", [128, D], F32) for i in range(2)]
            for t in range(NT):
                DMA('sp', xc_[t % 2][:], dr['x'][t * 128:(t + 1) * 128, :], [], [f"xcp{t % 2}"])
                DMA('sp', out[t * 128:(t + 1) * 128, :], xc_[t % 2][:], [f"xcp{t % 2}"], ['xout'])
            fw.barrier()
        stage_moe(0)
        nlayers = 0
    for l in range(nlayers):
        stage0(l)
        if stop_after == ('stage0', l):
            break
        for b in seqs:
            with ExitStack() as ms:
                oT_mla = sb(ms, "oT_mla", [128, 4, S], BF16)
                oT_dil = sb(ms, "oT_dil", [128, 2, S], BF16)
                oT_win = sb(ms, "oT_win", [128, 4, S], BF16)
                stageA(l, b)
                if stop_after in (('A', l), ('A', l, b)):
                    done = True
                    break
                stage_mla(l, b, oT_mla)
                if stop_after in (('mla', l), ('mla', l, b)):
                    done = True
                    break
                stage_band(l, b, oT_dil, oT_win)
                if stop_after in (('band', l), ('band', l, b)):
                    done = True
                    break
                stage_merge(l, b, oT_mla, oT_dil, oT_win)
                if stop_after in (('merge', l), ('merge', l, b)):
                    done = True
                    break
        if done:
            break
        xsrc[0] = out
        if stop_after == ('mixer', l):
            break
        stage_moe(l)
        if stop_after in (('layer', l), ('slots', l), ('e1tiles', l)):
            break
    fw.barrier()
    top.close()
    build_program.stats = (fw.ninst, fw.nwaits, len(fw.semobj))
    return nc


def kernel(**inputs):
    inputs = {k: np.asarray(v) for k, v in inputs.items()}
    shared = host_shared(inputs)
    in_maps = []
    for core in range(8):
        m = dict(shared)
        m.update(host_layout(inputs, core))
        in_maps.append(m)
    nc = build_program()
    res = run_bass_kernel_spmd(nc, in_maps, core_ids=list(range(8)))
    outs = [np.asarray(r["out"]).reshape(NSEQ, S, D) for r in res.results]
    return np.concatenate(outs, axis=0).astype(np.float32)
```
